# Optimizing a Trainium2 kernel written in Bass

```python
import math
import numpy as np
import jax, jax.numpy as jnp
from jax import lax

D_MODEL = 1024
BATCH = 16
SEQ = 2048
DEPTH = 2

HEAD_DIM = 64
ROT_DIM = HEAD_DIM // 4
ROPE_THETA = 500000.0
EPS = 1e-6
Q_BLOCK = 128
A_HEADS = 4
IDX_HEADS = 8
IDX_DIM = 64
INDEX_TOPK = 256
B_HEADS = 4
MOBA_BLOCK = 256
MOBA_TOPK = 3
MOBA_Q_CHUNK = 16
C_HEADS = 4
C_VDIM = 2 * HEAD_DIM
D_FF = 2816

WIDTH_A = A_HEADS * HEAD_DIM
WIDTH_B = B_HEADS * HEAD_DIM
WIDTH_C = C_HEADS * C_VDIM
MIX_WIDTH = WIDTH_A + WIDTH_B + WIDTH_C
IN_SPLITS = (A_HEADS * HEAD_DIM, HEAD_DIM, HEAD_DIM, IDX_HEADS * IDX_DIM, IDX_DIM, IDX_HEADS,
             B_HEADS * HEAD_DIM, B_HEADS * HEAD_DIM, B_HEADS * HEAD_DIM,
             C_HEADS * 2 * HEAD_DIM, C_HEADS * 2 * HEAD_DIM, C_HEADS * C_VDIM,
             D_MODEL, D_MODEL, D_MODEL)
IN_WIDTH = sum(IN_SPLITS)

kernel_name = 'hybrid_dsa_moba_diffattn_macaron'


def rmsnorm(x, g):
    xf = x.astype(jnp.float32)
    y = xf * lax.rsqrt(jnp.mean(xf * xf, axis=-1, keepdims=True) + EPS)
    return y.astype(x.dtype) * g


def rope_tables(positions):
    inv_freq = jnp.power(ROPE_THETA, -jnp.arange(0, ROT_DIM, 2, dtype=jnp.float32) / ROT_DIM)
    ang = positions.astype(jnp.float32)[..., None] * inv_freq
    return jnp.cos(ang), jnp.sin(ang)


def rope(x, cos, sin):
    shape = cos.shape[:2] + (1,) * (x.ndim - 3) + cos.shape[-1:]
    c = cos.reshape(shape).astype(x.dtype)
    s = sin.reshape(shape).astype(x.dtype)
    half = ROT_DIM // 2
    x1, x2 = x[..., :half], x[..., half:ROT_DIM]
    return jnp.concatenate([x1 * c - x2 * s, x2 * c + x1 * s, x[..., ROT_DIM:]], axis=-1)


def masked_softmax(scores, mask):
    return jax.nn.softmax(jnp.where(mask, scores.astype(jnp.float32), -jnp.inf), axis=-1)


def swiglu(h, w_gate, w_up, w_down):
    return (jax.nn.silu(h @ w_gate) * (h @ w_up)) @ w_down


def dsa_attention(q, k, v, qi, ki, wi):
    B, S = q.shape[:2]
    topk = min(INDEX_TOPK, S // 4)
    nb = S // Q_BLOCK
    s_pos = jnp.arange(S)
    b_idx = jnp.arange(B)[:, None, None]
    scale = HEAD_DIM ** -0.5

    def blk(t):
        return t.reshape((B, nb, Q_BLOCK) + t.shape[2:]).swapaxes(0, 1)

    def body(args):
        qb, qib, wib, start = args
        t = start + jnp.arange(Q_BLOCK)
        causal = s_pos[None, :] <= t[:, None]
        logits = jnp.einsum('bqhd,bsd->bqhs', qib, ki)
        iscore = jnp.einsum('bqhs,bqh->bqs', jax.nn.relu(logits).astype(jnp.float32), wib.astype(jnp.float32))
        iscore = jnp.where(causal[None], iscore, -jnp.inf)
        _, idx = lax.top_k(iscore, topk)
        k_sel = k[b_idx, idx]
        v_sel = v[b_idx, idx]
        sc = jnp.einsum('bqhd,bqkd->bhqk', qb, k_sel) * scale
        valid = (idx <= t[None, :, None])[:, None]
        p = masked_softmax(sc, valid).astype(v.dtype)
        return jnp.einsum('bhqk,bqkd->bqhd', p, v_sel)

    starts = jnp.arange(nb) * Q_BLOCK
    out = lax.map(body, (blk(q), blk(qi), blk(wi), starts))
    return out.swapaxes(0, 1).reshape(B, S, -1)


def moba_attention(q, k, v):
    B, S, H, d = q.shape
    nblk = -(-S // MOBA_BLOCK)
    pad = nblk * MOBA_BLOCK - S
    scale = d ** -0.5

    def to_blocks(t):
        t = jnp.pad(t, ((0, 0), (0, pad), (0, 0), (0, 0)))
        return t.reshape(B, nblk, MOBA_BLOCK, H, d).transpose(0, 3, 1, 2, 4)

    kbt, vbt = to_blocks(k), to_blocks(v)
    kmean = jnp.mean(kbt.astype(jnp.float32), axis=3).astype(k.dtype)
    n_sel = min(MOBA_TOPK, nblk)
    nc = S // MOBA_Q_CHUNK
    q_chunks = q.reshape(B, nc, MOBA_Q_CHUNK, H, d).transpose(1, 0, 3, 2, 4)
    b_idx = jnp.arange(B)[:, None, None, None]
    h_idx = jnp.arange(H)[None, :, None, None]
    blk_ids = jnp.arange(nblk)
    own_offsets = jnp.arange(MOBA_BLOCK)

    def body(args):
        qb, start = args
        t = start + jnp.arange(MOBA_Q_CHUNK)
        j = start // MOBA_BLOCK
        gate = jnp.einsum('bhqd,bhnd->bhqn', qb, kmean).astype(jnp.float32)
        gate = jnp.where(blk_ids < j, gate, -jnp.inf)
        _, sel = lax.top_k(gate, n_sel)
        sel_valid = sel < j
        k_sel = kbt[b_idx, h_idx, sel]
        v_sel = vbt[b_idx, h_idx, sel]
        s_sel = jnp.einsum('bhqd,bhqnkd->bhqnk', qb, k_sel) * scale
        s_sel = jnp.where(sel_valid[..., None], s_sel.astype(jnp.float32), -jnp.inf)
        s_sel = s_sel.reshape(B, H, MOBA_Q_CHUNK, n_sel * MOBA_BLOCK)
        k_own = lax.dynamic_index_in_dim(kbt, j, axis=2, keepdims=False)
        v_own = lax.dynamic_index_in_dim(vbt, j, axis=2, keepdims=False)
        s_own = jnp.einsum('bhqd,bhkd->bhqk', qb, k_own) * scale
        own_causal = (j * MOBA_BLOCK + own_offsets)[None, :] <= t[:, None]
        s_own = jnp.where(own_causal, s_own.astype(jnp.float32), -jnp.inf)
        p = jax.nn.softmax(jnp.concatenate([s_sel, s_own], axis=-1), axis=-1).astype(v.dtype)
        p_sel = p[..., :n_sel * MOBA_BLOCK].reshape(B, H, MOBA_Q_CHUNK, n_sel, MOBA_BLOCK)
        p_own = p[..., n_sel * MOBA_BLOCK:]
        return (jnp.einsum('bhqnk,bhqnkd->bhqd', p_sel, v_sel)
                + jnp.einsum('bhqk,bhkd->bhqd', p_own, v_own))

    starts = jnp.arange(nc) * MOBA_Q_CHUNK
    out = lax.map(body, (q_chunks, starts))
    return out.transpose(1, 0, 3, 2, 4).reshape(B, S, H * d)


def diff_attention(q, k, v, lam, lam_init, subln_g):
    B, S, H = q.shape[:3]
    nb = S // Q_BLOCK
    s_pos = jnp.arange(S)
    scale = HEAD_DIM ** -0.5
    q_blocks = q.reshape(B, nb, Q_BLOCK, H, 2, HEAD_DIM).swapaxes(0, 1)

    def body(args):
        qb, start = args
        t = start + jnp.arange(Q_BLOCK)
        causal = s_pos[None, :] <= t[:, None]
        sc = jnp.einsum('bqhcd,bshcd->bhcqs', qb, k) * scale
        p = masked_softmax(sc, causal)
        attn = (p[:, :, 0] - lam * p[:, :, 1]).astype(v.dtype)
        return jnp.einsum('bhqs,bshe->bqhe', attn, v)

    starts = jnp.arange(nb) * Q_BLOCK
    out = lax.map(body, (q_blocks, starts))
    out = out.swapaxes(0, 1).reshape(B, S, H, C_VDIM)
    out = rmsnorm(out, subln_g) * (1.0 - lam_init)
    return out.reshape(B, S, H * C_VDIM)


def hybrid_mixer(h, cos, sin, w_in, qk_g, lam_p, subln_g, w_branch, w_out, lam_init):
    B, S, _ = h.shape
    offsets = np.cumsum(IN_SPLITS)[:-1].tolist()
    proj = h @ w_in
    (qa, ka, va, qi, ki, wi, qb, kb, vb, qc, kc, vc, ga, gb, gc) = jnp.split(proj, offsets, axis=-1)
    qa = rope(rmsnorm(qa.reshape(B, S, A_HEADS, HEAD_DIM), qk_g[0]), cos, sin)
    ka = rope(rmsnorm(ka, qk_g[1]), cos, sin)
    qi = rope(qi.reshape(B, S, IDX_HEADS, IDX_DIM), cos, sin)
    ki = rope(ki, cos, sin)
    wi = wi * (IDX_HEADS * IDX_DIM) ** -0.5
    o_a = dsa_attention(qa, ka, va, qi, ki, wi)
    qb = rope(rmsnorm(qb.reshape(B, S, B_HEADS, HEAD_DIM), qk_g[2]), cos, sin)
    kb = rope(rmsnorm(kb.reshape(B, S, B_HEADS, HEAD_DIM), qk_g[3]), cos, sin)
    o_b = moba_attention(qb, kb, vb.reshape(B, S, B_HEADS, HEAD_DIM))
    qc = rope(rmsnorm(qc.reshape(B, S, C_HEADS, 2, HEAD_DIM), qk_g[4]), cos, sin)
    kc = rope(rmsnorm(kc.reshape(B, S, C_HEADS, 2, HEAD_DIM), qk_g[5]), cos, sin)
    lp = lam_p.astype(jnp.float32)
    lam = jnp.exp(jnp.sum(lp[0] * lp[1])) - jnp.exp(jnp.sum(lp[2] * lp[3])) + lam_init
    o_c = diff_attention(qc, kc, vc.reshape(B, S, C_HEADS, C_VDIM), lam, lam_init, subln_g)
    y_a = o_a @ w_branch[:WIDTH_A]
    y_b = o_b @ w_branch[WIDTH_A:WIDTH_A + WIDTH_B]
    y_c = o_c @ w_branch[WIDTH_A + WIDTH_B:]
    merged = jax.nn.sigmoid(ga) * y_a + jax.nn.sigmoid(gb) * y_b + jax.nn.sigmoid(gc) * y_c
    return merged @ w_out


def setup_inputs(seed: int = 0) -> dict:
    key = jax.random.key(seed)
    ks = jax.random.split(key, 14)
    x = jax.random.normal(ks[0], (BATCH, SEQ, D_MODEL), jnp.float32)
    offset = jax.random.randint(ks[1], (BATCH, 1), 0, 4096, dtype=jnp.int32)
    positions = (offset + jnp.arange(SEQ, dtype=jnp.int32)[None, :]).astype(jnp.int32)
    norm_g = 1.0 + 0.02 * jax.random.normal(ks[2], (DEPTH, 3, D_MODEL), jnp.float32)
    w_in = jax.random.normal(ks[3], (DEPTH, D_MODEL, IN_WIDTH), jnp.float32) * D_MODEL ** -0.5
    qk_norm_g = 1.0 + 0.02 * jax.random.normal(ks[4], (DEPTH, 6, HEAD_DIM), jnp.float32)
    lambda_params = 0.1 * jax.random.normal(ks[5], (DEPTH, 4, HEAD_DIM), jnp.float32)
    diff_subln_g = 1.0 + 0.02 * jax.random.normal(ks[6], (DEPTH, C_VDIM), jnp.float32)
    w_branch = jnp.concatenate([
        jax.random.normal(ks[7], (DEPTH, WIDTH_A, D_MODEL), jnp.float32) * WIDTH_A ** -0.5,
        jax.random.normal(ks[8], (DEPTH, WIDTH_B, D_MODEL), jnp.float32) * WIDTH_B ** -0.5,
        jax.random.normal(ks[9], (DEPTH, WIDTH_C, D_MODEL), jnp.float32) * WIDTH_C ** -0.5], axis=1)
    w_out = jax.random.normal(ks[10], (DEPTH, D_MODEL, D_MODEL), jnp.float32) * D_MODEL ** -0.5
    ffn_w_gate = jax.random.normal(ks[11], (DEPTH, 2, D_MODEL, D_FF), jnp.float32) * D_MODEL ** -0.5
    ffn_w_up = jax.random.normal(ks[12], (DEPTH, 2, D_MODEL, D_FF), jnp.float32) * D_MODEL ** -0.5
    ffn_w_down = jax.random.normal(ks[13], (DEPTH, 2, D_FF, D_MODEL), jnp.float32) * D_FF ** -0.5
    return {'x': x, 'positions': positions, 'norm_g': norm_g, 'w_in': w_in, 'qk_norm_g': qk_norm_g,
            'lambda_params': lambda_params, 'diff_subln_g': diff_subln_g, 'w_branch': w_branch,
            'w_out': w_out, 'ffn_w_gate': ffn_w_gate, 'ffn_w_up': ffn_w_up, 'ffn_w_down': ffn_w_down}


def reference(x, positions, norm_g, w_in, qk_norm_g, lambda_params, diff_subln_g, w_branch,
              w_out, ffn_w_gate, ffn_w_up, ffn_w_down):
    cos, sin = rope_tables(positions)
    for layer in range(DEPTH):
        lam_init = 0.8 - 0.6 * math.exp(-0.3 * layer)
        x = x + 0.5 * swiglu(rmsnorm(x, norm_g[layer, 0]), ffn_w_gate[layer, 0], ffn_w_up[layer, 0], ffn_w_down[layer, 0])
        x = x + hybrid_mixer(rmsnorm(x, norm_g[layer, 1]), cos, sin, w_in[layer], qk_norm_g[layer],
                             lambda_params[layer], diff_subln_g[layer], w_branch[layer], w_out[layer], lam_init)
        x = x + 0.5 * swiglu(rmsnorm(x, norm_g[layer, 2]), ffn_w_gate[layer, 1], ffn_w_up[layer, 1], ffn_w_down[layer, 1])
    return x
```

```python
import contextlib
import math
import numpy as np
import concourse.bass as bass
import concourse.mybir as mybir
from concourse.bass_utils import run_bass_kernel_spmd

F32 = mybir.dt.float32
BF16 = mybir.dt.bfloat16
I32 = mybir.dt.int32
AF = mybir.ActivationFunctionType
ALU = mybir.AluOpType
AX = mybir.AxisListType

S = 2048
D = 1024
NT = 16
DFF = 2816
EPS = 1e-6
NCORES = 8
OFF = dict(qa=0, ka=256, va=320, qi=384, ki=896, wi=960, qb=968, kb=1224, vb=1480,
           qc=1736, kc=2248, vc=2760, ga=3272, gb=4296, gc=5320)
NBIS = 20
C_ID, C_BD, C_RM, C_TRI, C_INVF, C_POW = 0, 128, 256, 384, 512, 513
NCST = C_POW + NBIS
NEG = -1.0e30
import os
SUB = int(os.environ.get('SUB', '9'))
PI = math.pi


class Trk:
    __slots__ = ("name", "w", "r")

    def __init__(self, name=""):
        self.name = name
        self.w = None
        self.r = []


class _Rec:
    def __init__(self):
        self.calls = []

    def __getattr__(self, name):
        def f(*a, **k):
            self.calls.append((name, a, k))
            return self
        return f


class Prog:
    ENG = ("tensor", "vector", "scalar", "gpsimd", "sync")

    def __init__(self, nc, n_dma_sems=24):
        self.nc = nc
        self.stack = []
        self.ops = {e: [] for e in self.ENG}
        self.sems = {}
        self.cnt = {}
        self.wm = {e: {} for e in self.ENG}
        for e in self.ENG:
            self._newsem(e)
        self.dma_keys = {}
        self.dma_rr = {}
        for q in ("sync", "gpsimd", "scalar"):
            self.dma_keys[q] = []
            self.dma_rr[q] = 0
            for i in range(n_dma_sems if q != "scalar" else 4):
                k = "dma_%s%d" % (q, i)
                self._newsem(k)
                self.dma_keys[q].append(k)
        self.nins = 0
        self.fill_reg = None

    def _newsem(self, key):
        cm = self.nc.semaphore(key)
        h = cm.__enter__()
        self.stack.append(cm)
        self.sems[key] = h
        self.cnt[key] = 0

    def _waits(self, eng, reads, writes):
        need = {}
        for t in reads:
            if t.w is not None:
                k, v = t.w
                if need.get(k, 0) < v:
                    need[k] = v
        for t in writes:
            if t.w is not None:
                k, v = t.w
                if need.get(k, 0) < v:
                    need[k] = v
            for (k, v) in t.r:
                if need.get(k, 0) < v:
                    need[k] = v
        out = []
        wm = self.wm[eng]
        for k, v in need.items():
            if wm.get(k, 0) < v:
                wm[k] = v
                out.append((k, v))
        return out

    def _mark(self, dep, reads, writes):
        for t in reads:
            t.r.append(dep)
        for t in writes:
            t.w = dep
            t.r = []

    def op(self, eng, fn, reads=(), writes=()):
        return self.group(eng, [fn], reads, writes)

    def group(self, eng, fns, reads=(), writes=()):
        waits = self._waits(eng, reads, writes)
        self.cnt[eng] += 1
        dep = (eng, self.cnt[eng])
        self._mark(dep, reads, writes)
        sem = self.sems[eng]
        sems = self.sems
        self.nins += len(fns)
        rec = _Rec()
        for f in fns:
            f(rec)
        calls = rec.calls

        def run(e, calls=calls, waits=waits, sem=sem):
            for (k, val) in waits:
                e.wait_ge(sems[k], val)
            last = None
            for (name, a, kw) in calls:
                if name == "affine_select":
                    kw = dict(kw)
                    if self.fill_reg is None:
                        self.fill_reg = e.to_reg(kw["fill"])
                    kw["fill"] = self.fill_reg
                last = getattr(e, name)(*a, **kw)
            last.then_inc(sem, 1)
        self.ops[eng].append(run)
        return dep

    def dma(self, q, out, in_, reads=(), writes=(), **kw):
        waits = self._waits(q, reads, writes)
        k = self.dma_keys[q][self.dma_rr[q] % len(self.dma_keys[q])]
        self.dma_rr[q] += 1
        prev = self.cnt[k]
        if prev > 0 and self.wm[q].get(k, 0) < prev:
            self.wm[q][k] = prev
            waits.append((k, prev))
        self.cnt[k] += 16
        dep = (k, self.cnt[k])
        self._mark(dep, reads, writes)
        sems = self.sems
        sem = sems[k]
        self.nins += 1

        def run(e, waits=waits, sem=sem, out=out, in_=in_, kw=kw):
            for (kk, val) in waits:
                e.wait_ge(sems[kk], val)
            e.dma_start(out=out, in_=in_, **kw).then_inc(sem, 16)
        self.ops[q].append(run)
        return dep

    def barrier(self):
        sems = self.sems
        snap = [(k, v) for k, v in self.cnt.items() if v > 0]
        for eng in self.ENG:
            waits = []
            for (k, v) in snap:
                if k == eng:
                    continue
                if self.wm[eng].get(k, 0) < v:
                    self.wm[eng][k] = v
                    waits.append((k, v))

            def run(e, waits=waits):
                for (k, val) in waits:
                    e.wait_ge(sems[k], val)
            self.ops[eng].append(run)

    def flush(self):
        nc = self.nc
        ops = self.ops
        with nc.Block() as block:
            @block.tensor
            def _(e):
                for f in ops["tensor"]:
                    f(e)

            @block.vector
            def _(e):
                for f in ops["vector"]:
                    f(e)

            @block.scalar
            def _(e):
                for f in ops["scalar"]:
                    f(e)

            @block.gpsimd
            def _(e):
                for f in ops["gpsimd"]:
                    f(e)

            @block.sync
            def _(e):
                for f in ops["sync"]:
                    f(e)
        self.ops = {e: [] for e in self.ENG}

    def close(self):
        for cm in reversed(self.stack):
            cm.__exit__(None, None, None)


def make_consts():
    c = np.zeros((128, NCST), np.float32)
    c[:, C_ID:C_ID + 128] = np.eye(128, dtype=np.float32)
    for p in range(128):
        for f in range(128):
            if p // 64 == f // 64:
                c[p, C_BD + f] = 1.0
    for f in range(128):
        r = f % 64
        if r < 8:
            c[f + 8, C_RM + f] = -1.0
        elif r < 16:
            c[f - 8, C_RM + f] = 1.0
    for k in range(128):
        c[k, C_TRI + k:C_TRI + 128] = 1.0
    inv = np.power(np.float32(500000.0), -np.arange(0, 16, 2, dtype=np.float32) / np.float32(16.0)).astype(np.float32)
    for p in range(128):
        r = p % 64
        c[p, C_INVF] = inv[r % 8] if r < 16 else 0.0
    for i in range(NBIS):
        c[:, C_POW + i] = 2.0 ** (-(i + 1))
    return c


def build(n_seq=2, layers=(0, 1), stage=99, dbg=False):
    nc = bass.Bass("TRN2", target_bir_lowering=False)
    dt = nc.dram_tensor
    x_d = dt("x", [n_seq, S, D], F32, kind="ExternalInput").ap()
    pos_d = dt("pos", [n_seq, S], I32, kind="ExternalInput").ap()
    win_d = dt("w_in", [2, D, 6344], F32, kind="ExternalInput").ap()
    wbr_d = dt("w_branch", [2, D, D], F32, kind="ExternalInput").ap()
    wout_d = dt("w_out", [2, D, D], F32, kind="ExternalInput").ap()
    wg_d = dt("ffn_w_gate", [2, 2, D, DFF], F32, kind="ExternalInput").ap()
    wu_d = dt("ffn_w_up", [2, 2, D, DFF], F32, kind="ExternalInput").ap()
    wd_d = dt("ffn_w_down", [2, 2, DFF, D], F32, kind="ExternalInput").ap()
    cst_d = dt("cst", [128, NCST], F32, kind="ExternalInput").ap()
    ngT_d = dt("ngT", [128, 48], F32, kind="ExternalInput").ap()
    qkgT_d = dt("qkgT", [128, 12], F32, kind="ExternalInput").ap()
    lam_d = dt("lamp", [512], F32, kind="ExternalInput").ap()
    sub_d = dt("subg", [256], F32, kind="ExternalInput").ap()
    out_d = dt("out", [n_seq, S, D], F32, kind="ExternalOutput").ap()
    if dbg:
        dbg_ot = dt("dbg_ot", [128, 8, S], BF16, kind="ExternalOutput").ap()

    P = Prog(nc)
    es0 = contextlib.ExitStack()

    uid = [0]

    def sbuf(es, name, shape, dtype):
        uid[0] += 1
        return es.enter_context(nc.sbuf_tensor("s%d_%s" % (uid[0], name), shape, dtype))

    X = sbuf(es0, "X", [128, NT, D], F32)
    CT = sbuf(es0, "CT", [128, S], BF16)
    STb = sbuf(es0, "ST", [128, S], BF16)
    cstf = sbuf(es0, "cstf", [128, NCST], F32)
    identb = sbuf(es0, "identb", [128, 128], BF16)
    BDb = sbuf(es0, "BDb", [128, 128], BF16)
    RMb = sbuf(es0, "RMb", [128, 128], BF16)
    trib = sbuf(es0, "trib", [128, 128], BF16)
    onesb = sbuf(es0, "onesb", [128, 128], BF16)
    ngT = sbuf(es0, "ngT", [128, 48], F32)
    qkgT = sbuf(es0, "qkgT", [128, 12], F32)
    subg = sbuf(es0, "subg", [128, 256], F32)
    lamv = sbuf(es0, "lamv", [128, 8], F32)
    rs_x = sbuf(es0, "rs_x", [128, 2 * NT], F32)
    PSB = [es0.enter_context(nc.psum_tensor("psb%d" % i, [128, 512], F32)) for i in range(7)]
    PSH = es0.enter_context(nc.psum_tensor("psh", [128, 1024], BF16))
    T_ps = [Trk("ps%d" % i) for i in range(7)]
    T_psh = Trk("psh")
    T_X = [Trk("X%d" % i) for i in range(NT)]
    T_cst = Trk("cst")
    T_tab = Trk("tab")
    T_out = Trk("out")
    T_lam = Trk("lam")

    def lam_init(l):
        return 0.8 - 0.6 * math.exp(-0.3 * l)

    P.dma("sync", cstf[:], cst_d, writes=[T_cst])
    P.dma("gpsimd", identb[:], cst_d[:, C_ID:C_ID + 128], writes=[T_cst])
    P.dma("gpsimd", BDb[:], cst_d[:, C_BD:C_BD + 128], writes=[T_cst])
    P.dma("gpsimd", RMb[:], cst_d[:, C_RM:C_RM + 128], writes=[T_cst])
    P.dma("gpsimd", trib[:], cst_d[:, C_TRI:C_TRI + 128], writes=[T_cst])
    P.dma("sync", ngT[:], ngT_d, writes=[T_cst])
    P.dma("sync", qkgT[:], qkgT_d, writes=[T_cst])
    P.dma("sync", subg[:], sub_d.partition_broadcast(128), writes=[T_cst])
    P.op("vector", lambda e: e.memset(onesb[:], 1.0), writes=[T_cst])
    with contextlib.ExitStack() as es:
        lamp = sbuf(es, "lamp", [128, 512], F32)
        tmp = sbuf(es, "lamtmp", [128, 512], F32)
        sums = sbuf(es, "lamsum", [128, 8], F32)
        T_t = Trk()
        P.dma("sync", lamp[:], lam_d.partition_broadcast(128), writes=[T_lam])
        for l in range(2):
            for j in range(2):
                a0 = l * 256 + (2 * j) * 64
                P.op("vector", lambda e, a0=a0: e.tensor_tensor(out=tmp[:, a0:a0 + 64], in0=lamp[:, a0:a0 + 64], in1=lamp[:, a0 + 64:a0 + 128], op=ALU.mult),
                     reads=[T_lam], writes=[T_t])
                P.op("vector", lambda e, a0=a0, l=l, j=j: e.reduce_sum(out=sums[:, 2 * l + j:2 * l + j + 1], in_=tmp[:, a0:a0 + 64], axis=AX.X),
                     reads=[T_t], writes=[T_t])
        P.op("scalar", lambda e: e.activation(out=sums[:, 4:8], in_=sums[:, 0:4], func=AF.Exp), reads=[T_t], writes=[T_t])
        for l in range(2):
            P.op("vector", lambda e, l=l: e.scalar_tensor_tensor(out=lamv[:, l:l + 1], in0=sums[:, 5 + 2 * l:6 + 2 * l], scalar=-lam_init(l),
                                                               in1=sums[:, 4 + 2 * l:5 + 2 * l], op0=ALU.add, op1=ALU.subtract),
                 reads=[T_t], writes=[T_lam])
        P.barrier()
        P.flush()

    def rope_tables(b):
        with contextlib.ExitStack() as es:
            posi = sbuf(es, "posi", [128, S], I32)
            a = sbuf(es, "ta", [128, S], F32)
            r = sbuf(es, "tr", [128, S], F32)
            ki = sbuf(es, "tki", [128, S], I32)
            kf = sbuf(es, "tkf", [128, S], F32)
            T = Trk()
            P.dma("sync", posi[:], pos_d[b].partition_broadcast(128), writes=[T])
            V = lambda fn: P.op("vector", fn, reads=[T, T_cst], writes=[T, T_tab])
            V(lambda e: e.tensor_copy(out=a[:], in_=posi[:]))
            V(lambda e: e.tensor_scalar(out=a[:], in0=a[:], scalar1=cstf[:, C_INVF:C_INVF + 1], scalar2=None, op0=ALU.mult))
            V(lambda e: e.tensor_scalar(out=r[:], in0=a[:], scalar1=float(1.0 / (2 * PI)), scalar2=None, op0=ALU.mult))
            V(lambda e: e.tensor_copy(out=ki[:], in_=r[:]))
            V(lambda e: e.tensor_copy(out=kf[:], in_=ki[:]))
            V(lambda e: e.scalar_tensor_tensor(out=r[:], in0=kf[:], scalar=-6.28125, in1=a[:], op0=ALU.mult, op1=ALU.add))
            V(lambda e: e.scalar_tensor_tensor(out=r[:], in0=kf[:], scalar=-(2 * PI - 6.28125), in1=r[:], op0=ALU.mult, op1=ALU.add))

            def wrap(t):
                V(lambda e: e.tensor_scalar(out=kf[:], in0=t[:], scalar1=PI, scalar2=-2 * PI, op0=ALU.is_gt, op1=ALU.mult))
                V(lambda e: e.tensor_tensor(out=t[:], in0=t[:], in1=kf[:], op=ALU.add))
                V(lambda e: e.tensor_scalar(out=kf[:], in0=t[:], scalar1=-PI, scalar2=2 * PI, op0=ALU.is_lt, op1=ALU.mult))
                V(lambda e: e.tensor_tensor(out=t[:], in0=t[:], in1=kf[:], op=ALU.add))
                V(lambda e: e.tensor_scalar(out=t[:], in0=t[:], scalar1=3.1415925, scalar2=-3.1415925, op0=ALU.min, op1=ALU.max))
            wrap(r)
            P.op("scalar", lambda e: e.activation(out=STb[:], in_=r[:], func=AF.Sin), reads=[T], writes=[T_tab, T])
            V(lambda e: e.tensor_scalar(out=a[:], in0=r[:], scalar1=float(PI / 2), scalar2=None, op0=ALU.add))
            wrap(a)
            P.op("scalar", lambda e: e.activation(out=CT[:], in_=a[:], func=AF.Sin), reads=[T], writes=[T_tab, T])
            P.barrier()
            P.flush()

    def norm_T(es, l, i, HT, T_HT):
        xsq = sbuf(es, "n_xsq", [128, D], BF16)
        xn = [sbuf(es, "n_xn%d" % k, [128, D], BF16) for k in range(2)]
        T_sq = Trk()
        T_xn = [Trk(), Trk()]
        T_rs = Trk()
        for t in range(NT):
            P.op("scalar", lambda e, t=t: e.activation(out=xsq[:], in_=X[:, t, :], func=AF.Square, accum_out=rs_x[:, t:t + 1]),
                 reads=[T_X[t]], writes=[T_sq, T_rs])
        P.op("scalar", lambda e: e.activation(out=rs_x[:, NT:2 * NT], in_=rs_x[:, 0:NT], func=AF.Sqrt, scale=1.0 / D, bias=EPS),
             reads=[T_rs], writes=[T_rs])
        P.op("vector", lambda e: e.reciprocal(out=rs_x[:, 0:NT], in_=rs_x[:, NT:2 * NT]), reads=[T_rs], writes=[T_rs])
        g0 = (l * 3 + i) * 8
        psT = PSH[:, :].rearrange("p (c t) -> p c t", c=8)
        for t in range(NT):
            k = t % 2
            P.op("vector", lambda e, t=t, k=k: e.tensor_scalar(out=xn[k][:], in0=X[:, t, :], scalar1=rs_x[:, t:t + 1], scalar2=None, op0=ALU.mult),
                 reads=[T_X[t], T_rs], writes=[T_xn[k]])
            P.group("tensor", [(lambda e, c=c, k=k: e.transpose(out=psT[:, c, :], in_=xn[k][:, c * 128:(c + 1) * 128], identity=identb[:])) for c in range(8)],
                    reads=[T_xn[k], T_cst], writes=[T_psh])
            P.op("vector", lambda e, t=t: e.tensor_tensor(out=HT[:, :, t * 128:(t + 1) * 128], in0=psT,
                                                         in1=ngT[:, g0:g0 + 8].unsqueeze(2).to_broadcast([128, 8, 128]), op=ALU.mult),
                 reads=[T_psh, T_cst], writes=[T_HT[t]])

    def ffn(l, i):
        with contextlib.ExitStack() as es:
            HT = sbuf(es, "f_HT", [128, 8, S], BF16)
            T_HT = [Trk() for _ in range(NT)]
            norm_T(es, l, i, HT, T_HT)
            WG = [sbuf(es, "f_wg%d" % k, [128, 8, 512], BF16) for k in range(2)]
            WU = [sbuf(es, "f_wu%d" % k, [128, 8, 512], BF16) for k in range(2)]
            WD = [sbuf(es, "f_wd%d" % k, [128, 4, D], BF16) for k in range(2)]
            AT = [sbuf(es, "f_at%d" % k, [128, 4, 512], BF16) for k in range(2)]
            SG = [sbuf(es, "f_sg%d" % k, [128, 512], F32) for k in range(2)]
            T_W = [Trk(), Trk()]
            T_AT = [Trk(), Trk()]
            T_SG = [Trk(), Trk()]
            wgv = wg_d[l, i].rearrange("(c p) f -> p c f", p=128)
            wuv = wu_d[l, i].rearrange("(c p) f -> p c f", p=128)
            groups = [(f0, min(512, DFF - f0)) for f0 in range(0, DFF, 512)]
            it = 0
            for gi, (f0, fw) in enumerate(groups):
                wb = gi % 2
                nfb = fw // 128
                P.dma("gpsimd", WG[wb][:, :, 0:fw], wgv[:, :, f0:f0 + fw], writes=[T_W[wb]])
                P.dma("gpsimd", WU[wb][:, :, 0:fw], wuv[:, :, f0:f0 + fw], writes=[T_W[wb]])
                P.dma("gpsimd", WD[wb][:, 0:nfb, :], wd_d[l, i][f0:f0 + fw, :].rearrange("(c p) d -> p c d", p=128), writes=[T_W[wb]])
                for tg in range(4):
                    ab = tg % 2
                    for fb in range(nfb):
                        k = it % 2
                        it += 1
                        pg, pu = PSB[2 * k], PSB[2 * k + 1]
                        P.group("tensor", [(lambda e, c=c, fb=fb, wb=wb, tg=tg, pg=pg: e.matmul(pg[:, :], lhsT=WG[wb][:, c, fb * 128:(fb + 1) * 128], rhs=HT[:, c, tg * 512:(tg + 1) * 512], start=(c == 0), stop=(c == 7))) for c in range(8)],
                                reads=[T_W[wb]] + T_HT[tg * 4:tg * 4 + 4], writes=[T_ps[2 * k]])
                        P.group("tensor", [(lambda e, c=c, fb=fb, wb=wb, tg=tg, pu=pu: e.matmul(pu[:, :], lhsT=WU[wb][:, c, fb * 128:(fb + 1) * 128], rhs=HT[:, c, tg * 512:(tg + 1) * 512], start=(c == 0), stop=(c == 7))) for c in range(8)],
                                reads=[T_W[wb]] + T_HT[tg * 4:tg * 4 + 4], writes=[T_ps[2 * k + 1]])
                        P.op("scalar", lambda e, k=k, pg=pg: e.activation(out=SG[k][:], in_=pg[:, :], func=AF.Silu), reads=[T_ps[2 * k]], writes=[T_SG[k]])
                        P.op("vector", lambda e, k=k, pu=pu, ab=ab, fb=fb: e.tensor_tensor(out=AT[ab][:, fb, :], in0=pu[:, :], in1=SG[k][:], op=ALU.mult),
                             reads=[T_ps[2 * k + 1], T_SG[k]], writes=[T_AT[ab]])
                    for tt in range(4):
                        t = tg * 4 + tt
                        for hf in range(2):
                            pb = 4 + (t * 2 + hf) % 2
                            pd = PSB[pb]
                            P.group("tensor", [(lambda e, fb=fb, ab=ab, tt=tt, wb=wb, hf=hf, pd=pd: e.matmul(pd[:, :], lhsT=AT[ab][:, fb, tt * 128:(tt + 1) * 128], rhs=WD[wb][:, fb, hf * 512:(hf + 1) * 512], start=(fb == 0), stop=(fb == nfb - 1))) for fb in range(nfb)],
                                    reads=[T_AT[ab], T_W[wb]], writes=[T_ps[pb]])
                            P.op("vector", lambda e, t=t, hf=hf, pd=pd: e.scalar_tensor_tensor(out=X[:, t, hf * 512:(hf + 1) * 512], in0=pd[:, :], scalar=0.5, in1=X[:, t, hf * 512:(hf + 1) * 512], op0=ALU.mult, op1=ALU.add),
                                 reads=[T_ps[pb], T_X[t]], writes=[T_X[t]])
            P.barrier()
            P.flush()

    def proj_fm(es_tmp, HT, T_HT, W, T_W, col0, tg, dst, T_dst, mode, gcol):
        k = proj_fm.it % 2
        proj_fm.it += 1
        tm = proj_fm.tmp
        pq = PSB[k]
        tsl = slice(tg * 512, (tg + 1) * 512)
        P.group("tensor", [(lambda e, c=c: e.matmul(pq[:, :], lhsT=W[:, c, col0:col0 + 128], rhs=HT[:, c, tsl], start=(c == 0), stop=(c == 7))) for c in range(8)],
                reads=[T_W] + T_HT[tg * 4:tg * 4 + 4], writes=[T_ps[k]])
        xn = tm["xn"][k]
        T_xn = tm["T_xn"][k]
        if mode == "norm":
            xsq = tm["xsq"][k]
            T_xsq = tm["T_xsq"][k]
            sd = tm["sd"][k]
            T_sd = tm["T_sd"][k]
            pss = PSB[2 + k]
            P.op("scalar", lambda e: e.activation(out=xsq[:], in_=pq[:, :], func=AF.Square), reads=[T_ps[k]], writes=[T_xsq])
            P.group("tensor", [lambda e: e.matmul(pss[:, :], lhsT=BDb[:], rhs=xsq[:], start=True, stop=True)], reads=[T_xsq, T_cst], writes=[T_ps[2 + k]])
            P.op("scalar", lambda e: e.activation(out=sd[:], in_=pss[:, :], func=AF.Sqrt, scale=1.0 / 64, bias=EPS), reads=[T_ps[2 + k]], writes=[T_sd])
            P.op("vector", lambda e: e.reciprocal(out=sd[:], in_=sd[:]), reads=[T_sd], writes=[T_sd])
            P.op("vector", lambda e: e.scalar_tensor_tensor(out=xn[:], in0=pq[:, :], scalar=gcol, in1=sd[:], op0=ALU.mult, op1=ALU.mult),
                 reads=[T_ps[k], T_sd, T_cst], writes=[T_xn])
        else:
            P.op("scalar", lambda e: e.copy(out=xn[:], in_=pq[:, :]), reads=[T_ps[k]], writes=[T_xn])
        pr = PSB[4 + k]
        t1 = tm["t1"][k]
        T_t1 = tm["T_t1"][k]
        t2 = tm["t2"][k]
        T_t2 = tm["T_t2"][k]
        P.group("tensor", [lambda e: e.matmul(pr[:, :], lhsT=RMb[:], rhs=xn[:], start=True, stop=True)], reads=[T_xn, T_cst], writes=[T_ps[4 + k]])
        P.op("gpsimd", lambda e: e.tensor_tensor(out=t1[:], in0=xn[:], in1=CT[:, tsl], op=ALU.mult), reads=[T_xn, T_tab], writes=[T_t1])
        P.op("vector", lambda e: e.tensor_tensor(out=t2[:], in0=pr[:, :], in1=STb[:, tsl], op=ALU.mult), reads=[T_ps[4 + k], T_tab], writes=[T_t2])
        P.op("vector", lambda e: e.tensor_tensor(out=dst, in0=t1[:], in1=t2[:], op=ALU.add), reads=[T_t1, T_t2], writes=[T_dst])
    proj_fm.it = 0

    def proj_tmp(es):
        tm = {}
        for nm, dtp in (("xn", BF16), ("xsq", BF16), ("sd", F32)):
            tm[nm] = [sbuf(es, "pj_%s%d" % (nm, k), [128, 512], dtp) for k in range(2)]
            tm["T_" + nm] = [Trk(), Trk()]
        for nm, dtp in (("t1", F32), ("t2", F32)):
            buf = sbuf(es, "pj_%s" % nm, [128, 512], dtp)
            tk = Trk()
            tm[nm] = [buf, buf]
            tm["T_" + nm] = [tk, tk]
        proj_fm.tmp = tm

    def load_w_in(Wt, T_W, l, segs):
        wv = win_d[l].rearrange("(c p) f -> p c f", p=128)
        for (d0, s0, n) in segs:
            P.dma("gpsimd", Wt[:, :, d0:d0 + n], wv[:, :, s0:s0 + n], writes=[T_W])

    def proj_tm(HT, T_HT, Wv, T_Wv, ncol, t, pbank):
        pv = PSB[pbank]
        P.group("tensor", [(lambda e, c=c: e.matmul(pv[:, 0:ncol], lhsT=HT[:, c, t * 128:(t + 1) * 128], rhs=Wv[:, c, 0:ncol], start=(c == 0), stop=(c == 7))) for c in range(8)],
                reads=[T_Wv, T_HT[t]], writes=[T_ps[pbank]])
        return pv

    def transpose_out(o_tile, T_o, ncol, OT, T_OT, chunk0, t):
        n = ncol // 128
        psT = PSH[:, 0:n * 128].rearrange("p (c t) -> p c t", c=n)
        P.group("tensor", [(lambda e, j=j: e.transpose(out=psT[:, j, :], in_=o_tile[:, j * 128:(j + 1) * 128], identity=identb[:])) for j in range(n)],
                reads=[T_o, T_cst], writes=[T_psh])
        P.op("scalar", lambda e: e.copy(out=OT[:, chunk0:chunk0 + n, t * 128:(t + 1) * 128], in_=psT), reads=[T_psh], writes=[T_OT[t]])

    def mixer_A(l, HT, T_HT, OT, T_OT):
        with contextlib.ExitStack() as es:
            QA = sbuf(es, "a_qa", [128, 2, S], BF16)
            KA = sbuf(es, "a_ka", [128, S], BF16)
            QI = sbuf(es, "a_qi", [128, 4, S], BF16)
            KI = sbuf(es, "a_ki", [128, S], BF16)
            VA = sbuf(es, "a_va", [128, NT, 65], BF16)
            WI = sbuf(es, "a_wi", [128, NT, 8], F32)
            T_Q = [Trk() for _ in range(4)]
            T_V = [Trk() for _ in range(NT)]
            with contextlib.ExitStack() as es1:
                W = sbuf(es1, "a_w", [128, 8, 1024], BF16)
                Wv = sbuf(es1, "a_wv", [128, 8, 72], BF16)
                T_W = Trk()
                T_Wv = Trk()
                proj_tmp(es1)
                load_w_in(W, T_W, l, [(0, OFF["qa"], 256), (256, OFF["ka"], 64), (320, OFF["ka"], 64),
                                      (384, OFF["qi"], 512), (896, OFF["ki"], 64), (960, OFF["ki"], 64)])
                load_w_in(Wv, T_Wv, l, [(0, OFF["va"], 64), (64, OFF["wi"], 8)])
                P.op("vector", lambda e: e.memset(VA[:, :, 64:65], 1.0), writes=T_V)
                gq = qkgT[:, l * 6 + 0:l * 6 + 1]
                gk = qkgT[:, l * 6 + 1:l * 6 + 2]
                for tg in range(4):
                    tsl = slice(tg * 512, (tg + 1) * 512)
                    for p in range(2):
                        proj_fm(es1, HT, T_HT, W, T_W, p * 128, tg, QA[:, p, tsl], T_Q[tg], "norm", gq)
                    proj_fm(es1, HT, T_HT, W, T_W, 256, tg, KA[:, tsl], T_Q[tg], "norm", gk)
                    for p in range(4):
                        proj_fm(es1, HT, T_HT, W, T_W, 384 + p * 128, tg, QI[:, p, tsl], T_Q[tg], "rope", None)
                    proj_fm(es1, HT, T_HT, W, T_W, 896, tg, KI[:, tsl], T_Q[tg], "rope", None)
                for t in range(NT):
                    pv = proj_tm(HT, T_HT, Wv, T_Wv, 72, t, 6)
                    P.op("scalar", lambda e, t=t, pv=pv: e.copy(out=VA[:, t, 0:64], in_=pv[:, 0:64]), reads=[T_ps[6]], writes=[T_V[t]])
                    P.op("vector", lambda e, t=t, pv=pv: e.tensor_copy(out=WI[:, t, :], in_=pv[:, 64:72]), reads=[T_ps[6]], writes=[T_V[t]])
                P.barrier()
                P.flush()
            if SUB == 1:
                return
            with contextlib.ExitStack() as es2:
                ISC = sbuf(es2, "a_isc", [128, S], F32)
                M = sbuf(es2, "a_m", [128, S], BF16)
                MT = sbuf(es2, "a_mt", [128, NT, 128], BF16)
                PT = sbuf(es2, "a_pt", [128, NT, 2, 128], BF16)
                RL = [sbuf(es2, "a_rl%d" % k, [128, 512], F32) for k in range(2)]
                bis = sbuf(es2, "a_bis", [128, 32], F32)
                stp = sbuf(es2, "a_stp", [128, NBIS], F32)
                oa = sbuf(es2, "a_oa", [128, 256], BF16)
                rc = sbuf(es2, "a_rc", [128, 4], F32)
                T_isc, T_M, T_MT, T_PT, T_bis, T_oa = Trk(), Trk(), Trk(), Trk(), Trk(), Trk()
                T_PTk = [Trk() for _ in range(NT)]
                T_RL = [Trk(), Trk()]
                it = 0
                for qt in range(NT):
                    L = 128 * (qt + 1)
                    qsl = slice(qt * 128, (qt + 1) * 128)
                    tgq = qt // 4
                    nch = (L + 511) // 512
                    for ch in range(nch):
                        c0 = ch * 512
                        cw = min(512, L - c0)
                        for h in range(8):
                            k = it % 2
                            it += 1
                            hp = (h % 2) * 64
                            pl = PSB[k]
                            P.group("tensor", [lambda e, h=h, hp=hp, c0=c0, cw=cw, pl=pl: e.matmul(pl[:, 0:cw], lhsT=QI[hp:hp + 64, h // 2, qsl], rhs=KI[hp:hp + 64, c0:c0 + cw], start=True, stop=True)],
                                    reads=[T_Q[tgq]] + T_Q[0:tgq + 1], writes=[T_ps[k]])
                            P.op("scalar", lambda e, k=k, cw=cw, pl=pl: e.activation(out=RL[k][:, 0:cw], in_=pl[:, 0:cw], func=AF.Relu), reads=[T_ps[k]], writes=[T_RL[k]])
                            if h == 0:
                                P.op("vector", lambda e, k=k, c0=c0, cw=cw, h=h: e.tensor_scalar(out=ISC[:, c0:c0 + cw], in0=RL[k][:, 0:cw], scalar1=WI[:, qt, h:h + 1], scalar2=None, op0=ALU.mult),
                                     reads=[T_RL[k], T_V[qt]], writes=[T_isc])
                            else:
                                P.op("vector", lambda e, k=k, c0=c0, cw=cw, h=h: e.scalar_tensor_tensor(out=ISC[:, c0:c0 + cw], in0=RL[k][:, 0:cw], scalar=WI[:, qt, h:h + 1], in1=ISC[:, c0:c0 + cw], op0=ALU.mult, op1=ALU.add),
                                     reads=[T_RL[k], T_V[qt], T_isc], writes=[T_isc])
                    Vb = lambda fn: P.op("vector", fn, reads=[T_isc, T_bis, T_cst], writes=[T_bis])
                    if qt >= 2:
                        Vb(lambda e, L=L: e.tensor_reduce(out=bis[:, 0:1], in_=ISC[:, 0:L], axis=AX.X, op=ALU.min))
                        Vb(lambda e, L=L: e.tensor_reduce(out=bis[:, 1:2], in_=ISC[:, 0:L], axis=AX.X, op=ALU.max))
                    P.op("gpsimd", lambda e, L=L: e.affine_select(out=ISC[:, L - 128:L], in_=ISC[:, L - 128:L], pattern=[[-1, 128]], compare_op=ALU.is_ge, fill=NEG, base=0, channel_multiplier=1),
                         reads=[T_isc, T_bis], writes=[T_isc])
                    if qt >= 2:
                        Vb(lambda e: e.tensor_tensor(out=bis[:, 2:3], in0=bis[:, 1:2], in1=bis[:, 0:1], op=ALU.subtract))
                        Vb(lambda e: e.tensor_scalar(out=bis[:, 2:3], in0=bis[:, 2:3], scalar1=1.0001, scalar2=1e-20, op0=ALU.mult, op1=ALU.add))
                        Vb(lambda e: e.tensor_scalar(out=stp[:, :], in0=cstf[:, C_POW:C_POW + NBIS], scalar1=bis[:, 2:3], scalar2=None, op0=ALU.mult))
                        for i in range(NBIS):
                            Vb(lambda e, i=i: e.tensor_tensor(out=bis[:, 3:4], in0=bis[:, 0:1], in1=stp[:, i:i + 1], op=ALU.add))
                            P.op("vector", lambda e, L=L: e.tensor_scalar(out=M[:, 0:L], in0=ISC[:, 0:L], scalar1=bis[:, 3:4], scalar2=0.0, op0=ALU.is_ge, op1=ALU.add, accum_out=bis[:, 4:5]),
                                 reads=[T_isc, T_bis], writes=[T_M, T_bis])
                            Vb(lambda e, i=i: e.tensor_scalar(out=bis[:, 5:6], in0=bis[:, 4:5], scalar1=255.5, scalar2=stp[:, i:i + 1], op0=ALU.is_ge, op1=ALU.mult))
                            Vb(lambda e: e.tensor_tensor(out=bis[:, 0:1], in0=bis[:, 0:1], in1=bis[:, 5:6], op=ALU.add))
                        thr = bis[:, 0:1]
                        P.op("vector", lambda e, L=L, thr=thr: e.tensor_scalar(out=M[:, 0:L], in0=ISC[:, 0:L], scalar1=thr, scalar2=None, op0=ALU.is_ge),
                             reads=[T_isc, T_bis], writes=[T_M])
                    else:
                        P.op("vector", lambda e, L=L: e.tensor_scalar(out=M[:, 0:L], in0=ISC[:, 0:L], scalar1=-1.0e29, scalar2=None, op0=ALU.is_ge),
                             reads=[T_isc, T_bis], writes=[T_M])
                    if SUB == 2:
                        continue
                    for k0 in range(0, qt + 1, 8):
                        n = min(8, qt + 1 - k0)
                        psT = PSH[:, 0:n * 128].rearrange("p (c t) -> p c t", c=n)
                        P.group("tensor", [(lambda e, j=j, k0=k0: e.transpose(out=psT[:, j, :], in_=M[:, (k0 + j) * 128:(k0 + j + 1) * 128], identity=identb[:])) for j in range(n)],
                                reads=[T_M, T_cst], writes=[T_psh])
                        P.op("scalar", lambda e, k0=k0, n=n, psT=psT: e.copy(out=MT[:, k0:k0 + n, :], in_=psT), reads=[T_psh], writes=[T_MT])
                    if SUB == 3:
                        continue
                    for pr in range(2):
                        for kt in range(qt + 1):
                            k = it % 2
                            it += 1
                            ksl = slice(kt * 128, (kt + 1) * 128)
                            for hh in range(2):
                                pb = 2 + 2 * hh + k
                                ps_s = PSB[pb]
                                P.group("tensor", [lambda e, hh=hh, pr=pr, ksl=ksl, ps_s=ps_s: e.matmul(ps_s[:, 0:128], lhsT=KA[hh * 64:hh * 64 + 64, ksl], rhs=QA[hh * 64:hh * 64 + 64, pr, qsl], start=True, stop=True)],
                                        reads=T_Q[0:tgq + 1], writes=[T_ps[pb]])
                                P.op("scalar", lambda e, kt=kt, hh=hh, ps_s=ps_s: e.activation(out=PT[:, kt, hh, :], in_=ps_s[:, 0:128], func=AF.Exp, scale=0.125), reads=[T_ps[pb]], writes=[T_PTk[kt]])
                            P.op("vector", lambda e, kt=kt: e.tensor_tensor(out=PT[:, kt, :, :], in0=PT[:, kt, :, :], in1=MT[:, kt:kt + 1, :].to_broadcast([128, 2, 128]), op=ALU.mult),
                                 reads=[T_PTk[kt], T_MT], writes=[T_PTk[kt]])
                        if SUB == 4:
                            continue
                        po = PSB[6]
                        pov = po[:, pr * 130:(pr + 1) * 130].rearrange("p (h e) -> p h e", h=2)
                        for hh in range(2):
                            P.group("tensor", [(lambda e, kt=kt, hh=hh, pov=pov: e.matmul(pov[:, hh, :], lhsT=PT[:, kt, hh, :], rhs=VA[:, kt, :], start=(kt == 0), stop=(kt == qt))) for kt in range(qt + 1)],
                                    reads=T_PTk[0:qt + 1] + T_V[0:qt + 1], writes=[T_ps[6]])
                        if SUB == 5:
                            continue
                        P.op("vector", lambda e, pr=pr, pov=pov: e.reciprocal(out=rc[:, 2 * pr:2 * pr + 2], in_=pov[:, :, 64]), reads=[T_ps[6]], writes=[T_bis])
                        P.op("vector", lambda e, pr=pr, pov=pov: e.tensor_tensor(out=oa[:, pr * 128:(pr + 1) * 128].rearrange("p (h e) -> p h e", h=2), in0=pov[:, :, 0:64],
                                                                               in1=rc[:, 2 * pr:2 * pr + 2].unsqueeze(2).to_broadcast([128, 2, 64]), op=ALU.mult),
                             reads=[T_ps[6], T_bis], writes=[T_oa])
                    if SUB in (4, 5, 6):
                        continue
                    transpose_out(oa, T_oa, 256, OT, T_OT, 0, qt)
                P.barrier()
                P.flush()

    def mixer_B(l, HT, T_HT, OT, T_OT):
        with contextlib.ExitStack() as es:
            QB = sbuf(es, "b_q", [128, 2, S], BF16)
            KB = sbuf(es, "b_k", [128, 2, S], BF16)
            VB = sbuf(es, "b_v", [128, NT, 4, 65], BF16)
            KM = sbuf(es, "b_km", [128, 2, 8], BF16)
            T_Q = [Trk() for _ in range(4)]
            T_V = [Trk() for _ in range(NT)]
            T_KM = Trk()
            with contextlib.ExitStack() as es1:
                W = sbuf(es1, "b_w", [128, 8, 512], BF16)
                Wv = sbuf(es1, "b_wv", [128, 8, 256], BF16)
                kmf = sbuf(es1, "b_kmf", [128, 2, 8], F32)
                T_W, T_Wv = Trk(), Trk()
                proj_tmp(es1)
                load_w_in(W, T_W, l, [(0, OFF["qb"], 256), (256, OFF["kb"], 256)])
                load_w_in(Wv, T_Wv, l, [(0, OFF["vb"], 256)])
                P.op("vector", lambda e: e.memset(VB[:, :, :, 64:65], 1.0), writes=T_V)
                gq = qkgT[:, l * 6 + 2:l * 6 + 3]
                gk = qkgT[:, l * 6 + 3:l * 6 + 4]
                for tg in range(4):
                    tsl = slice(tg * 512, (tg + 1) * 512)
                    for p in range(2):
                        proj_fm(es1, HT, T_HT, W, T_W, p * 128, tg, QB[:, p, tsl], T_Q[tg], "norm", gq)
                        proj_fm(es1, HT, T_HT, W, T_W, 256 + p * 128, tg, KB[:, p, tsl], T_Q[tg], "norm", gk)
                for t in range(NT):
                    pv = proj_tm(HT, T_HT, Wv, T_Wv, 256, t, 6)
                    P.op("scalar", lambda e, t=t, pv=pv: e.copy(out=VB[:, t, :, 0:64], in_=pv[:, 0:256].rearrange("p (h e) -> p h e", h=4)), reads=[T_ps[6]], writes=[T_V[t]])
                for p in range(2):
                    P.op("vector", lambda e, p=p: e.tensor_reduce(out=kmf[:, p, :], in_=KB[:, p, :].rearrange("p (n k) -> p n k", n=8), axis=AX.X, op=ALU.add), reads=T_Q, writes=[T_KM])
                P.op("vector", lambda e: e.tensor_scalar(out=KM[:, :, :], in0=kmf[:, :, :], scalar1=1.0 / 256, scalar2=None, op0=ALU.mult), reads=[T_KM], writes=[T_KM])
                P.barrier()
                P.flush()
            with contextlib.ExitStack() as es2:
                PT = [sbuf(es2, "b_pt%d" % k, [128, 2, 256], BF16) for k in range(2)]
                T_PT = [Trk(), Trk()]
                gate = sbuf(es2, "b_gate", [128, 2, 4, 8], F32)
                top8 = sbuf(es2, "b_top8", [128, 8], F32)
                BM = sbuf(es2, "b_bm", [128, 2, 4, 8], F32)
                acc = sbuf(es2, "b_acc", [128, 2, 4, 65], F32)
                ob = sbuf(es2, "b_ob", [128, 2, 256], BF16)
                rc = sbuf(es2, "b_rc", [128, 2, 4], F32)
                T_g, T_acc, T_ob = Trk(), Trk(), Trk()
                it = 0
                for j in range(8):
                    tgq = j // 2
                    if j > 0:
                        for par in range(2):
                            pg = PSB[6 - par]
                            pgv = pg[:, 0:64].rearrange("p (a h n) -> p a h n", a=2, h=4)
                            P.group("tensor", [(lambda e, a=a, h=h, pgv=pgv: e.matmul(pgv[:, a, h, :], lhsT=QB[(h % 2) * 64:(h % 2) * 64 + 64, h // 2, (2 * j + a) * 128:(2 * j + a + 1) * 128],
                                                                                       rhs=KM[(h % 2) * 64:(h % 2) * 64 + 64, h // 2, :], start=True, stop=True)) for a in range(2) for h in (par, par + 2)],
                                    reads=[T_Q[tgq], T_KM], writes=[T_ps[6 - par]])
                            for h in (par, par + 2):
                                P.op("vector", lambda e, pgv=pgv, h=h: e.tensor_copy(out=gate[:, :, h, :], in_=pgv[:, :, h, :]), reads=[T_ps[6 - par]], writes=[T_g])
                        P.op("vector", lambda e, j=j: e.memset(gate[:, :, :, j:8], NEG), reads=[T_g], writes=[T_g])
                        for a in range(2):
                            for h in range(4):
                                P.op("vector", lambda e, a=a, h=h: e.max(out=top8[:, :], in_=gate[:, a, h, :]), reads=[T_g], writes=[T_g])
                                P.op("vector", lambda e, a=a, h=h: e.tensor_scalar(out=BM[:, a, h, :], in0=gate[:, a, h, :], scalar1=top8[:, 2:3], scalar2=None, op0=ALU.is_ge), reads=[T_g], writes=[T_g])
                    for h in range(4):
                        hp = (h % 2) * 64
                        pp = h // 2
                        k = it % 2
                        it += 1
                        ps_s = PSB[k]
                        psv = ps_s[:, 0:512].rearrange("p (c q) -> p c q", c=2)
                        ks0 = slice((2 * j) * 128, (2 * j + 1) * 128)
                        ks1 = slice((2 * j + 1) * 128, (2 * j + 2) * 128)
                        qs_all = slice((2 * j) * 128, (2 * j + 2) * 128)
                        P.group("tensor", [lambda e, psv=psv, hp=hp, pp=pp, ks0=ks0, qs_all=qs_all: e.matmul(psv[:, 0, :], lhsT=KB[hp:hp + 64, pp, ks0], rhs=QB[hp:hp + 64, pp, qs_all], start=True, stop=True),
                                           lambda e, psv=psv, hp=hp, pp=pp, ks1=ks1, qs_all=qs_all: e.matmul(psv[:, 1, :], lhsT=KB[hp:hp + 64, pp, ks1], rhs=QB[hp:hp + 64, pp, qs_all], start=True, stop=True)],
                                reads=[T_Q[tgq]], writes=[T_ps[k]])
                        P.op("scalar", lambda e, k=k, psv=psv: e.activation(out=PT[k][:, :, :], in_=psv, func=AF.Exp, scale=0.125), reads=[T_ps[k]], writes=[T_PT[k]])
                        P.op("vector", lambda e, k=k: e.tensor_tensor(out=PT[k][:, 0, 0:128], in0=PT[k][:, 0, 0:128], in1=trib[:], op=ALU.mult), reads=[T_PT[k], T_cst], writes=[T_PT[k]])
                        P.op("vector", lambda e, k=k: e.tensor_tensor(out=PT[k][:, 1, 128:256], in0=PT[k][:, 1, 128:256], in1=trib[:], op=ALU.mult), reads=[T_PT[k], T_cst], writes=[T_PT[k]])
                        po = PSB[2 + k]
                        pov = po[:, 0:130].rearrange("p (a e) -> p a e", a=2)
                        P.group("tensor", [lambda e, k=k, pov=pov, h=h: e.matmul(pov[:, 0, :], lhsT=PT[k][:, 0, 0:128], rhs=VB[:, 2 * j, h, :], start=True, stop=True)],
                                reads=[T_PT[k], T_V[2 * j]], writes=[T_ps[2 + k]])
                        P.group("tensor", [lambda e, k=k, pov=pov, h=h: e.matmul(pov[:, 1, :], lhsT=PT[k][:, 0, 128:256], rhs=VB[:, 2 * j, h, :], start=True, stop=False),
                                           lambda e, k=k, pov=pov, h=h: e.matmul(pov[:, 1, :], lhsT=PT[k][:, 1, 128:256], rhs=VB[:, 2 * j + 1, h, :], start=False, stop=True)],
                                reads=[T_PT[k], T_V[2 * j], T_V[2 * j + 1]], writes=[T_ps[2 + k]])
                        P.op("vector", lambda e, pov=pov, h=h: e.tensor_copy(out=acc[:, :, h, :], in_=pov), reads=[T_ps[2 + k]], writes=[T_acc])
                        for n in range(j):
                            k = it % 2
                            it += 1
                            ps_s = PSB[k]
                            psv = ps_s[:, 0:512].rearrange("p (c q) -> p c q", c=2)
                            P.group("tensor", [(lambda e, c=c, psv=psv, hp=hp, pp=pp, n=n, qs_all=qs_all: e.matmul(psv[:, c, :], lhsT=KB[hp:hp + 64, pp, (2 * n + c) * 128:(2 * n + c + 1) * 128], rhs=QB[hp:hp + 64, pp, qs_all], start=True, stop=True)) for c in range(2)],
                                    reads=[T_Q[tgq], T_Q[n // 2]], writes=[T_ps[k]])
                            P.op("scalar", lambda e, k=k, psv=psv: e.activation(out=PT[k][:, :, :], in_=psv, func=AF.Exp, scale=0.125), reads=[T_ps[k]], writes=[T_PT[k]])
                            po = PSB[2 + k]
                            pov = po[:, 0:130].rearrange("p (a e) -> p a e", a=2)
                            for a in range(2):
                                P.group("tensor", [(lambda e, c=c, a=a, k=k, pov=pov, n=n, h=h: e.matmul(pov[:, a, :], lhsT=PT[k][:, c, a * 128:(a + 1) * 128], rhs=VB[:, 2 * n + c, h, :], start=(c == 0), stop=(c == 1))) for c in range(2)],
                                        reads=[T_PT[k], T_V[2 * n], T_V[2 * n + 1]], writes=[T_ps[2 + k]])
                            for a in range(2):
                                P.op("vector", lambda e, a=a, pov=pov, h=h, n=n: e.scalar_tensor_tensor(out=acc[:, a, h, :], in0=pov[:, a, :], scalar=BM[:, a, h, n:n + 1], in1=acc[:, a, h, :], op0=ALU.mult, op1=ALU.add),
                                     reads=[T_ps[2 + k], T_g, T_acc], writes=[T_acc])
                    P.op("vector", lambda e: e.reciprocal(out=rc[:, :, :], in_=acc[:, :, :, 64]), reads=[T_acc], writes=[T_g])
                    for a in range(2):
                        P.op("vector", lambda e, a=a: e.tensor_tensor(out=ob[:, a, :].rearrange("p (h e) -> p h e", h=4), in0=acc[:, a, :, 0:64], in1=rc[:, a, :].unsqueeze(2).to_broadcast([128, 4, 64]), op=ALU.mult),
                             reads=[T_acc, T_g], writes=[T_ob])
                        transpose_out(ob[:, a, :], T_ob, 256, OT, T_OT, 2, 2 * j + a)
                P.barrier()
                P.flush()

    def mixer_C(l, HT, T_HT, OT, T_OT, half):
        with contextlib.ExitStack() as es:
            QC = sbuf(es, "c_q", [128, 2, S], BF16)
            KC = sbuf(es, "c_k", [128, 2, S], BF16)
            VC = sbuf(es, "c_v", [128, NT, 2, 129], BF16)
            T_Q = [Trk() for _ in range(4)]
            T_V = [Trk() for _ in range(NT)]
            with contextlib.ExitStack() as es1:
                W = sbuf(es1, "c_w", [128, 8, 512], BF16)
                Wv = sbuf(es1, "c_wv", [128, 8, 256], BF16)
                T_W, T_Wv = Trk(), Trk()
                proj_tmp(es1)
                load_w_in(W, T_W, l, [(0, OFF["qc"] + half * 256, 256), (256, OFF["kc"] + half * 256, 256)])
                load_w_in(Wv, T_Wv, l, [(0, OFF["vc"] + half * 256, 256)])
                P.op("vector", lambda e: e.memset(VC[:, :, :, 128:129], 1.0), writes=T_V)
                gq = qkgT[:, l * 6 + 4:l * 6 + 5]
                gk = qkgT[:, l * 6 + 5:l * 6 + 6]
                for tg in range(4):
                    tsl = slice(tg * 512, (tg + 1) * 512)
                    for p in range(2):
                        proj_fm(es1, HT, T_HT, W, T_W, p * 128, tg, QC[:, p, tsl], T_Q[tg], "norm", gq)
                        proj_fm(es1, HT, T_HT, W, T_W, 256 + p * 128, tg, KC[:, p, tsl], T_Q[tg], "norm", gk)
                for t in range(NT):
                    pv = proj_tm(HT, T_HT, Wv, T_Wv, 256, t, 6)
                    P.op("scalar", lambda e, t=t, pv=pv: e.copy(out=VC[:, t, :, 0:128], in_=pv[:, 0:256].rearrange("p (h e) -> p h e", h=2)), reads=[T_ps[6]], writes=[T_V[t]])
                P.barrier()
                P.flush()
            with contextlib.ExitStack() as es2:
                PT = sbuf(es2, "c_pt", [128, NT, 512], BF16)
                T_PT = Trk()
                t0 = sbuf(es2, "c_t0", [128, 4, 128], F32)
                o32 = sbuf(es2, "c_o32", [128, 4, 128], F32)
                oc = sbuf(es2, "c_oc", [128, 4, 256], BF16)
                sq = sbuf(es2, "c_sq", [128, 128], F32)
                st = sbuf(es2, "c_st", [128, 16], F32)
                T_t0, T_o32, T_oc, T_st = Trk(), Trk(), Trk(), Trk()
                it = 0
                for G in range(4):
                    for hh in range(2):
                        for c in range(2):
                            cp = c * 64
                            nkt = 4 * G + 4
                            for kt in range(nkt):
                                k = it % 2
                                it += 1
                                ps_s = PSB[k]
                                qs0 = max(kt - 4 * G, 0)
                                q0 = (4 * G + qs0) * 128
                                nq = (4 - qs0) * 128
                                P.group("tensor", [lambda e, ps_s=ps_s, cp=cp, hh=hh, kt=kt, q0=q0, nq=nq: e.matmul(ps_s[:, 0:nq], lhsT=KC[cp:cp + 64, hh, kt * 128:(kt + 1) * 128], rhs=QC[cp:cp + 64, hh, q0:q0 + nq], start=True, stop=True)],
                                        reads=[T_Q[G], T_Q[kt // 4]], writes=[T_ps[k]])
                                P.op("scalar", lambda e, ps_s=ps_s, kt=kt, qs0=qs0, nq=nq: e.activation(out=PT[:, kt, qs0 * 128:qs0 * 128 + nq], in_=ps_s[:, 0:nq], func=AF.Exp, scale=0.125), reads=[T_ps[k]], writes=[T_PT])
                                if kt >= 4 * G:
                                    P.op("vector", lambda e, kt=kt, qs0=qs0: e.tensor_tensor(out=PT[:, kt, qs0 * 128:(qs0 + 1) * 128], in0=PT[:, kt, qs0 * 128:(qs0 + 1) * 128], in1=trib[:], op=ALU.mult),
                                         reads=[T_PT, T_cst], writes=[T_PT])
                            for qs in range(4):
                                pb = 2 + (it % 2)
                                it += 1
                                po = PSB[pb]
                                nk = 4 * G + qs + 1
                                P.group("tensor", [(lambda e, kt=kt, qs=qs, po=po, hh=hh, nk=nk: e.matmul(po[:, 0:129], lhsT=PT[:, kt, qs * 128:(qs + 1) * 128], rhs=VC[:, kt, hh, :], start=(kt == 0), stop=(kt == nk - 1))) for kt in range(nk)],
                                        reads=[T_PT] + T_V[0:nk], writes=[T_ps[pb]])
                                P.op("vector", lambda e, po=po, qs=qs, c=c: e.reciprocal(out=st[:, qs * 2 + c:qs * 2 + c + 1], in_=po[:, 128:129]), reads=[T_ps[pb]], writes=[T_st])
                                if c == 0:
                                    P.op("vector", lambda e, po=po, qs=qs: e.tensor_scalar(out=t0[:, qs, :], in0=po[:, 0:128], scalar1=st[:, qs * 2:qs * 2 + 1], scalar2=None, op0=ALU.mult),
                                         reads=[T_ps[pb], T_st], writes=[T_t0])
                                else:
                                    P.op("vector", lambda e, qs=qs: e.tensor_tensor(out=st[:, 8 + qs:9 + qs], in0=st[:, qs * 2 + 1:qs * 2 + 2], in1=lamv[:, l:l + 1], op=ALU.mult), reads=[T_st, T_lam], writes=[T_st])
                                    P.op("vector", lambda e, po=po, qs=qs: e.scalar_tensor_tensor(out=o32[:, qs, :], in0=po[:, 0:128], scalar=st[:, 8 + qs:9 + qs], in1=t0[:, qs, :], op0=ALU.mult, op1=ALU.add),
                                         reads=[T_ps[pb], T_st, T_t0], writes=[T_o32])
                                    P.op("vector", lambda e, qs=qs: e.tensor_tensor(out=sq[:], in0=o32[:, qs, :], in1=o32[:, qs, :], op=ALU.mult), reads=[T_o32], writes=[T_st])
                                    P.op("vector", lambda e, qs=qs: e.reduce_sum(out=st[:, 12:13], in_=sq[:], axis=AX.X), reads=[T_st], writes=[T_st])
                                    P.op("scalar", lambda e: e.activation(out=st[:, 13:14], in_=st[:, 12:13], func=AF.Sqrt, scale=1.0 / 128, bias=EPS), reads=[T_st], writes=[T_st])
                                    P.op("vector", lambda e: e.reciprocal(out=st[:, 14:15], in_=st[:, 13:14]), reads=[T_st], writes=[T_st])
                                    P.op("vector", lambda e, qs=qs: e.tensor_scalar(out=o32[:, qs, :], in0=o32[:, qs, :], scalar1=st[:, 14:15], scalar2=float(1.0 - lam_init(l)), op0=ALU.mult, op1=ALU.mult), reads=[T_st, T_o32], writes=[T_o32])
                                    P.op("vector", lambda e, qs=qs, hh=hh: e.tensor_tensor(out=oc[:, qs, hh * 128:(hh + 1) * 128], in0=o32[:, qs, :], in1=subg[:, l * 128:(l + 1) * 128], op=ALU.mult), reads=[T_o32, T_cst], writes=[T_oc])
                    for qs in range(4):
                        transpose_out(oc[:, qs, :], T_oc, 256, OT, T_OT, 4 + 2 * half, 4 * G + qs)
                P.barrier()
                P.flush()

    def mixer_out(l, HT, T_HT, OT, T_OT):
        with contextlib.ExitStack() as es:
            MG = sbuf(es, "o_mg", [128, 8, S], BF16)
            T_MG = [Trk() for _ in range(4)]
            es_a = contextlib.ExitStack()
            WBR = [sbuf(es_a, "o_wbr%d" % k, [128, 8, 128], BF16) for k in range(2)]
            WGT = [sbuf(es_a, "o_wgt%d" % k, [128, 8, 384], BF16) for k in range(2)]
            sg = [sbuf(es_a, "o_sg%d" % k, [128, 512], F32) for k in range(2)]
            mg32 = [sbuf(es_a, "o_m32%d" % k, [128, 512], F32) for k in range(2)]
            T_W = [Trk(), Trk()]
            T_sg = [Trk(), Trk()]
            T_m32 = [Trk(), Trk()]
            wbv = wbr_d[l].rearrange("(c p) f -> p c f", p=128)
            wiv = win_d[l].rearrange("(c p) f -> p c f", p=128)
            feat = [(0, 2), (2, 2), (4, 4)]
            it = 0
            for dc in range(8):
                wb = dc % 2
                P.dma("gpsimd", WBR[wb][:, :, :], wbv[:, :, dc * 128:(dc + 1) * 128], writes=[T_W[wb]])
                for br, nm in enumerate(("ga", "gb", "gc")):
                    P.dma("gpsimd", WGT[wb][:, :, br * 128:(br + 1) * 128], wiv[:, :, OFF[nm] + dc * 128:OFF[nm] + (dc + 1) * 128], writes=[T_W[wb]])
                for tg in range(4):
                    tsl = slice(tg * 512, (tg + 1) * 512)
                    mk = it % 2
                    it += 1
                    for br in range(3):
                        k = (it + br) % 2
                        pgt = PSB[k]
                        py = PSB[2 + k]
                        c0, ncn = feat[br]
                        P.group("tensor", [(lambda e, c=c, br=br, pgt=pgt, wb=wb: e.matmul(pgt[:, :], lhsT=WGT[wb][:, c, br * 128:(br + 1) * 128], rhs=HT[:, c, tsl], start=(c == 0), stop=(c == 7))) for c in range(8)],
                                reads=[T_W[wb]] + T_HT[tg * 4:tg * 4 + 4], writes=[T_ps[k]])
                        P.group("tensor", [(lambda e, c=c, c0=c0, ncn=ncn, py=py, wb=wb: e.matmul(py[:, :], lhsT=WBR[wb][:, c0 + c, :], rhs=OT[:, c0 + c, tsl], start=(c == 0), stop=(c == ncn - 1))) for c in range(ncn)],
                                reads=[T_W[wb]] + T_OT[tg * 4:tg * 4 + 4], writes=[T_ps[2 + k]])
                        P.op("scalar", lambda e, k=k, pgt=pgt: e.activation(out=sg[k][:], in_=pgt[:, :], func=AF.Sigmoid), reads=[T_ps[k]], writes=[T_sg[k]])
                        if br == 0:
                            P.op("vector", lambda e, k=k, py=py, mk=mk: e.tensor_tensor(out=mg32[mk][:], in0=py[:, :], in1=sg[k][:], op=ALU.mult), reads=[T_ps[2 + k], T_sg[k]], writes=[T_m32[mk]])
                        else:
                            P.op("vector", lambda e, k=k, py=py: e.tensor_tensor(out=sg[k][:], in0=py[:, :], in1=sg[k][:], op=ALU.mult), reads=[T_ps[2 + k], T_sg[k]], writes=[T_sg[k]])
                            if br == 1:
                                P.op("gpsimd", lambda e, k=k, mk=mk: e.tensor_tensor(out=mg32[mk][:], in0=mg32[mk][:], in1=sg[k][:], op=ALU.add), reads=[T_sg[k], T_m32[mk]], writes=[T_m32[mk]])
                            else:
                                P.op("gpsimd", lambda e, k=k, mk=mk, dc=dc: e.tensor_tensor(out=MG[:, dc, tsl], in0=mg32[mk][:], in1=sg[k][:], op=ALU.add), reads=[T_sg[k], T_m32[mk]], writes=[T_MG[tg]])
            P.barrier()
            P.flush()
            es_a.close()
            WO = sbuf(es, "o_wo", [128, 8, D], BF16)
            T_WO = Trk()
            wov = wout_d[l].rearrange("(c p) f -> p c f", p=128)
            for hf in range(2):
                P.dma("gpsimd", WO[:, :, hf * 512:(hf + 1) * 512], wov[:, :, hf * 512:(hf + 1) * 512], writes=[T_WO])
            for t in range(NT):
                for hf in range(2):
                    pb = 4 + (t * 2 + hf) % 2
                    pd = PSB[pb]
                    P.group("tensor", [(lambda e, c=c, t=t, hf=hf, pd=pd: e.matmul(pd[:, :], lhsT=MG[:, c, t * 128:(t + 1) * 128], rhs=WO[:, c, hf * 512:(hf + 1) * 512], start=(c == 0), stop=(c == 7))) for c in range(8)],
                            reads=[T_MG[t // 4], T_WO], writes=[T_ps[pb]])
                    P.op("vector", lambda e, t=t, hf=hf, pd=pd: e.tensor_tensor(out=X[:, t, hf * 512:(hf + 1) * 512], in0=pd[:, :], in1=X[:, t, hf * 512:(hf + 1) * 512], op=ALU.add),
                         reads=[T_ps[pb], T_X[t]], writes=[T_X[t]])
            P.barrier()
            P.flush()

    def mixer(l, b):
        with contextlib.ExitStack() as es:
            HT = sbuf(es, "m_HT", [128, 8, S], BF16)
            OT = sbuf(es, "m_OT", [128, 8, S], BF16)
            T_HT = [Trk() for _ in range(NT)]
            T_OT = [Trk() for _ in range(NT)]
            with contextlib.ExitStack() as esn:
                norm_T(esn, l, 1, HT, T_HT)
                P.barrier()
                P.flush()
            if stage >= 2:
                mixer_A(l, HT, T_HT, OT, T_OT)
            if stage >= 3:
                mixer_B(l, HT, T_HT, OT, T_OT)
            if stage >= 4:
                mixer_C(l, HT, T_HT, OT, T_OT, 0)
                mixer_C(l, HT, T_HT, OT, T_OT, 1)
            if dbg and l == layers[0] and b == 0:
                nch = {2: 2, 3: 4}.get(stage, 8)
                P.dma("sync", dbg_ot[:, 0:nch, :], OT[:, 0:nch, :], reads=T_OT, writes=[T_out])
            if stage >= 5:
                mixer_out(l, HT, T_HT, OT, T_OT)
            P.barrier()
            P.flush()

    for b in range(n_seq):
        for t in range(NT):
            P.dma("sync", X[:, t, :], x_d[b, t * 128:(t + 1) * 128, :], writes=[T_X[t]])
        rope_tables(b)
        for l in layers:
            if stage >= 1:
                ffn(l, 0)
            if stage >= 2:
                mixer(l, b)
            if stage >= 6:
                ffn(l, 1)
        for t in range(NT):
            P.dma("sync", out_d[b, t * 128:(t + 1) * 128, :], X[:, t, :], reads=[T_X[t]], writes=[Trk()])
    P.barrier()
    P.flush()
    es0.close()
    P.close()
    build.nins = P.nins
    return nc


def prep_shared(inputs):
    norm_g = np.asarray(inputs["norm_g"], np.float32)
    qk = np.asarray(inputs["qk_norm_g"], np.float32)
    ngT = np.ascontiguousarray(norm_g.reshape(2, 3, 8, 128).transpose(3, 0, 1, 2).reshape(128, 48))
    qkT = qk.reshape(12, 64).T
    qkgT = np.ascontiguousarray(np.concatenate([qkT, qkT], axis=0))
    return {
        "w_in": np.ascontiguousarray(inputs["w_in"], np.float32),
        "w_branch": np.ascontiguousarray(inputs["w_branch"], np.float32),
        "w_out": np.ascontiguousarray(inputs["w_out"], np.float32),
        "ffn_w_gate": np.ascontiguousarray(inputs["ffn_w_gate"], np.float32),
        "ffn_w_up": np.ascontiguousarray(inputs["ffn_w_up"], np.float32),
        "ffn_w_down": np.ascontiguousarray(inputs["ffn_w_down"], np.float32),
        "cst": make_consts(),
        "ngT": ngT,
        "qkgT": qkgT,
        "lamp": np.ascontiguousarray(np.asarray(inputs["lambda_params"], np.float32).reshape(512)),
        "subg": np.ascontiguousarray(np.asarray(inputs["diff_subln_g"], np.float32).reshape(256)),
    }


def kernel(**inputs):
    x = np.asarray(inputs["x"], np.float32)
    pos = np.asarray(inputs["positions"], np.int32)
    shared = prep_shared(inputs)
    nc = build(n_seq=2, layers=(0, 1))
    in_maps = []
    for c in range(NCORES):
        m = dict(shared)
        m["x"] = np.ascontiguousarray(x[2 * c:2 * c + 2])
        m["pos"] = np.ascontiguousarray(pos[2 * c:2 * c + 2])
        in_maps.append(m)
    res = run_bass_kernel_spmd(nc, in_maps, core_ids=list(range(NCORES)))
    out = np.concatenate([np.asarray(r["out"]) for r in res.results], axis=0)
    return out.astype(np.float32)
```

```python
import contextlib
import math
import numpy as np
import concourse.bass as bass
import concourse.mybir as mybir
from concourse.bass_utils import run_bass_kernel_spmd

F32 = mybir.dt.float32
BF16 = mybir.dt.bfloat16
I32 = mybir.dt.int32
AF = mybir.ActivationFunctionType
ALU = mybir.AluOpType
AX = mybir.AxisListType

S = 2048
D = 1024
NT = 16
DFF = 2816
EPS = 1e-6
NCORES = 8
OFF = dict(qa=0, ka=256, va=320, qi=384, ki=896, wi=960, qb=968, kb=1224, vb=1480,
           qc=1736, kc=2248, vc=2760, ga=3272, gb=4296, gc=5320)
NBIS = 16
C_ID, C_BD, C_RM, C_TRI, C_INVF, C_POW = 0, 128, 256, 384, 512, 513
NCST = C_POW + NBIS
NEG = -1.0e30
import os
SUB = int(os.environ.get('SUB', '9'))
PI = math.pi


class Trk:
    __slots__ = ("name", "w", "r")

    def __init__(self, name=""):
        self.name = name
        self.w = None
        self.r = []


class _Rec:
    def __init__(self):
        self.calls = []

    def __getattr__(self, name):
        def f(*a, **k):
            self.calls.append((name, a, k))
            return self
        return f


class Prog:
    ENG = ("tensor", "vector", "scalar", "gpsimd", "sync")

    def __init__(self, nc, n_dma_sems=24):
        self.nc = nc
        self.stack = []
        self.ops = {e: [] for e in self.ENG}
        self.sems = {}
        self.cnt = {}
        self.wm = {e: {} for e in self.ENG}
        for e in self.ENG:
            self._newsem(e)
        self.dma_keys = {}
        self.dma_rr = {}
        for q in ("sync", "gpsimd", "scalar"):
            self.dma_keys[q] = []
            self.dma_rr[q] = 0
            for i in range(n_dma_sems if q != "scalar" else 4):
                k = "dma_%s%d" % (q, i)
                self._newsem(k)
                self.dma_keys[q].append(k)
        self.nins = 0
        self.fill_reg = None

    def _newsem(self, key):
        cm = self.nc.semaphore(key)
        h = cm.__enter__()
        self.stack.append(cm)
        self.sems[key] = h
        self.cnt[key] = 0

    def _waits(self, eng, reads, writes):
        need = {}
        for t in reads:
            if t.w is not None:
                k, v = t.w
                if need.get(k, 0) < v:
                    need[k] = v
        for t in writes:
            if t.w is not None:
                k, v = t.w
                if need.get(k, 0) < v:
                    need[k] = v
            for (k, v) in t.r:
                if need.get(k, 0) < v:
                    need[k] = v
        out = []
        wm = self.wm[eng]
        for k, v in need.items():
            if wm.get(k, 0) < v:
                wm[k] = v
                out.append((k, v))
        return out

    def _mark(self, dep, reads, writes):
        for t in reads:
            t.r.append(dep)
        for t in writes:
            t.w = dep
            t.r = []

    def op(self, eng, fn, reads=(), writes=()):
        return self.group(eng, [fn], reads, writes)

    def group(self, eng, fns, reads=(), writes=()):
        waits = self._waits(eng, reads, writes)
        self.cnt[eng] += 1
        dep = (eng, self.cnt[eng])
        self._mark(dep, reads, writes)
        sem = self.sems[eng]
        sems = self.sems
        self.nins += len(fns)
        rec = _Rec()
        for f in fns:
            f(rec)
        calls = rec.calls

        def run(e, calls=calls, waits=waits, sem=sem):
            for (k, val) in waits:
                e.wait_ge(sems[k], val)
            last = None
            for (name, a, kw) in calls:
                if name == "affine_select":
                    kw = dict(kw)
                    if self.fill_reg is None:
                        self.fill_reg = e.to_reg(kw["fill"])
                    kw["fill"] = self.fill_reg
                last = getattr(e, name)(*a, **kw)
            last.then_inc(sem, 1)
        self.ops[eng].append(run)
        return dep

    def dma(self, q, out, in_, reads=(), writes=(), **kw):
        waits = self._waits(q, reads, writes)
        k = self.dma_keys[q][self.dma_rr[q] % len(self.dma_keys[q])]
        self.dma_rr[q] += 1
        prev = self.cnt[k]
        if prev > 0 and self.wm[q].get(k, 0) < prev:
            self.wm[q][k] = prev
            waits.append((k, prev))
        self.cnt[k] += 16
        dep = (k, self.cnt[k])
        self._mark(dep, reads, writes)
        sems = self.sems
        sem = sems[k]
        self.nins += 1

        def run(e, waits=waits, sem=sem, out=out, in_=in_, kw=kw):
            for (kk, val) in waits:
                e.wait_ge(sems[kk], val)
            e.dma_start(out=out, in_=in_, **kw).then_inc(sem, 16)
        self.ops[q].append(run)
        return dep

    def barrier(self):
        sems = self.sems
        snap = [(k, v) for k, v in self.cnt.items() if v > 0]
        for eng in self.ENG:
            waits = []
            for (k, v) in snap:
                if k == eng:
                    continue
                if self.wm[eng].get(k, 0) < v:
                    self.wm[eng][k] = v
                    waits.append((k, v))

            def run(e, waits=waits):
                for (k, val) in waits:
                    e.wait_ge(sems[k], val)
            self.ops[eng].append(run)

    def flush(self):
        nc = self.nc
        ops = self.ops
        with nc.Block() as block:
            @block.tensor
            def _(e):
                for f in ops["tensor"]:
                    f(e)

            @block.vector
            def _(e):
                for f in ops["vector"]:
                    f(e)

            @block.scalar
            def _(e):
                for f in ops["scalar"]:
                    f(e)

            @block.gpsimd
            def _(e):
                for f in ops["gpsimd"]:
                    f(e)

            @block.sync
            def _(e):
                for f in ops["sync"]:
                    f(e)
        self.ops = {e: [] for e in self.ENG}

    def close(self):
        for cm in reversed(self.stack):
            cm.__exit__(None, None, None)


def make_consts():
    c = np.zeros((128, NCST), np.float32)
    c[:, C_ID:C_ID + 128] = np.eye(128, dtype=np.float32)
    for p in range(128):
        for f in range(128):
            if p // 64 == f // 64:
                c[p, C_BD + f] = 1.0
    for f in range(128):
        r = f % 64
        if r < 8:
            c[f + 8, C_RM + f] = -1.0
        elif r < 16:
            c[f - 8, C_RM + f] = 1.0
    for k in range(128):
        c[k, C_TRI + k:C_TRI + 128] = 1.0
    inv = np.power(np.float32(500000.0), -np.arange(0, 16, 2, dtype=np.float32) / np.float32(16.0)).astype(np.float32)
    for p in range(128):
        r = p % 64
        c[p, C_INVF] = inv[r % 8] if r < 16 else 0.0
    for i in range(NBIS):
        c[:, C_POW + i] = 2.0 ** (-(i + 1))
    return c


def build(n_seq=2, layers=(0, 1), stage=99, dbg=False):
    nc = bass.Bass("TRN2", target_bir_lowering=False)
    dt = nc.dram_tensor
    x_d = dt("x", [n_seq, S, D], F32, kind="ExternalInput").ap()
    pos_d = dt("pos", [n_seq, S], I32, kind="ExternalInput").ap()
    win_d = dt("w_in", [2, D, 6344], F32, kind="ExternalInput").ap()
    wbr_d = dt("w_branch", [2, D, D], F32, kind="ExternalInput").ap()
    wout_d = dt("w_out", [2, D, D], F32, kind="ExternalInput").ap()
    wg_d = dt("ffn_w_gate", [2, 2, D, DFF], F32, kind="ExternalInput").ap()
    wu_d = dt("ffn_w_up", [2, 2, D, DFF], F32, kind="ExternalInput").ap()
    wd_d = dt("ffn_w_down", [2, 2, DFF, D], F32, kind="ExternalInput").ap()
    cst_d = dt("cst", [128, NCST], F32, kind="ExternalInput").ap()
    ngT_d = dt("ngT", [128, 48], F32, kind="ExternalInput").ap()
    qkgT_d = dt("qkgT", [128, 12], F32, kind="ExternalInput").ap()
    lam_d = dt("lamp", [512], F32, kind="ExternalInput").ap()
    sub_d = dt("subg", [256], F32, kind="ExternalInput").ap()
    out_d = dt("out", [n_seq, S, D], F32, kind="ExternalOutput").ap()
    if dbg:
        dbg_ot = dt("dbg_ot", [128, 8, S], BF16, kind="ExternalOutput").ap()

    P = Prog(nc)
    es0 = contextlib.ExitStack()

    uid = [0]

    def sbuf(es, name, shape, dtype):
        uid[0] += 1
        return es.enter_context(nc.sbuf_tensor("s%d_%s" % (uid[0], name), shape, dtype))

    X = sbuf(es0, "X", [128, NT, D], F32)
    CT = sbuf(es0, "CT", [128, S], BF16)
    STb = sbuf(es0, "ST", [128, S], BF16)
    cstf = sbuf(es0, "cstf", [128, NCST], F32)
    identb = sbuf(es0, "identb", [128, 128], BF16)
    BDb = sbuf(es0, "BDb", [128, 128], BF16)
    RMb = sbuf(es0, "RMb", [128, 128], BF16)
    trib = sbuf(es0, "trib", [128, 128], BF16)
    onesb = sbuf(es0, "onesb", [128, 128], BF16)
    ngT = sbuf(es0, "ngT", [128, 48], F32)
    qkgT = sbuf(es0, "qkgT", [128, 12], F32)
    subg = sbuf(es0, "subg", [128, 256], F32)
    lamv = sbuf(es0, "lamv", [128, 8], F32)
    rs_x = sbuf(es0, "rs_x", [128, 2 * NT], F32)
    PSB = [es0.enter_context(nc.psum_tensor("psb%d" % i, [128, 512], F32)) for i in range(7)]
    PSH = es0.enter_context(nc.psum_tensor("psh", [128, 1024], BF16))
    T_ps = [Trk("ps%d" % i) for i in range(7)]
    T_psh = Trk("psh")
    T_X = [Trk("X%d" % i) for i in range(NT)]
    T_cst = Trk("cst")
    T_tab = Trk("tab")
    T_out = Trk("out")
    T_lam = Trk("lam")

    def lam_init(l):
        return 0.8 - 0.6 * math.exp(-0.3 * l)

    P.dma("sync", cstf[:], cst_d, writes=[T_cst])
    P.dma("gpsimd", identb[:], cst_d[:, C_ID:C_ID + 128], writes=[T_cst])
    P.dma("gpsimd", BDb[:], cst_d[:, C_BD:C_BD + 128], writes=[T_cst])
    P.dma("gpsimd", RMb[:], cst_d[:, C_RM:C_RM + 128], writes=[T_cst])
    P.dma("gpsimd", trib[:], cst_d[:, C_TRI:C_TRI + 128], writes=[T_cst])
    P.dma("sync", ngT[:], ngT_d, writes=[T_cst])
    P.dma("sync", qkgT[:], qkgT_d, writes=[T_cst])
    P.dma("sync", subg[:], sub_d.partition_broadcast(128), writes=[T_cst])
    P.op("vector", lambda e: e.memset(onesb[:], 1.0), writes=[T_cst])
    with contextlib.ExitStack() as es:
        lamp = sbuf(es, "lamp", [128, 512], F32)
        tmp = sbuf(es, "lamtmp", [128, 512], F32)
        sums = sbuf(es, "lamsum", [128, 8], F32)
        T_t = Trk()
        P.dma("sync", lamp[:], lam_d.partition_broadcast(128), writes=[T_lam])
        for l in range(2):
            for j in range(2):
                a0 = l * 256 + (2 * j) * 64
                P.op("vector", lambda e, a0=a0: e.tensor_tensor(out=tmp[:, a0:a0 + 64], in0=lamp[:, a0:a0 + 64], in1=lamp[:, a0 + 64:a0 + 128], op=ALU.mult),
                     reads=[T_lam], writes=[T_t])
                P.op("vector", lambda e, a0=a0, l=l, j=j: e.reduce_sum(out=sums[:, 2 * l + j:2 * l + j + 1], in_=tmp[:, a0:a0 + 64], axis=AX.X),
                     reads=[T_t], writes=[T_t])
        P.op("scalar", lambda e: e.activation(out=sums[:, 4:8], in_=sums[:, 0:4], func=AF.Exp), reads=[T_t], writes=[T_t])
        for l in range(2):
            P.op("vector", lambda e, l=l: e.scalar_tensor_tensor(out=lamv[:, l:l + 1], in0=sums[:, 5 + 2 * l:6 + 2 * l], scalar=-lam_init(l),
                                                               in1=sums[:, 4 + 2 * l:5 + 2 * l], op0=ALU.add, op1=ALU.subtract),
                 reads=[T_t], writes=[T_lam])
        P.barrier()
        P.flush()

    def rope_tables(b):
        with contextlib.ExitStack() as es:
            posi = sbuf(es, "posi", [128, S], I32)
            a = sbuf(es, "ta", [128, S], F32)
            r = sbuf(es, "tr", [128, S], F32)
            ki = sbuf(es, "tki", [128, S], I32)
            kf = sbuf(es, "tkf", [128, S], F32)
            T = Trk()
            P.dma("sync", posi[:], pos_d[b].partition_broadcast(128), writes=[T])
            V = lambda fn: P.op("vector", fn, reads=[T, T_cst], writes=[T, T_tab])
            V(lambda e: e.tensor_copy(out=a[:], in_=posi[:]))
            V(lambda e: e.tensor_scalar(out=a[:], in0=a[:], scalar1=cstf[:, C_INVF:C_INVF + 1], scalar2=None, op0=ALU.mult))
            V(lambda e: e.tensor_scalar(out=r[:], in0=a[:], scalar1=float(1.0 / (2 * PI)), scalar2=None, op0=ALU.mult))
            V(lambda e: e.tensor_copy(out=ki[:], in_=r[:]))
            V(lambda e: e.tensor_copy(out=kf[:], in_=ki[:]))
            V(lambda e: e.scalar_tensor_tensor(out=r[:], in0=kf[:], scalar=-6.28125, in1=a[:], op0=ALU.mult, op1=ALU.add))
            V(lambda e: e.scalar_tensor_tensor(out=r[:], in0=kf[:], scalar=-(2 * PI - 6.28125), in1=r[:], op0=ALU.mult, op1=ALU.add))

            def wrap(t):
                V(lambda e: e.tensor_scalar(out=kf[:], in0=t[:], scalar1=PI, scalar2=-2 * PI, op0=ALU.is_gt, op1=ALU.mult))
                V(lambda e: e.tensor_tensor(out=t[:], in0=t[:], in1=kf[:], op=ALU.add))
                V(lambda e: e.tensor_scalar(out=kf[:], in0=t[:], scalar1=-PI, scalar2=2 * PI, op0=ALU.is_lt, op1=ALU.mult))
                V(lambda e: e.tensor_tensor(out=t[:], in0=t[:], in1=kf[:], op=ALU.add))
                V(lambda e: e.tensor_scalar(out=t[:], in0=t[:], scalar1=3.1415925, scalar2=-3.1415925, op0=ALU.min, op1=ALU.max))
            wrap(r)
            P.op("scalar", lambda e: e.activation(out=STb[:], in_=r[:], func=AF.Sin), reads=[T], writes=[T_tab, T])
            V(lambda e: e.tensor_scalar(out=a[:], in0=r[:], scalar1=float(PI / 2), scalar2=None, op0=ALU.add))
            wrap(a)
            P.op("scalar", lambda e: e.activation(out=CT[:], in_=a[:], func=AF.Sin), reads=[T], writes=[T_tab, T])
            P.barrier()
            P.flush()

    def norm_T(es, l, i, HT, T_HT):
        xsq = sbuf(es, "n_xsq", [128, D], BF16)
        xn = [sbuf(es, "n_xn%d" % k, [128, D], BF16) for k in range(2)]
        T_sq = Trk()
        T_xn = [Trk(), Trk()]
        T_rs = Trk()
        for t in range(NT):
            P.op("scalar", lambda e, t=t: e.activation(out=xsq[:], in_=X[:, t, :], func=AF.Square, accum_out=rs_x[:, t:t + 1]),
                 reads=[T_X[t]], writes=[T_sq, T_rs])
        P.op("scalar", lambda e: e.activation(out=rs_x[:, NT:2 * NT], in_=rs_x[:, 0:NT], func=AF.Sqrt, scale=1.0 / D, bias=EPS),
             reads=[T_rs], writes=[T_rs])
        P.op("vector", lambda e: e.reciprocal(out=rs_x[:, 0:NT], in_=rs_x[:, NT:2 * NT]), reads=[T_rs], writes=[T_rs])
        g0 = (l * 3 + i) * 8
        psT = PSH[:, :].rearrange("p (c t) -> p c t", c=8)
        for t in range(NT):
            k = t % 2
            P.op("vector", lambda e, t=t, k=k: e.tensor_scalar(out=xn[k][:], in0=X[:, t, :], scalar1=rs_x[:, t:t + 1], scalar2=None, op0=ALU.mult),
                 reads=[T_X[t], T_rs], writes=[T_xn[k]])
            P.group("tensor", [(lambda e, c=c, k=k: e.transpose(out=psT[:, c, :], in_=xn[k][:, c * 128:(c + 1) * 128], identity=identb[:])) for c in range(8)],
                    reads=[T_xn[k], T_cst], writes=[T_psh])
            P.op("vector", lambda e, t=t: e.tensor_tensor(out=HT[:, :, t * 128:(t + 1) * 128], in0=psT,
                                                         in1=ngT[:, g0:g0 + 8].unsqueeze(2).to_broadcast([128, 8, 128]), op=ALU.mult),
                 reads=[T_psh, T_cst], writes=[T_HT[t]])

    def ffn(l, i):
        with contextlib.ExitStack() as es:
            HT = sbuf(es, "f_HT", [128, 8, S], BF16)
            T_HT = [Trk() for _ in range(NT)]
            norm_T(es, l, i, HT, T_HT)
            WG = [sbuf(es, "f_wg%d" % k, [128, 8, 512], BF16) for k in range(2)]
            WU = [sbuf(es, "f_wu%d" % k, [128, 8, 512], BF16) for k in range(2)]
            WD = [sbuf(es, "f_wd%d" % k, [128, 4, D], BF16) for k in range(2)]
            AT = [sbuf(es, "f_at%d" % k, [128, 4, 512], BF16) for k in range(2)]
            SG = [sbuf(es, "f_sg%d" % k, [128, 512], F32) for k in range(2)]
            T_W = [Trk(), Trk()]
            T_AT = [Trk(), Trk()]
            T_SG = [Trk(), Trk()]
            wgv = wg_d[l, i].rearrange("(c p) f -> p c f", p=128)
            wuv = wu_d[l, i].rearrange("(c p) f -> p c f", p=128)
            groups = [(f0, min(512, DFF - f0)) for f0 in range(0, DFF, 512)]
            it = 0
            for gi, (f0, fw) in enumerate(groups):
                wb = gi % 2
                nfb = fw // 128
                P.dma("gpsimd", WG[wb][:, :, 0:fw], wgv[:, :, f0:f0 + fw], writes=[T_W[wb]])
                P.dma("gpsimd", WU[wb][:, :, 0:fw], wuv[:, :, f0:f0 + fw], writes=[T_W[wb]])
                P.dma("gpsimd", WD[wb][:, 0:nfb, :], wd_d[l, i][f0:f0 + fw, :].rearrange("(c p) d -> p c d", p=128), writes=[T_W[wb]])
                for tg in range(4):
                    ab = tg % 2
                    for fb in range(nfb):
                        k = it % 2
                        it += 1
                        pg, pu = PSB[2 * k], PSB[2 * k + 1]
                        P.group("tensor", [(lambda e, c=c, fb=fb, wb=wb, tg=tg, pg=pg: e.matmul(pg[:, :], lhsT=WG[wb][:, c, fb * 128:(fb + 1) * 128], rhs=HT[:, c, tg * 512:(tg + 1) * 512], start=(c == 0), stop=(c == 7))) for c in range(8)],
                                reads=[T_W[wb]] + T_HT[tg * 4:tg * 4 + 4], writes=[T_ps[2 * k]])
                        P.group("tensor", [(lambda e, c=c, fb=fb, wb=wb, tg=tg, pu=pu: e.matmul(pu[:, :], lhsT=WU[wb][:, c, fb * 128:(fb + 1) * 128], rhs=HT[:, c, tg * 512:(tg + 1) * 512], start=(c == 0), stop=(c == 7))) for c in range(8)],
                                reads=[T_W[wb]] + T_HT[tg * 4:tg * 4 + 4], writes=[T_ps[2 * k + 1]])
                        P.op("scalar", lambda e, k=k, pg=pg: e.activation(out=SG[k][:], in_=pg[:, :], func=AF.Silu), reads=[T_ps[2 * k]], writes=[T_SG[k]])
                        P.op("vector", lambda e, k=k, pu=pu, ab=ab, fb=fb: e.tensor_tensor(out=AT[ab][:, fb, :], in0=pu[:, :], in1=SG[k][:], op=ALU.mult),
                             reads=[T_ps[2 * k + 1], T_SG[k]], writes=[T_AT[ab]])
                    for tt in range(4):
                        t = tg * 4 + tt
                        for hf in range(2):
                            pb = 4 + (t * 2 + hf) % 2
                            pd = PSB[pb]
                            P.group("tensor", [(lambda e, fb=fb, ab=ab, tt=tt, wb=wb, hf=hf, pd=pd: e.matmul(pd[:, :], lhsT=AT[ab][:, fb, tt * 128:(tt + 1) * 128], rhs=WD[wb][:, fb, hf * 512:(hf + 1) * 512], start=(fb == 0), stop=(fb == nfb - 1))) for fb in range(nfb)],
                                    reads=[T_AT[ab], T_W[wb]], writes=[T_ps[pb]])
                            P.op("vector", lambda e, t=t, hf=hf, pd=pd: e.scalar_tensor_tensor(out=X[:, t, hf * 512:(hf + 1) * 512], in0=pd[:, :], scalar=0.5, in1=X[:, t, hf * 512:(hf + 1) * 512], op0=ALU.mult, op1=ALU.add),
                                 reads=[T_ps[pb], T_X[t]], writes=[T_X[t]])
            P.barrier()
            P.flush()

    def proj_fm(es_tmp, HT, T_HT, W, T_W, col0, tg, dst, T_dst, mode, gcol):
        k = proj_fm.it % 2
        proj_fm.it += 1
        tm = proj_fm.tmp
        pq = PSB[k]
        tsl = slice(tg * 512, (tg + 1) * 512)
        P.group("tensor", [(lambda e, c=c: e.matmul(pq[:, :], lhsT=W[:, c, col0:col0 + 128], rhs=HT[:, c, tsl], start=(c == 0), stop=(c == 7))) for c in range(8)],
                reads=[T_W] + T_HT[tg * 4:tg * 4 + 4], writes=[T_ps[k]])
        xn = tm["xn"][k]
        T_xn = tm["T_xn"][k]
        if mode == "norm":
            xsq = tm["xsq"][k]
            T_xsq = tm["T_xsq"][k]
            sd = tm["sd"][k]
            T_sd = tm["T_sd"][k]
            pss = PSB[2 + k]
            P.op("scalar", lambda e: e.activation(out=xsq[:], in_=pq[:, :], func=AF.Square), reads=[T_ps[k]], writes=[T_xsq])
            P.group("tensor", [lambda e: e.matmul(pss[:, :], lhsT=BDb[:], rhs=xsq[:], start=True, stop=True)], reads=[T_xsq, T_cst], writes=[T_ps[2 + k]])
            P.op("scalar", lambda e: e.activation(out=sd[:], in_=pss[:, :], func=AF.Sqrt, scale=1.0 / 64, bias=EPS), reads=[T_ps[2 + k]], writes=[T_sd])
            P.op("vector", lambda e: e.reciprocal(out=sd[:], in_=sd[:]), reads=[T_sd], writes=[T_sd])
            P.op("vector", lambda e: e.scalar_tensor_tensor(out=xn[:], in0=pq[:, :], scalar=gcol, in1=sd[:], op0=ALU.mult, op1=ALU.mult),
                 reads=[T_ps[k], T_sd, T_cst], writes=[T_xn])
        else:
            P.op("scalar", lambda e: e.copy(out=xn[:], in_=pq[:, :]), reads=[T_ps[k]], writes=[T_xn])
        pr = PSB[4 + k]
        t1 = tm["t1"][k]
        T_t1 = tm["T_t1"][k]
        t2 = tm["t2"][k]
        T_t2 = tm["T_t2"][k]
        P.group("tensor", [lambda e: e.matmul(pr[:, :], lhsT=RMb[:], rhs=xn[:], start=True, stop=True)], reads=[T_xn, T_cst], writes=[T_ps[4 + k]])
        P.op("gpsimd", lambda e: e.tensor_tensor(out=t1[:], in0=xn[:], in1=CT[:, tsl], op=ALU.mult), reads=[T_xn, T_tab], writes=[T_t1])
        P.op("vector", lambda e: e.tensor_tensor(out=t2[:], in0=pr[:, :], in1=STb[:, tsl], op=ALU.mult), reads=[T_ps[4 + k], T_tab], writes=[T_t2])
        P.op("vector", lambda e: e.tensor_tensor(out=dst, in0=t1[:], in1=t2[:], op=ALU.add), reads=[T_t1, T_t2], writes=[T_dst])
    proj_fm.it = 0

    def proj_tmp(es):
        tm = {}
        for nm, dtp in (("xn", BF16), ("xsq", BF16), ("sd", F32)):
            tm[nm] = [sbuf(es, "pj_%s%d" % (nm, k), [128, 512], dtp) for k in range(2)]
            tm["T_" + nm] = [Trk(), Trk()]
        for nm, dtp in (("t1", F32), ("t2", F32)):
            buf = sbuf(es, "pj_%s" % nm, [128, 512], dtp)
            tk = Trk()
            tm[nm] = [buf, buf]
            tm["T_" + nm] = [tk, tk]
        proj_fm.tmp = tm

    def load_w_in(Wt, T_W, l, segs):
        wv = win_d[l].rearrange("(c p) f -> p c f", p=128)
        for (d0, s0, n) in segs:
            P.dma("gpsimd", Wt[:, :, d0:d0 + n], wv[:, :, s0:s0 + n], writes=[T_W])

    def proj_tm(HT, T_HT, Wv, T_Wv, ncol, t, pbank):
        pv = PSB[pbank]
        P.group("tensor", [(lambda e, c=c: e.matmul(pv[:, 0:ncol], lhsT=HT[:, c, t * 128:(t + 1) * 128], rhs=Wv[:, c, 0:ncol], start=(c == 0), stop=(c == 7))) for c in range(8)],
                reads=[T_Wv, T_HT[t]], writes=[T_ps[pbank]])
        return pv

    def transpose_out(o_tile, T_o, ncol, OT, T_OT, chunk0, t):
        n = ncol // 128
        psT = PSH[:, 0:n * 128].rearrange("p (c t) -> p c t", c=n)
        P.group("tensor", [(lambda e, j=j: e.transpose(out=psT[:, j, :], in_=o_tile[:, j * 128:(j + 1) * 128], identity=identb[:])) for j in range(n)],
                reads=[T_o, T_cst], writes=[T_psh])
        P.op("scalar", lambda e: e.copy(out=OT[:, chunk0:chunk0 + n, t * 128:(t + 1) * 128], in_=psT), reads=[T_psh], writes=[T_OT[t]])

    def mixer_A(l, HT, T_HT, OT, T_OT):
        with contextlib.ExitStack() as es:
            QA = sbuf(es, "a_qa", [128, 2, S], BF16)
            KA = sbuf(es, "a_ka", [128, S], BF16)
            QI = sbuf(es, "a_qi", [128, 4, S], BF16)
            KI = sbuf(es, "a_ki", [128, S], BF16)
            VA = sbuf(es, "a_va", [128, NT, 65], BF16)
            WI = sbuf(es, "a_wi", [128, NT, 8], F32)
            T_Q = [Trk() for _ in range(4)]
            T_V = [Trk() for _ in range(NT)]
            with contextlib.ExitStack() as es1:
                W = sbuf(es1, "a_w", [128, 8, 1024], BF16)
                Wv = sbuf(es1, "a_wv", [128, 8, 72], BF16)
                T_W = Trk()
                T_Wv = Trk()
                proj_tmp(es1)
                load_w_in(W, T_W, l, [(0, OFF["qa"], 256), (256, OFF["ka"], 64), (320, OFF["ka"], 64),
                                      (384, OFF["qi"], 512), (896, OFF["ki"], 64), (960, OFF["ki"], 64)])
                load_w_in(Wv, T_Wv, l, [(0, OFF["va"], 64), (64, OFF["wi"], 8)])
                P.op("vector", lambda e: e.memset(VA[:, :, 64:65], 1.0), writes=T_V)
                gq = qkgT[:, l * 6 + 0:l * 6 + 1]
                gk = qkgT[:, l * 6 + 1:l * 6 + 2]
                for tg in range(4):
                    tsl = slice(tg * 512, (tg + 1) * 512)
                    for p in range(2):
                        proj_fm(es1, HT, T_HT, W, T_W, p * 128, tg, QA[:, p, tsl], T_Q[tg], "norm", gq)
                    proj_fm(es1, HT, T_HT, W, T_W, 256, tg, KA[:, tsl], T_Q[tg], "norm", gk)
                    for p in range(4):
                        proj_fm(es1, HT, T_HT, W, T_W, 384 + p * 128, tg, QI[:, p, tsl], T_Q[tg], "rope", None)
                    proj_fm(es1, HT, T_HT, W, T_W, 896, tg, KI[:, tsl], T_Q[tg], "rope", None)
                for t in range(NT):
                    pv = proj_tm(HT, T_HT, Wv, T_Wv, 72, t, 6)
                    P.op("scalar", lambda e, t=t, pv=pv: e.copy(out=VA[:, t, 0:64], in_=pv[:, 0:64]), reads=[T_ps[6]], writes=[T_V[t]])
                    P.op("vector", lambda e, t=t, pv=pv: e.tensor_copy(out=WI[:, t, :], in_=pv[:, 64:72]), reads=[T_ps[6]], writes=[T_V[t]])
                P.barrier()
                P.flush()
            if SUB == 1:
                return
            with contextlib.ExitStack() as es2:
                ISC = [sbuf(es2, "a_isc%d" % k, [128, S], F32) for k in range(2)]
                M = sbuf(es2, "a_m", [128, S], BF16)
                MT = sbuf(es2, "a_mt", [128, NT, 128], BF16)
                RL = [sbuf(es2, "a_rl%d" % k, [128, 512], F32) for k in range(2)]
                bis = sbuf(es2, "a_bis", [128, 32], F32)
                stp = sbuf(es2, "a_stp", [128, NBIS], F32)
                oa = sbuf(es2, "a_oa", [128, 256], BF16)
                rc = sbuf(es2, "a_rc", [128, 4], F32)
                T_isc = [Trk(), Trk()]
                T_dead = [Trk(), Trk()]
                T_M, T_MT, T_bis, T_oa = Trk(), Trk(), Trk(), Trk()
                T_PTk = [Trk() for _ in range(NT)]
                T_RL = [Trk(), Trk()]
                itc = [0, 0]

                def indexer(qt):
                    ib = qt % 2
                    L = 128 * (qt + 1)
                    qsl = slice(qt * 128, (qt + 1) * 128)
                    tgq = qt // 4
                    nch = (L + 511) // 512
                    for ch in range(nch):
                        c0 = ch * 512
                        cw = min(512, L - c0)
                        for h in range(8):
                            k = itc[0] % 2
                            itc[0] += 1
                            hp = (h % 2) * 64
                            pl = PSB[k]
                            P.group("tensor", [lambda e: e.matmul(pl[:, 0:cw], lhsT=QI[hp:hp + 64, h // 2, qsl], rhs=KI[hp:hp + 64, c0:c0 + cw], start=True, stop=True)],
                                    reads=T_Q[0:tgq + 1], writes=[T_ps[k]])
                            if h == 0:
                                P.op("vector", lambda e: e.tensor_scalar(out=ISC[ib][:, c0:c0 + cw], in0=pl[:, 0:cw], scalar1=0.0, scalar2=WI[:, qt, h:h + 1], op0=ALU.max, op1=ALU.mult),
                                     reads=[T_ps[k], T_V[qt]], writes=[T_isc[ib]])
                            else:
                                P.op("vector", lambda e: e.tensor_scalar(out=RL[k][:, 0:cw], in0=pl[:, 0:cw], scalar1=0.0, scalar2=WI[:, qt, h:h + 1], op0=ALU.max, op1=ALU.mult),
                                     reads=[T_ps[k], T_V[qt]], writes=[T_RL[k]])
                                P.op("gpsimd", lambda e: e.tensor_tensor(out=ISC[ib][:, c0:c0 + cw], in0=ISC[ib][:, c0:c0 + cw], in1=RL[k][:, 0:cw], op=ALU.add),
                                     reads=[T_RL[k], T_isc[ib]], writes=[T_isc[ib]])
                            yield

                def rest(qt):
                    ib = qt % 2
                    L = 128 * (qt + 1)
                    qsl = slice(qt * 128, (qt + 1) * 128)
                    tgq = qt // 4
                    isc = ISC[ib]
                    PT = isc[:, :].bitcast(BF16).rearrange("p (k h q) -> p k h q", k=NT, h=2)
                    Vb = lambda fn: P.op("vector", fn, reads=[T_isc[ib], T_bis, T_cst], writes=[T_bis])
                    if qt >= 2:
                        Vb(lambda e: e.tensor_reduce(out=bis[:, 0:1], in_=isc[:, 0:L], axis=AX.X, op=ALU.min))
                        Vb(lambda e: e.tensor_reduce(out=bis[:, 1:2], in_=isc[:, 0:L], axis=AX.X, op=ALU.max))
                        yield
                    P.op("gpsimd", lambda e: e.affine_select(out=isc[:, L - 128:L], in_=isc[:, L - 128:L], pattern=[[-1, 128]], compare_op=ALU.is_ge, fill=NEG, base=0, channel_multiplier=1),
                         reads=[T_isc[ib], T_bis], writes=[T_isc[ib]])
                    if qt >= 2:
                        Vb(lambda e: e.tensor_tensor(out=bis[:, 2:3], in0=bis[:, 1:2], in1=bis[:, 0:1], op=ALU.subtract))
                        Vb(lambda e: e.tensor_scalar(out=bis[:, 2:3], in0=bis[:, 2:3], scalar1=1.0001, scalar2=1e-20, op0=ALU.mult, op1=ALU.add))
                        Vb(lambda e: e.tensor_scalar(out=stp[:, :], in0=cstf[:, C_POW:C_POW + NBIS], scalar1=bis[:, 2:3], scalar2=None, op0=ALU.mult))
                        for i in range(NBIS):
                            Vb(lambda e: e.scalar_tensor_tensor(out=bis[:, 3:4], in0=bis[:, 0:1], scalar=-1.0, in1=stp[:, i:i + 1], op0=ALU.mult, op1=ALU.subtract))
                            P.op("scalar", lambda e: e.activation(out=M[:, 0:L], in_=isc[:, 0:L], func=AF.Sign, bias=bis[:, 3:4], scale=1.0, accum_out=bis[:, 4:5]),
                                 reads=[T_isc[ib], T_bis], writes=[T_M, T_bis])
                            Vb(lambda e: e.tensor_scalar(out=bis[:, 5:6], in0=bis[:, 4:5], scalar1=float(511 - L), scalar2=stp[:, i:i + 1], op0=ALU.is_ge, op1=ALU.mult))
                            Vb(lambda e: e.tensor_tensor(out=bis[:, 0:1], in0=bis[:, 0:1], in1=bis[:, 5:6], op=ALU.add))
                            yield
                        P.op("vector", lambda e: e.tensor_scalar(out=M[:, 0:L], in0=isc[:, 0:L], scalar1=bis[:, 0:1], scalar2=None, op0=ALU.is_ge),
                             reads=[T_isc[ib], T_bis], writes=[T_M, T_dead[ib]])
                    else:
                        P.op("vector", lambda e: e.tensor_scalar(out=M[:, 0:L], in0=isc[:, 0:L], scalar1=-1.0e29, scalar2=None, op0=ALU.is_ge),
                             reads=[T_isc[ib], T_bis], writes=[T_M, T_dead[ib]])
                    yield
                    for k0 in range(0, qt + 1, 8):
                        n = min(8, qt + 1 - k0)
                        psT = PSH[:, 0:n * 128].rearrange("p (c t) -> p c t", c=n)
                        P.group("tensor", [(lambda e, j=j: e.transpose(out=psT[:, j, :], in_=M[:, (k0 + j) * 128:(k0 + j + 1) * 128], identity=identb[:])) for j in range(n)],
                                reads=[T_M, T_cst], writes=[T_psh])
                        P.op("scalar", lambda e: e.copy(out=MT[:, k0:k0 + n, :], in_=psT), reads=[T_psh], writes=[T_MT])
                        yield
                    po = PSB[6]
                    for pr in range(2):
                        for kt in range(qt + 1):
                            k = itc[1] % 2
                            itc[1] += 1
                            ksl = slice(kt * 128, (kt + 1) * 128)
                            for hh in range(2):
                                pb = 2 + 2 * hh + k
                                ps_s = PSB[pb]
                                P.group("tensor", [lambda e: e.matmul(ps_s[:, 0:128], lhsT=KA[hh * 64:hh * 64 + 64, ksl], rhs=QA[hh * 64:hh * 64 + 64, pr, qsl], start=True, stop=True)],
                                        reads=T_Q[0:tgq + 1], writes=[T_ps[pb]])
                                P.op("scalar", lambda e: e.activation(out=PT[:, kt, hh, :], in_=ps_s[:, 0:128], func=AF.Exp, scale=0.125), reads=[T_ps[pb], T_dead[ib]], writes=[T_PTk[kt]])
                            P.op("vector", lambda e: e.tensor_tensor(out=PT[:, kt, :, :], in0=PT[:, kt, :, :], in1=MT[:, kt:kt + 1, :].to_broadcast([128, 2, 128]), op=ALU.mult),
                                 reads=[T_PTk[kt], T_MT], writes=[T_PTk[kt]])
                            yield
                        pov = po[:, pr * 130:(pr + 1) * 130].rearrange("p (h e) -> p h e", h=2)
                        for hh in range(2):
                            P.group("tensor", [(lambda e, kt=kt: e.matmul(pov[:, hh, :], lhsT=PT[:, kt, hh, :], rhs=VA[:, kt, :], start=(kt == 0), stop=(kt == qt))) for kt in range(qt + 1)],
                                    reads=T_PTk[0:qt + 1] + T_V[0:qt + 1] + [T_isc[ib]], writes=[T_ps[6]])
                        P.op("vector", lambda e: e.reciprocal(out=rc[:, 2 * pr:2 * pr + 2], in_=pov[:, :, 64]), reads=[T_ps[6]], writes=[T_bis])
                        P.op("vector", lambda e: e.tensor_tensor(out=oa[:, pr * 128:(pr + 1) * 128].rearrange("p (h e) -> p h e", h=2), in0=pov[:, :, 0:64],
                                                                 in1=rc[:, 2 * pr:2 * pr + 2].unsqueeze(2).to_broadcast([128, 2, 64]), op=ALU.mult),
                             reads=[T_ps[6], T_bis], writes=[T_oa])
                        yield
                    transpose_out(oa, T_oa, 256, OT, T_OT, 0, qt)
                    yield

                def interleave(ga, gb):
                    da = db = False
                    while not (da and db):
                        if not da:
                            try:
                                next(ga)
                            except StopIteration:
                                da = True
                        if not db:
                            try:
                                next(gb)
                            except StopIteration:
                                db = True

                for _ in indexer(0):
                    pass
                for qt in range(NT):
                    interleave(rest(qt), indexer(qt + 1) if qt + 1 < NT else iter(()))
                P.barrier()
                P.flush()

    def mixer_B(l, HT, T_HT, OT, T_OT):
        with contextlib.ExitStack() as es:
            QB = sbuf(es, "b_q", [128, 2, S], BF16)
            KB = sbuf(es, "b_k", [128, 2, S], BF16)
            VB = sbuf(es, "b_v", [128, NT, 4, 65], BF16)
            KM = sbuf(es, "b_km", [128, 2, 8], BF16)
            T_Q = [Trk() for _ in range(4)]
            T_V = [Trk() for _ in range(NT)]
            T_KM = Trk()
            with contextlib.ExitStack() as es1:
                W = sbuf(es1, "b_w", [128, 8, 512], BF16)
                Wv = sbuf(es1, "b_wv", [128, 8, 256], BF16)
                kmf = sbuf(es1, "b_kmf", [128, 2, 8], F32)
                T_W, T_Wv = Trk(), Trk()
                proj_tmp(es1)
                load_w_in(W, T_W, l, [(0, OFF["qb"], 256), (256, OFF["kb"], 256)])
                load_w_in(Wv, T_Wv, l, [(0, OFF["vb"], 256)])
                P.op("vector", lambda e: e.memset(VB[:, :, :, 64:65], 1.0), writes=T_V)
                gq = qkgT[:, l * 6 + 2:l * 6 + 3]
                gk = qkgT[:, l * 6 + 3:l * 6 + 4]
                for tg in range(4):
                    tsl = slice(tg * 512, (tg + 1) * 512)
                    for p in range(2):
                        proj_fm(es1, HT, T_HT, W, T_W, p * 128, tg, QB[:, p, tsl], T_Q[tg], "norm", gq)
                        proj_fm(es1, HT, T_HT, W, T_W, 256 + p * 128, tg, KB[:, p, tsl], T_Q[tg], "norm", gk)
                for t in range(NT):
                    pv = proj_tm(HT, T_HT, Wv, T_Wv, 256, t, 6)
                    P.op("scalar", lambda e, t=t, pv=pv: e.copy(out=VB[:, t, :, 0:64], in_=pv[:, 0:256].rearrange("p (h e) -> p h e", h=4)), reads=[T_ps[6]], writes=[T_V[t]])
                for p in range(2):
                    P.op("vector", lambda e, p=p: e.tensor_reduce(out=kmf[:, p, :], in_=KB[:, p, :].rearrange("p (n k) -> p n k", n=8), axis=AX.X, op=ALU.add), reads=T_Q, writes=[T_KM])
                P.op("vector", lambda e: e.tensor_scalar(out=KM[:, :, :], in0=kmf[:, :, :], scalar1=1.0 / 256, scalar2=None, op0=ALU.mult), reads=[T_KM], writes=[T_KM])
                P.barrier()
                P.flush()
            with contextlib.ExitStack() as es2:
                PT = [sbuf(es2, "b_pt%d" % k, [128, 2, 256], BF16) for k in range(2)]
                T_PT = [Trk(), Trk()]
                gate = sbuf(es2, "b_gate", [128, 2, 4, 8], F32)
                top8 = sbuf(es2, "b_top8", [128, 8], F32)
                BM = sbuf(es2, "b_bm", [128, 2, 4, 8], F32)
                acc = sbuf(es2, "b_acc", [128, 2, 4, 65], F32)
                ob = sbuf(es2, "b_ob", [128, 2, 256], BF16)
                rc = sbuf(es2, "b_rc", [128, 2, 4], F32)
                T_g, T_acc, T_ob = Trk(), Trk(), Trk()
                it = 0
                for j in range(8):
                    tgq = j // 2
                    if j > 0:
                        for par in range(2):
                            pg = PSB[6 - par]
                            pgv = pg[:, 0:64].rearrange("p (a h n) -> p a h n", a=2, h=4)
                            P.group("tensor", [(lambda e, a=a, h=h, pgv=pgv: e.matmul(pgv[:, a, h, :], lhsT=QB[(h % 2) * 64:(h % 2) * 64 + 64, h // 2, (2 * j + a) * 128:(2 * j + a + 1) * 128],
                                                                                       rhs=KM[(h % 2) * 64:(h % 2) * 64 + 64, h // 2, :], start=True, stop=True)) for a in range(2) for h in (par, par + 2)],
                                    reads=[T_Q[tgq], T_KM], writes=[T_ps[6 - par]])
                            for h in (par, par + 2):
                                P.op("vector", lambda e, pgv=pgv, h=h: e.tensor_copy(out=gate[:, :, h, :], in_=pgv[:, :, h, :]), reads=[T_ps[6 - par]], writes=[T_g])
                        P.op("vector", lambda e, j=j: e.memset(gate[:, :, :, j:8], NEG), reads=[T_g], writes=[T_g])
                        for a in range(2):
                            for h in range(4):
                                P.op("vector", lambda e, a=a, h=h: e.max(out=top8[:, :], in_=gate[:, a, h, :]), reads=[T_g], writes=[T_g])
                                P.op("vector", lambda e, a=a, h=h: e.tensor_scalar(out=BM[:, a, h, :], in0=gate[:, a, h, :], scalar1=top8[:, 2:3], scalar2=None, op0=ALU.is_ge), reads=[T_g], writes=[T_g])
                    for h in range(4):
                        hp = (h % 2) * 64
                        pp = h // 2
                        k = it % 2
                        it += 1
                        ps_s = PSB[k]
                        psv = ps_s[:, 0:512].rearrange("p (c q) -> p c q", c=2)
                        ks0 = slice((2 * j) * 128, (2 * j + 1) * 128)
                        ks1 = slice((2 * j + 1) * 128, (2 * j + 2) * 128)
                        qs_all = slice((2 * j) * 128, (2 * j + 2) * 128)
                        P.group("tensor", [lambda e, psv=psv, hp=hp, pp=pp, ks0=ks0, qs_all=qs_all: e.matmul(psv[:, 0, :], lhsT=KB[hp:hp + 64, pp, ks0], rhs=QB[hp:hp + 64, pp, qs_all], start=True, stop=True),
                                           lambda e, psv=psv, hp=hp, pp=pp, ks1=ks1, qs_all=qs_all: e.matmul(psv[:, 1, :], lhsT=KB[hp:hp + 64, pp, ks1], rhs=QB[hp:hp + 64, pp, qs_all], start=True, stop=True)],
                                reads=[T_Q[tgq]], writes=[T_ps[k]])
                        P.op("scalar", lambda e, k=k, psv=psv: e.activation(out=PT[k][:, :, :], in_=psv, func=AF.Exp, scale=0.125), reads=[T_ps[k]], writes=[T_PT[k]])
                        P.op("vector", lambda e, k=k: e.tensor_tensor(out=PT[k][:, 0, 0:128], in0=PT[k][:, 0, 0:128], in1=trib[:], op=ALU.mult), reads=[T_PT[k], T_cst], writes=[T_PT[k]])
                        P.op("vector", lambda e, k=k: e.tensor_tensor(out=PT[k][:, 1, 128:256], in0=PT[k][:, 1, 128:256], in1=trib[:], op=ALU.mult), reads=[T_PT[k], T_cst], writes=[T_PT[k]])
                        po = PSB[2 + k]
                        pov = po[:, 0:130].rearrange("p (a e) -> p a e", a=2)
                        P.group("tensor", [lambda e, k=k, pov=pov, h=h: e.matmul(pov[:, 0, :], lhsT=PT[k][:, 0, 0:128], rhs=VB[:, 2 * j, h, :], start=True, stop=True)],
                                reads=[T_PT[k], T_V[2 * j]], writes=[T_ps[2 + k]])
                        P.group("tensor", [lambda e, k=k, pov=pov, h=h: e.matmul(pov[:, 1, :], lhsT=PT[k][:, 0, 128:256], rhs=VB[:, 2 * j, h, :], start=True, stop=False),
                                           lambda e, k=k, pov=pov, h=h: e.matmul(pov[:, 1, :], lhsT=PT[k][:, 1, 128:256], rhs=VB[:, 2 * j + 1, h, :], start=False, stop=True)],
                                reads=[T_PT[k], T_V[2 * j], T_V[2 * j + 1]], writes=[T_ps[2 + k]])
                        P.op("vector", lambda e, pov=pov, h=h: e.tensor_copy(out=acc[:, :, h, :], in_=pov), reads=[T_ps[2 + k]], writes=[T_acc])
                        for n in range(j):
                            k = it % 2
                            it += 1
                            ps_s = PSB[k]
                            psv = ps_s[:, 0:512].rearrange("p (c q) -> p c q", c=2)
                            P.group("tensor", [(lambda e, c=c, psv=psv, hp=hp, pp=pp, n=n, qs_all=qs_all: e.matmul(psv[:, c, :], lhsT=KB[hp:hp + 64, pp, (2 * n + c) * 128:(2 * n + c + 1) * 128], rhs=QB[hp:hp + 64, pp, qs_all], start=True, stop=True)) for c in range(2)],
                                    reads=[T_Q[tgq], T_Q[n // 2]], writes=[T_ps[k]])
                            P.op("scalar", lambda e, k=k, psv=psv: e.activation(out=PT[k][:, :, :], in_=psv, func=AF.Exp, scale=0.125), reads=[T_ps[k]], writes=[T_PT[k]])
                            po = PSB[2 + k]
                            pov = po[:, 0:130].rearrange("p (a e) -> p a e", a=2)
                            for a in range(2):
                                P.group("tensor", [(lambda e, c=c, a=a, k=k, pov=pov, n=n, h=h: e.matmul(pov[:, a, :], lhsT=PT[k][:, c, a * 128:(a + 1) * 128], rhs=VB[:, 2 * n + c, h, :], start=(c == 0), stop=(c == 1))) for c in range(2)],
                                        reads=[T_PT[k], T_V[2 * n], T_V[2 * n + 1]], writes=[T_ps[2 + k]])
                            for a in range(2):
                                P.op("vector", lambda e, a=a, pov=pov, h=h, n=n: e.scalar_tensor_tensor(out=acc[:, a, h, :], in0=pov[:, a, :], scalar=BM[:, a, h, n:n + 1], in1=acc[:, a, h, :], op0=ALU.mult, op1=ALU.add),
                                     reads=[T_ps[2 + k], T_g, T_acc], writes=[T_acc])
                    P.op("vector", lambda e: e.reciprocal(out=rc[:, :, :], in_=acc[:, :, :, 64]), reads=[T_acc], writes=[T_g])
                    for a in range(2):
                        P.op("vector", lambda e, a=a: e.tensor_tensor(out=ob[:, a, :].rearrange("p (h e) -> p h e", h=4), in0=acc[:, a, :, 0:64], in1=rc[:, a, :].unsqueeze(2).to_broadcast([128, 4, 64]), op=ALU.mult),
                             reads=[T_acc, T_g], writes=[T_ob])
                        transpose_out(ob[:, a, :], T_ob, 256, OT, T_OT, 2, 2 * j + a)
                P.barrier()
                P.flush()

    def mixer_C(l, HT, T_HT, OT, T_OT, half):
        with contextlib.ExitStack() as es:
            QC = sbuf(es, "c_q", [128, 2, S], BF16)
            KC = sbuf(es, "c_k", [128, 2, S], BF16)
            VC = sbuf(es, "c_v", [128, NT, 2, 129], BF16)
            T_Q = [Trk() for _ in range(4)]
            T_V = [Trk() for _ in range(NT)]
            with contextlib.ExitStack() as es1:
                W = sbuf(es1, "c_w", [128, 8, 512], BF16)
                Wv = sbuf(es1, "c_wv", [128, 8, 256], BF16)
                T_W, T_Wv = Trk(), Trk()
                proj_tmp(es1)
                load_w_in(W, T_W, l, [(0, OFF["qc"] + half * 256, 256), (256, OFF["kc"] + half * 256, 256)])
                load_w_in(Wv, T_Wv, l, [(0, OFF["vc"] + half * 256, 256)])
                P.op("vector", lambda e: e.memset(VC[:, :, :, 128:129], 1.0), writes=T_V)
                gq = qkgT[:, l * 6 + 4:l * 6 + 5]
                gk = qkgT[:, l * 6 + 5:l * 6 + 6]
                for tg in range(4):
                    tsl = slice(tg * 512, (tg + 1) * 512)
                    for p in range(2):
                        proj_fm(es1, HT, T_HT, W, T_W, p * 128, tg, QC[:, p, tsl], T_Q[tg], "norm", gq)
                        proj_fm(es1, HT, T_HT, W, T_W, 256 + p * 128, tg, KC[:, p, tsl], T_Q[tg], "norm", gk)
                for t in range(NT):
                    pv = proj_tm(HT, T_HT, Wv, T_Wv, 256, t, 6)
                    P.op("scalar", lambda e, t=t, pv=pv: e.copy(out=VC[:, t, :, 0:128], in_=pv[:, 0:256].rearrange("p (h e) -> p h e", h=2)), reads=[T_ps[6]], writes=[T_V[t]])
                P.barrier()
                P.flush()
            with contextlib.ExitStack() as es2:
                PT = sbuf(es2, "c_pt", [128, NT, 512], BF16)
                T_PT = Trk()
                t0 = sbuf(es2, "c_t0", [128, 4, 128], F32)
                o32 = sbuf(es2, "c_o32", [128, 4, 128], F32)
                oc = sbuf(es2, "c_oc", [128, 4, 256], BF16)
                sq = sbuf(es2, "c_sq", [128, 128], F32)
                st = sbuf(es2, "c_st", [128, 16], F32)
                T_t0, T_o32, T_oc, T_st = Trk(), Trk(), Trk(), Trk()
                it = 0
                for G in range(4):
                    for hh in range(2):
                        for c in range(2):
                            cp = c * 64
                            nkt = 4 * G + 4
                            for kt in range(nkt):
                                k = it % 2
                                it += 1
                                ps_s = PSB[k]
                                qs0 = max(kt - 4 * G, 0)
                                q0 = (4 * G + qs0) * 128
                                nq = (4 - qs0) * 128
                                P.group("tensor", [lambda e, ps_s=ps_s, cp=cp, hh=hh, kt=kt, q0=q0, nq=nq: e.matmul(ps_s[:, 0:nq], lhsT=KC[cp:cp + 64, hh, kt * 128:(kt + 1) * 128], rhs=QC[cp:cp + 64, hh, q0:q0 + nq], start=True, stop=True)],
                                        reads=[T_Q[G], T_Q[kt // 4]], writes=[T_ps[k]])
                                P.op("scalar", lambda e, ps_s=ps_s, kt=kt, qs0=qs0, nq=nq: e.activation(out=PT[:, kt, qs0 * 128:qs0 * 128 + nq], in_=ps_s[:, 0:nq], func=AF.Exp, scale=0.125), reads=[T_ps[k]], writes=[T_PT])
                                if kt >= 4 * G:
                                    P.op("vector", lambda e, kt=kt, qs0=qs0: e.tensor_tensor(out=PT[:, kt, qs0 * 128:(qs0 + 1) * 128], in0=PT[:, kt, qs0 * 128:(qs0 + 1) * 128], in1=trib[:], op=ALU.mult),
                                         reads=[T_PT, T_cst], writes=[T_PT])
                            for qs in range(4):
                                pb = 2 + (it % 2)
                                it += 1
                                po = PSB[pb]
                                nk = 4 * G + qs + 1
                                P.group("tensor", [(lambda e, kt=kt, qs=qs, po=po, hh=hh, nk=nk: e.matmul(po[:, 0:129], lhsT=PT[:, kt, qs * 128:(qs + 1) * 128], rhs=VC[:, kt, hh, :], start=(kt == 0), stop=(kt == nk - 1))) for kt in range(nk)],
                                        reads=[T_PT] + T_V[0:nk], writes=[T_ps[pb]])
                                P.op("vector", lambda e, po=po, qs=qs, c=c: e.reciprocal(out=st[:, qs * 2 + c:qs * 2 + c + 1], in_=po[:, 128:129]), reads=[T_ps[pb]], writes=[T_st])
                                if c == 0:
                                    P.op("vector", lambda e, po=po, qs=qs: e.tensor_scalar(out=t0[:, qs, :], in0=po[:, 0:128], scalar1=st[:, qs * 2:qs * 2 + 1], scalar2=None, op0=ALU.mult),
                                         reads=[T_ps[pb], T_st], writes=[T_t0])
                                else:
                                    P.op("vector", lambda e, qs=qs: e.tensor_tensor(out=st[:, 8 + qs:9 + qs], in0=st[:, qs * 2 + 1:qs * 2 + 2], in1=lamv[:, l:l + 1], op=ALU.mult), reads=[T_st, T_lam], writes=[T_st])
                                    P.op("vector", lambda e, po=po, qs=qs: e.scalar_tensor_tensor(out=o32[:, qs, :], in0=po[:, 0:128], scalar=st[:, 8 + qs:9 + qs], in1=t0[:, qs, :], op0=ALU.mult, op1=ALU.add),
                                         reads=[T_ps[pb], T_st, T_t0], writes=[T_o32])
                                    P.op("vector", lambda e, qs=qs: e.tensor_tensor(out=sq[:], in0=o32[:, qs, :], in1=o32[:, qs, :], op=ALU.mult), reads=[T_o32], writes=[T_st])
                                    P.op("vector", lambda e, qs=qs: e.reduce_sum(out=st[:, 12:13], in_=sq[:], axis=AX.X), reads=[T_st], writes=[T_st])
                                    P.op("scalar", lambda e: e.activation(out=st[:, 13:14], in_=st[:, 12:13], func=AF.Sqrt, scale=1.0 / 128, bias=EPS), reads=[T_st], writes=[T_st])
                                    P.op("vector", lambda e: e.reciprocal(out=st[:, 14:15], in_=st[:, 13:14]), reads=[T_st], writes=[T_st])
                                    P.op("vector", lambda e, qs=qs: e.tensor_scalar(out=o32[:, qs, :], in0=o32[:, qs, :], scalar1=st[:, 14:15], scalar2=float(1.0 - lam_init(l)), op0=ALU.mult, op1=ALU.mult), reads=[T_st, T_o32], writes=[T_o32])
                                    P.op("vector", lambda e, qs=qs, hh=hh: e.tensor_tensor(out=oc[:, qs, hh * 128:(hh + 1) * 128], in0=o32[:, qs, :], in1=subg[:, l * 128:(l + 1) * 128], op=ALU.mult), reads=[T_o32, T_cst], writes=[T_oc])
                    for qs in range(4):
                        transpose_out(oc[:, qs, :], T_oc, 256, OT, T_OT, 4 + 2 * half, 4 * G + qs)
                P.barrier()
                P.flush()

    def mixer_out(l, HT, T_HT, OT, T_OT):
        with contextlib.ExitStack() as es:
            MG = sbuf(es, "o_mg", [128, 8, S], BF16)
            T_MG = [Trk() for _ in range(4)]
            es_a = contextlib.ExitStack()
            WBR = [sbuf(es_a, "o_wbr%d" % k, [128, 8, 128], BF16) for k in range(2)]
            WGT = [sbuf(es_a, "o_wgt%d" % k, [128, 8, 384], BF16) for k in range(2)]
            sg = [sbuf(es_a, "o_sg%d" % k, [128, 512], F32) for k in range(2)]
            mg32 = [sbuf(es_a, "o_m32%d" % k, [128, 512], F32) for k in range(2)]
            T_W = [Trk(), Trk()]
            T_sg = [Trk(), Trk()]
            T_m32 = [Trk(), Trk()]
            wbv = wbr_d[l].rearrange("(c p) f -> p c f", p=128)
            wiv = win_d[l].rearrange("(c p) f -> p c f", p=128)
            feat = [(0, 2), (2, 2), (4, 4)]
            it = 0
            for dc in range(8):
                wb = dc % 2
                P.dma("gpsimd", WBR[wb][:, :, :], wbv[:, :, dc * 128:(dc + 1) * 128], writes=[T_W[wb]])
                for br, nm in enumerate(("ga", "gb", "gc")):
                    P.dma("gpsimd", WGT[wb][:, :, br * 128:(br + 1) * 128], wiv[:, :, OFF[nm] + dc * 128:OFF[nm] + (dc + 1) * 128], writes=[T_W[wb]])
                for tg in range(4):
                    tsl = slice(tg * 512, (tg + 1) * 512)
                    mk = it % 2
                    it += 1
                    for br in range(3):
                        k = (it + br) % 2
                        pgt = PSB[k]
                        py = PSB[2 + k]
                        c0, ncn = feat[br]
                        P.group("tensor", [(lambda e, c=c, br=br, pgt=pgt, wb=wb: e.matmul(pgt[:, :], lhsT=WGT[wb][:, c, br * 128:(br + 1) * 128], rhs=HT[:, c, tsl], start=(c == 0), stop=(c == 7))) for c in range(8)],
                                reads=[T_W[wb]] + T_HT[tg * 4:tg * 4 + 4], writes=[T_ps[k]])
                        P.group("tensor", [(lambda e, c=c, c0=c0, ncn=ncn, py=py, wb=wb: e.matmul(py[:, :], lhsT=WBR[wb][:, c0 + c, :], rhs=OT[:, c0 + c, tsl], start=(c == 0), stop=(c == ncn - 1))) for c in range(ncn)],
                                reads=[T_W[wb]] + T_OT[tg * 4:tg * 4 + 4], writes=[T_ps[2 + k]])
                        P.op("scalar", lambda e, k=k, pgt=pgt: e.activation(out=sg[k][:], in_=pgt[:, :], func=AF.Sigmoid), reads=[T_ps[k]], writes=[T_sg[k]])
                        if br == 0:
                            P.op("vector", lambda e, k=k, py=py, mk=mk: e.tensor_tensor(out=mg32[mk][:], in0=py[:, :], in1=sg[k][:], op=ALU.mult), reads=[T_ps[2 + k], T_sg[k]], writes=[T_m32[mk]])
                        else:
                            P.op("vector", lambda e, k=k, py=py: e.tensor_tensor(out=sg[k][:], in0=py[:, :], in1=sg[k][:], op=ALU.mult), reads=[T_ps[2 + k], T_sg[k]], writes=[T_sg[k]])
                            if br == 1:
                                P.op("gpsimd", lambda e, k=k, mk=mk: e.tensor_tensor(out=mg32[mk][:], in0=mg32[mk][:], in1=sg[k][:], op=ALU.add), reads=[T_sg[k], T_m32[mk]], writes=[T_m32[mk]])
                            else:
                                P.op("gpsimd", lambda e, k=k, mk=mk, dc=dc: e.tensor_tensor(out=MG[:, dc, tsl], in0=mg32[mk][:], in1=sg[k][:], op=ALU.add), reads=[T_sg[k], T_m32[mk]], writes=[T_MG[tg]])
            P.barrier()
            P.flush()
            es_a.close()
            WO = sbuf(es, "o_wo", [128, 8, D], BF16)
            T_WO = Trk()
            wov = wout_d[l].rearrange("(c p) f -> p c f", p=128)
            for hf in range(2):
                P.dma("gpsimd", WO[:, :, hf * 512:(hf + 1) * 512], wov[:, :, hf * 512:(hf + 1) * 512], writes=[T_WO])
            for t in range(NT):
                for hf in range(2):
                    pb = 4 + (t * 2 + hf) % 2
                    pd = PSB[pb]
                    P.group("tensor", [(lambda e, c=c, t=t, hf=hf, pd=pd: e.matmul(pd[:, :], lhsT=MG[:, c, t * 128:(t + 1) * 128], rhs=WO[:, c, hf * 512:(hf + 1) * 512], start=(c == 0), stop=(c == 7))) for c in range(8)],
                            reads=[T_MG[t // 4], T_WO], writes=[T_ps[pb]])
                    P.op("vector", lambda e, t=t, hf=hf, pd=pd: e.tensor_tensor(out=X[:, t, hf * 512:(hf + 1) * 512], in0=pd[:, :], in1=X[:, t, hf * 512:(hf + 1) * 512], op=ALU.add),
                         reads=[T_ps[pb], T_X[t]], writes=[T_X[t]])
            P.barrier()
            P.flush()

    def mixer(l, b):
        with contextlib.ExitStack() as es:
            HT = sbuf(es, "m_HT", [128, 8, S], BF16)
            OT = sbuf(es, "m_OT", [128, 8, S], BF16)
            T_HT = [Trk() for _ in range(NT)]
            T_OT = [Trk() for _ in range(NT)]
            with contextlib.ExitStack() as esn:
                norm_T(esn, l, 1, HT, T_HT)
                P.barrier()
                P.flush()
            if stage >= 2:
                mixer_A(l, HT, T_HT, OT, T_OT)
            if stage >= 3:
                mixer_B(l, HT, T_HT, OT, T_OT)
            if stage >= 4:
                mixer_C(l, HT, T_HT, OT, T_OT, 0)
                mixer_C(l, HT, T_HT, OT, T_OT, 1)
            if dbg and l == layers[0] and b == 0:
                nch = {2: 2, 3: 4}.get(stage, 8)
                P.dma("sync", dbg_ot[:, 0:nch, :], OT[:, 0:nch, :], reads=T_OT, writes=[T_out])
            if stage >= 5:
                mixer_out(l, HT, T_HT, OT, T_OT)
            P.barrier()
            P.flush()

    for b in range(n_seq):
        for t in range(NT):
            P.dma("sync", X[:, t, :], x_d[b, t * 128:(t + 1) * 128, :], writes=[T_X[t]])
        rope_tables(b)
        for l in layers:
            if stage >= 1:
                ffn(l, 0)
            if stage >= 2:
                mixer(l, b)
            if stage >= 6:
                ffn(l, 1)
        for t in range(NT):
            P.dma("sync", out_d[b, t * 128:(t + 1) * 128, :], X[:, t, :], reads=[T_X[t]], writes=[Trk()])
    P.barrier()
    P.flush()
    es0.close()
    P.close()
    build.nins = P.nins
    return nc


def prep_shared(inputs):
    norm_g = np.asarray(inputs["norm_g"], np.float32)
    qk = np.asarray(inputs["qk_norm_g"], np.float32)
    ngT = np.ascontiguousarray(norm_g.reshape(2, 3, 8, 128).transpose(3, 0, 1, 2).reshape(128, 48))
    qkT = qk.reshape(12, 64).T
    qkgT = np.ascontiguousarray(np.concatenate([qkT, qkT], axis=0))
    return {
        "w_in": np.ascontiguousarray(inputs["w_in"], np.float32),
        "w_branch": np.ascontiguousarray(inputs["w_branch"], np.float32),
        "w_out": np.ascontiguousarray(inputs["w_out"], np.float32),
        "ffn_w_gate": np.ascontiguousarray(inputs["ffn_w_gate"], np.float32),
        "ffn_w_up": np.ascontiguousarray(inputs["ffn_w_up"], np.float32),
        "ffn_w_down": np.ascontiguousarray(inputs["ffn_w_down"], np.float32),
        "cst": make_consts(),
        "ngT": ngT,
        "qkgT": qkgT,
        "lamp": np.ascontiguousarray(np.asarray(inputs["lambda_params"], np.float32).reshape(512)),
        "subg": np.ascontiguousarray(np.asarray(inputs["diff_subln_g"], np.float32).reshape(256)),
    }


def kernel(**inputs):
    x = np.asarray(inputs["x"], np.float32)
    pos = np.asarray(inputs["positions"], np.int32)
    shared = prep_shared(inputs)
    nc = build(n_seq=2, layers=(0, 1))
    in_maps = []
    for c in range(NCORES):
        m = dict(shared)
        m["x"] = np.ascontiguousarray(x[2 * c:2 * c + 2])
        m["pos"] = np.ascontiguousarray(pos[2 * c:2 * c + 2])
        in_maps.append(m)
    res = run_bass_kernel_spmd(nc, in_maps, core_ids=list(range(NCORES)))
    out = np.concatenate([np.asarray(r["out"]) for r in res.results], axis=0)
    return out.astype(np.float32)
```

```python
import contextlib
import math
import numpy as np
import concourse.bass as bass
import concourse.mybir as mybir
from concourse.bass_utils import run_bass_kernel_spmd

F32 = mybir.dt.float32
BF16 = mybir.dt.bfloat16
I32 = mybir.dt.int32
AF = mybir.ActivationFunctionType
ALU = mybir.AluOpType
AX = mybir.AxisListType

S = 2048
D = 1024
NT = 16
DFF = 2816
EPS = 1e-6
NCORES = 8
OFF = dict(qa=0, ka=256, va=320, qi=384, ki=896, wi=960, qb=968, kb=1224, vb=1480,
           qc=1736, kc=2248, vc=2760, ga=3272, gb=4296, gc=5320)
NBIS = 16
C_ID, C_BD, C_RM, C_TRI, C_INVF, C_POW = 0, 128, 256, 384, 512, 513
NCST = C_POW + NBIS
NEG = -1.0e30
import os
SUB = int(os.environ.get('SUB', '9'))
PI = math.pi


class Trk:
    __slots__ = ("name", "w", "r")

    def __init__(self, name=""):
        self.name = name
        self.w = None
        self.r = []


class _Rec:
    def __init__(self):
        self.calls = []

    def __getattr__(self, name):
        def f(*a, **k):
            self.calls.append((name, a, k))
            return self
        return f


class Prog:
    ENG = ("tensor", "vector", "scalar", "gpsimd", "sync")

    def __init__(self, nc, n_dma_sems=24):
        self.nc = nc
        self.stack = []
        self.ops = {e: [] for e in self.ENG}
        self.sems = {}
        self.cnt = {}
        self.wm = {e: {} for e in self.ENG}
        for e in self.ENG:
            self._newsem(e)
        self.dma_keys = {}
        self.dma_rr = {}
        for q in ("sync", "gpsimd", "scalar"):
            self.dma_keys[q] = []
            self.dma_rr[q] = 0
            for i in range(n_dma_sems if q != "scalar" else 4):
                k = "dma_%s%d" % (q, i)
                self._newsem(k)
                self.dma_keys[q].append(k)
        self.nins = 0
        self.fill_reg = None

    def _newsem(self, key):
        cm = self.nc.semaphore(key)
        h = cm.__enter__()
        self.stack.append(cm)
        self.sems[key] = h
        self.cnt[key] = 0

    def _waits(self, eng, reads, writes):
        need = {}
        for t in reads:
            if t.w is not None:
                k, v = t.w
                if need.get(k, 0) < v:
                    need[k] = v
        for t in writes:
            if t.w is not None:
                k, v = t.w
                if need.get(k, 0) < v:
                    need[k] = v
            for (k, v) in t.r:
                if need.get(k, 0) < v:
                    need[k] = v
        out = []
        wm = self.wm[eng]
        for k, v in need.items():
            if wm.get(k, 0) < v:
                wm[k] = v
                out.append((k, v))
        return out

    def _mark(self, dep, reads, writes):
        for t in reads:
            t.r.append(dep)
        for t in writes:
            t.w = dep
            t.r = []

    def op(self, eng, fn, reads=(), writes=()):
        return self.group(eng, [fn], reads, writes)

    def group(self, eng, fns, reads=(), writes=()):
        waits = self._waits(eng, reads, writes)
        self.cnt[eng] += 1
        dep = (eng, self.cnt[eng])
        self._mark(dep, reads, writes)
        sem = self.sems[eng]
        sems = self.sems
        self.nins += len(fns)
        rec = _Rec()
        for f in fns:
            f(rec)
        calls = rec.calls

        def run(e, calls=calls, waits=waits, sem=sem):
            for (k, val) in waits:
                e.wait_ge(sems[k], val)
            last = None
            for (name, a, kw) in calls:
                if name == "affine_select":
                    kw = dict(kw)
                    if self.fill_reg is None:
                        self.fill_reg = e.to_reg(kw["fill"])
                    kw["fill"] = self.fill_reg
                last = getattr(e, name)(*a, **kw)
            last.then_inc(sem, 1)
        self.ops[eng].append(run)
        return dep

    def dma(self, q, out, in_, reads=(), writes=(), **kw):
        waits = self._waits(q, reads, writes)
        k = self.dma_keys[q][self.dma_rr[q] % len(self.dma_keys[q])]
        self.dma_rr[q] += 1
        prev = self.cnt[k]
        if prev > 0 and self.wm[q].get(k, 0) < prev:
            self.wm[q][k] = prev
            waits.append((k, prev))
        self.cnt[k] += 16
        dep = (k, self.cnt[k])
        self._mark(dep, reads, writes)
        sems = self.sems
        sem = sems[k]
        self.nins += 1

        def run(e, waits=waits, sem=sem, out=out, in_=in_, kw=kw):
            for (kk, val) in waits:
                e.wait_ge(sems[kk], val)
            e.dma_start(out=out, in_=in_, **kw).then_inc(sem, 16)
        self.ops[q].append(run)
        return dep

    def barrier(self):
        sems = self.sems
        snap = [(k, v) for k, v in self.cnt.items() if v > 0]
        for eng in self.ENG:
            waits = []
            for (k, v) in snap:
                if k == eng:
                    continue
                if self.wm[eng].get(k, 0) < v:
                    self.wm[eng][k] = v
                    waits.append((k, v))

            def run(e, waits=waits):
                for (k, val) in waits:
                    e.wait_ge(sems[k], val)
            self.ops[eng].append(run)

    def flush(self):
        nc = self.nc
        ops = self.ops
        with nc.Block() as block:
            @block.tensor
            def _(e):
                for f in ops["tensor"]:
                    f(e)

            @block.vector
            def _(e):
                for f in ops["vector"]:
                    f(e)

            @block.scalar
            def _(e):
                for f in ops["scalar"]:
                    f(e)

            @block.gpsimd
            def _(e):
                for f in ops["gpsimd"]:
                    f(e)

            @block.sync
            def _(e):
                for f in ops["sync"]:
                    f(e)
        self.ops = {e: [] for e in self.ENG}

    def close(self):
        for cm in reversed(self.stack):
            cm.__exit__(None, None, None)


def make_consts():
    c = np.zeros((128, NCST), np.float32)
    c[:, C_ID:C_ID + 128] = np.eye(128, dtype=np.float32)
    for p in range(128):
        for f in range(128):
            if p // 64 == f // 64:
                c[p, C_BD + f] = 1.0
    for f in range(128):
        r = f % 64
        if r < 8:
            c[f + 8, C_RM + f] = -1.0
        elif r < 16:
            c[f - 8, C_RM + f] = 1.0
    for k in range(128):
        c[k, C_TRI + k:C_TRI + 128] = 1.0
    inv = np.power(np.float32(500000.0), -np.arange(0, 16, 2, dtype=np.float32) / np.float32(16.0)).astype(np.float32)
    for p in range(128):
        r = p % 64
        c[p, C_INVF] = inv[r % 8] if r < 16 else 0.0
    for i in range(NBIS):
        c[:, C_POW + i] = 2.0 ** (-(i + 1))
    return c


def build(n_seq=2, layers=(0, 1), stage=99, dbg=False):
    nc = bass.Bass("TRN2", target_bir_lowering=False)
    dt = nc.dram_tensor
    x_d = dt("x", [n_seq, S, D], F32, kind="ExternalInput").ap()
    pos_d = dt("pos", [n_seq, S], I32, kind="ExternalInput").ap()
    win_d = dt("w_in", [2, D, 6344], F32, kind="ExternalInput").ap()
    wbr_d = dt("w_branch", [2, D, D], F32, kind="ExternalInput").ap()
    wout_d = dt("w_out", [2, D, D], F32, kind="ExternalInput").ap()
    wg_d = dt("ffn_w_gate", [2, 2, D, DFF], F32, kind="ExternalInput").ap()
    wu_d = dt("ffn_w_up", [2, 2, D, DFF], F32, kind="ExternalInput").ap()
    wd_d = dt("ffn_w_down", [2, 2, DFF, D], F32, kind="ExternalInput").ap()
    cst_d = dt("cst", [128, NCST], F32, kind="ExternalInput").ap()
    ngT_d = dt("ngT", [128, 48], F32, kind="ExternalInput").ap()
    qkgT_d = dt("qkgT", [128, 12], F32, kind="ExternalInput").ap()
    lam_d = dt("lamp", [512], F32, kind="ExternalInput").ap()
    sub_d = dt("subg", [256], F32, kind="ExternalInput").ap()
    out_d = dt("out", [n_seq, S, D], F32, kind="ExternalOutput").ap()
    if dbg:
        dbg_ot = dt("dbg_ot", [128, 8, S], BF16, kind="ExternalOutput").ap()

    P = Prog(nc)
    es0 = contextlib.ExitStack()

    uid = [0]

    def sbuf(es, name, shape, dtype):
        uid[0] += 1
        return es.enter_context(nc.sbuf_tensor("s%d_%s" % (uid[0], name), shape, dtype))

    X = sbuf(es0, "X", [128, NT, D], F32)
    tab_d = dt("tabs", [2, 128, S], BF16, kind="ExternalOutput").ap()
    cstf = sbuf(es0, "cstf", [128, NCST], F32)
    identb = sbuf(es0, "identb", [128, 128], BF16)
    BDb = sbuf(es0, "BDb", [128, 128], BF16)
    RMb = sbuf(es0, "RMb", [128, 128], BF16)
    trib = sbuf(es0, "trib", [128, 128], BF16)
    onesb = sbuf(es0, "onesb", [128, 128], BF16)
    ngT = sbuf(es0, "ngT", [128, 48], F32)
    qkgT = sbuf(es0, "qkgT", [128, 12], F32)
    subg = sbuf(es0, "subg", [128, 256], F32)
    lamv = sbuf(es0, "lamv", [128, 8], F32)
    rs_x = sbuf(es0, "rs_x", [128, 2 * NT], F32)
    PSB = [es0.enter_context(nc.psum_tensor("psb%d" % i, [128, 512], F32)) for i in range(7)]
    PSH = es0.enter_context(nc.psum_tensor("psh", [128, 1024], BF16))
    T_ps = [Trk("ps%d" % i) for i in range(7)]
    T_psh = Trk("psh")
    T_X = [Trk("X%d" % i) for i in range(NT)]
    T_cst = Trk("cst")
    T_tab = Trk("tab")
    T_out = Trk("out")
    T_lam = Trk("lam")

    def lam_init(l):
        return 0.8 - 0.6 * math.exp(-0.3 * l)

    P.dma("sync", cstf[:], cst_d, writes=[T_cst])
    P.dma("gpsimd", identb[:], cst_d[:, C_ID:C_ID + 128], writes=[T_cst])
    P.dma("gpsimd", BDb[:], cst_d[:, C_BD:C_BD + 128], writes=[T_cst])
    P.dma("gpsimd", RMb[:], cst_d[:, C_RM:C_RM + 128], writes=[T_cst])
    P.dma("gpsimd", trib[:], cst_d[:, C_TRI:C_TRI + 128], writes=[T_cst])
    P.dma("sync", ngT[:], ngT_d, writes=[T_cst])
    P.dma("sync", qkgT[:], qkgT_d, writes=[T_cst])
    P.dma("sync", subg[:], sub_d.partition_broadcast(128), writes=[T_cst])
    P.op("vector", lambda e: e.memset(onesb[:], 1.0), writes=[T_cst])
    with contextlib.ExitStack() as es:
        lamp = sbuf(es, "lamp", [128, 512], F32)
        tmp = sbuf(es, "lamtmp", [128, 512], F32)
        sums = sbuf(es, "lamsum", [128, 8], F32)
        T_t = Trk()
        P.dma("sync", lamp[:], lam_d.partition_broadcast(128), writes=[T_lam])
        for l in range(2):
            for j in range(2):
                a0 = l * 256 + (2 * j) * 64
                P.op("vector", lambda e, a0=a0: e.tensor_tensor(out=tmp[:, a0:a0 + 64], in0=lamp[:, a0:a0 + 64], in1=lamp[:, a0 + 64:a0 + 128], op=ALU.mult),
                     reads=[T_lam], writes=[T_t])
                P.op("vector", lambda e, a0=a0, l=l, j=j: e.reduce_sum(out=sums[:, 2 * l + j:2 * l + j + 1], in_=tmp[:, a0:a0 + 64], axis=AX.X),
                     reads=[T_t], writes=[T_t])
        P.op("scalar", lambda e: e.activation(out=sums[:, 4:8], in_=sums[:, 0:4], func=AF.Exp), reads=[T_t], writes=[T_t])
        for l in range(2):
            P.op("vector", lambda e, l=l: e.scalar_tensor_tensor(out=lamv[:, l:l + 1], in0=sums[:, 5 + 2 * l:6 + 2 * l], scalar=-lam_init(l),
                                                               in1=sums[:, 4 + 2 * l:5 + 2 * l], op0=ALU.add, op1=ALU.subtract),
                 reads=[T_t], writes=[T_lam])
        P.barrier()
        P.flush()

    def rope_tables(b):
        with contextlib.ExitStack() as es:
            CT = sbuf(es, "CT", [128, S], BF16)
            STb = sbuf(es, "ST", [128, S], BF16)
            posi = sbuf(es, "posi", [128, S], I32)
            a = sbuf(es, "ta", [128, S], F32)
            r = sbuf(es, "tr", [128, S], F32)
            ki = sbuf(es, "tki", [128, S], I32)
            kf = sbuf(es, "tkf", [128, S], F32)
            T = Trk()
            P.dma("sync", posi[:], pos_d[b].partition_broadcast(128), writes=[T])
            V = lambda fn: P.op("vector", fn, reads=[T, T_cst], writes=[T, T_tab])
            V(lambda e: e.tensor_copy(out=a[:], in_=posi[:]))
            V(lambda e: e.tensor_scalar(out=a[:], in0=a[:], scalar1=cstf[:, C_INVF:C_INVF + 1], scalar2=None, op0=ALU.mult))
            V(lambda e: e.tensor_scalar(out=r[:], in0=a[:], scalar1=float(1.0 / (2 * PI)), scalar2=None, op0=ALU.mult))
            V(lambda e: e.tensor_copy(out=ki[:], in_=r[:]))
            V(lambda e: e.tensor_copy(out=kf[:], in_=ki[:]))
            V(lambda e: e.scalar_tensor_tensor(out=r[:], in0=kf[:], scalar=-6.28125, in1=a[:], op0=ALU.mult, op1=ALU.add))
            V(lambda e: e.scalar_tensor_tensor(out=r[:], in0=kf[:], scalar=-(2 * PI - 6.28125), in1=r[:], op0=ALU.mult, op1=ALU.add))

            def wrap(t):
                V(lambda e: e.tensor_scalar(out=kf[:], in0=t[:], scalar1=PI, scalar2=-2 * PI, op0=ALU.is_gt, op1=ALU.mult))
                V(lambda e: e.tensor_tensor(out=t[:], in0=t[:], in1=kf[:], op=ALU.add))
                V(lambda e: e.tensor_scalar(out=kf[:], in0=t[:], scalar1=-PI, scalar2=2 * PI, op0=ALU.is_lt, op1=ALU.mult))
                V(lambda e: e.tensor_tensor(out=t[:], in0=t[:], in1=kf[:], op=ALU.add))
                V(lambda e: e.tensor_scalar(out=t[:], in0=t[:], scalar1=3.1415925, scalar2=-3.1415925, op0=ALU.min, op1=ALU.max))
            wrap(r)
            P.op("scalar", lambda e: e.activation(out=STb[:], in_=r[:], func=AF.Sin), reads=[T], writes=[T_tab, T])
            V(lambda e: e.tensor_scalar(out=a[:], in0=r[:], scalar1=float(PI / 2), scalar2=None, op0=ALU.add))
            wrap(a)
            P.op("scalar", lambda e: e.activation(out=CT[:], in_=a[:], func=AF.Sin), reads=[T], writes=[T_tab, T])
            P.dma("sync", tab_d[0], CT[:], reads=[T_tab], writes=[Trk()])
            P.dma("sync", tab_d[1], STb[:], reads=[T_tab], writes=[Trk()])
            P.barrier()
            P.flush()

    def norm_T(es, l, i, HT, T_HT):
        xsq = sbuf(es, "n_xsq", [128, D], BF16)
        xn = [sbuf(es, "n_xn%d" % k, [128, D], BF16) for k in range(2)]
        T_sq = Trk()
        T_xn = [Trk(), Trk()]
        T_rs = Trk()
        for t in range(NT):
            P.op("scalar", lambda e, t=t: e.activation(out=xsq[:], in_=X[:, t, :], func=AF.Square, accum_out=rs_x[:, t:t + 1]),
                 reads=[T_X[t]], writes=[T_sq, T_rs])
        P.op("scalar", lambda e: e.activation(out=rs_x[:, NT:2 * NT], in_=rs_x[:, 0:NT], func=AF.Sqrt, scale=1.0 / D, bias=EPS),
             reads=[T_rs], writes=[T_rs])
        P.op("vector", lambda e: e.reciprocal(out=rs_x[:, 0:NT], in_=rs_x[:, NT:2 * NT]), reads=[T_rs], writes=[T_rs])
        g0 = (l * 3 + i) * 8
        psT = PSH[:, :].rearrange("p (c t) -> p c t", c=8)
        for t in range(NT):
            k = t % 2
            P.op("vector", lambda e, t=t, k=k: e.tensor_scalar(out=xn[k][:], in0=X[:, t, :], scalar1=rs_x[:, t:t + 1], scalar2=None, op0=ALU.mult),
                 reads=[T_X[t], T_rs], writes=[T_xn[k]])
            P.group("tensor", [(lambda e, c=c, k=k: e.transpose(out=psT[:, c, :], in_=xn[k][:, c * 128:(c + 1) * 128], identity=identb[:])) for c in range(8)],
                    reads=[T_xn[k], T_cst], writes=[T_psh])
            P.op("vector", lambda e, t=t: e.tensor_tensor(out=HT[:, :, t * 128:(t + 1) * 128], in0=psT,
                                                         in1=ngT[:, g0:g0 + 8].unsqueeze(2).to_broadcast([128, 8, 128]), op=ALU.mult),
                 reads=[T_psh, T_cst], writes=[T_HT[t]])

    def ffn(l, i):
        with contextlib.ExitStack() as es:
            HT = sbuf(es, "f_HT", [128, 8, S], BF16)
            T_HT = [Trk() for _ in range(NT)]
            norm_T(es, l, i, HT, T_HT)
            WG = [sbuf(es, "f_wg%d" % k, [128, 8, 512], BF16) for k in range(2)]
            WU = [sbuf(es, "f_wu%d" % k, [128, 8, 512], BF16) for k in range(2)]
            WD = [sbuf(es, "f_wd%d" % k, [128, 4, D], BF16) for k in range(2)]
            AT = [sbuf(es, "f_at%d" % k, [128, 4, 512], BF16) for k in range(2)]
            SG = [sbuf(es, "f_sg%d" % k, [128, 512], F32) for k in range(2)]
            T_WG, T_WU, T_WD = [Trk(), Trk()], [Trk(), Trk()], [Trk(), Trk()]
            T_AT = [Trk(), Trk()]
            T_SG = [Trk(), Trk()]
            wgv = wg_d[l, i].rearrange("(c p) f -> p c f", p=128)
            wuv = wu_d[l, i].rearrange("(c p) f -> p c f", p=128)
            groups = [(f0, min(512, DFF - f0)) for f0 in range(0, DFF, 512)]
            itf = [0]

            def load(gi):
                f0, fw = groups[gi]
                wb = gi % 2
                nfb = fw // 128
                P.dma("gpsimd", WG[wb][:, :, 0:fw], wgv[:, :, f0:f0 + fw], writes=[T_WG[wb]])
                P.dma("gpsimd", WU[wb][:, :, 0:fw], wuv[:, :, f0:f0 + fw], writes=[T_WU[wb]])
                P.dma("gpsimd", WD[wb][:, 0:nfb, :], wd_d[l, i][f0:f0 + fw, :].rearrange("(c p) d -> p c d", p=128), writes=[T_WD[wb]])

            def gate_up(idx, gi, tg):
                f0, fw = groups[gi]
                wb = gi % 2
                ab = idx % 2
                for fb in range(fw // 128):
                    k = itf[0] % 2
                    itf[0] += 1
                    pg, pu = PSB[2 * k], PSB[2 * k + 1]
                    P.group("tensor", [(lambda e, c=c: e.matmul(pg[:, :], lhsT=WG[wb][:, c, fb * 128:(fb + 1) * 128], rhs=HT[:, c, tg * 512:(tg + 1) * 512], start=(c == 0), stop=(c == 7))) for c in range(8)],
                            reads=[T_WG[wb]] + T_HT[tg * 4:tg * 4 + 4], writes=[T_ps[2 * k]])
                    P.group("tensor", [(lambda e, c=c: e.matmul(pu[:, :], lhsT=WU[wb][:, c, fb * 128:(fb + 1) * 128], rhs=HT[:, c, tg * 512:(tg + 1) * 512], start=(c == 0), stop=(c == 7))) for c in range(8)],
                            reads=[T_WU[wb]] + T_HT[tg * 4:tg * 4 + 4], writes=[T_ps[2 * k + 1]])
                    P.op("scalar", lambda e: e.activation(out=SG[k][:], in_=pg[:, :], func=AF.Silu), reads=[T_ps[2 * k]], writes=[T_SG[k]])
                    P.op("vector", lambda e: e.tensor_tensor(out=AT[ab][:, fb, :], in0=pu[:, :], in1=SG[k][:], op=ALU.mult),
                         reads=[T_ps[2 * k + 1], T_SG[k]], writes=[T_AT[ab]])

            def down(idx, gi, tg):
                f0, fw = groups[gi]
                wb = gi % 2
                ab = idx % 2
                nfb = fw // 128
                for tt in range(4):
                    t = tg * 4 + tt
                    for hf in range(2):
                        pb = 4 + (t * 2 + hf) % 2
                        pd = PSB[pb]
                        P.group("tensor", [(lambda e, fb=fb: e.matmul(pd[:, :], lhsT=AT[ab][:, fb, tt * 128:(tt + 1) * 128], rhs=WD[wb][:, fb, hf * 512:(hf + 1) * 512], start=(fb == 0), stop=(fb == nfb - 1))) for fb in range(nfb)],
                                reads=[T_AT[ab], T_WD[wb]], writes=[T_ps[pb]])
                        P.op("vector", lambda e: e.scalar_tensor_tensor(out=X[:, t, hf * 512:(hf + 1) * 512], in0=pd[:, :], scalar=0.5, in1=X[:, t, hf * 512:(hf + 1) * 512], op0=ALU.mult, op1=ALU.add),
                             reads=[T_ps[pb], T_X[t]], writes=[T_X[t]])

            units = [(gi, tg) for gi in range(len(groups)) for tg in range(4)]
            load(0)
            load(1)
            for idx, (gi, tg) in enumerate(units):
                gate_up(idx, gi, tg)
                if idx >= 1:
                    pgi, ptg = units[idx - 1]
                    down(idx - 1, pgi, ptg)
                    if ptg == 3 and pgi + 2 < len(groups):
                        load(pgi + 2)
            down(len(units) - 1, *units[-1])
            P.barrier()
            P.flush()

    def proj_fm(es_tmp, HT, T_HT, W, T_W, col0, tg, dst, T_dst, mode, gcol):
        proj_fm.q.append((HT, T_HT, W, T_W, col0, tg, dst, T_dst, mode, gcol))
    proj_fm.q = []

    def proj_flush():
        q = proj_fm.q
        proj_fm.q = []
        tm = proj_fm.tmp
        CT, STb, T_tl = tm["CT"], tm["ST"], tm["T_tl"]

        def S1(i):
            HT, T_HT, W, T_W, col0, tg, dst, T_dst, mode, gcol = q[i]
            k = i % 2
            pq = PSB[k]
            tsl = slice(tg * 512, (tg + 1) * 512)
            P.group("tensor", [(lambda e, c=c: e.matmul(pq[:, :], lhsT=W[:, c, col0:col0 + 128], rhs=HT[:, c, tsl], start=(c == 0), stop=(c == 7))) for c in range(8)],
                    reads=[T_W] + T_HT[tg * 4:tg * 4 + 4], writes=[T_ps[k]])
            if mode == "norm":
                P.op("scalar", lambda e: e.activation(out=tm["xsq"][k][:], in_=pq[:, :], func=AF.Square), reads=[T_ps[k]], writes=[tm["T_xsq"][k]])

        def S2(i):
            HT, T_HT, W, T_W, col0, tg, dst, T_dst, mode, gcol = q[i]
            k = i % 2
            pq = PSB[k]
            xn, T_xn = tm["xn"][k], tm["T_xn"][k]
            if mode == "norm":
                xsq, T_xsq, sd, T_sd = tm["xsq"][k], tm["T_xsq"][k], tm["sd"][k], tm["T_sd"][k]
                pss = PSB[2 + k]
                P.group("tensor", [lambda e: e.matmul(pss[:, :], lhsT=BDb[:], rhs=xsq[:], start=True, stop=True)], reads=[T_xsq, T_cst], writes=[T_ps[2 + k]])
                P.op("scalar", lambda e: e.activation(out=sd[:], in_=pss[:, :], func=AF.Sqrt, scale=1.0 / 64, bias=EPS), reads=[T_ps[2 + k]], writes=[T_sd])
                P.op("vector", lambda e: e.reciprocal(out=sd[:], in_=sd[:]), reads=[T_sd], writes=[T_sd])
                P.op("vector", lambda e: e.scalar_tensor_tensor(out=xn[:], in0=pq[:, :], scalar=gcol, in1=sd[:], op0=ALU.mult, op1=ALU.mult),
                     reads=[T_ps[k], T_sd, T_cst], writes=[T_xn])
            else:
                P.op("scalar", lambda e: e.copy(out=xn[:], in_=pq[:, :]), reads=[T_ps[k]], writes=[T_xn])

        def S3(i):
            HT, T_HT, W, T_W, col0, tg, dst, T_dst, mode, gcol = q[i]
            k = i % 2
            tsl = slice(tg * 512, (tg + 1) * 512)
            xn, T_xn = tm["xn"][k], tm["T_xn"][k]
            pr = PSB[4 + k]
            t1, T_t1, t2, T_t2 = tm["t1"][k], tm["T_t1"][k], tm["t2"][k], tm["T_t2"][k]
            P.group("tensor", [lambda e: e.matmul(pr[:, :], lhsT=RMb[:], rhs=xn[:], start=True, stop=True)], reads=[T_xn, T_cst], writes=[T_ps[4 + k]])
            P.op("gpsimd", lambda e: e.tensor_tensor(out=t1[:], in0=xn[:], in1=CT[:, tsl], op=ALU.mult), reads=[T_xn, T_tl], writes=[T_t1])
            P.op("vector", lambda e: e.tensor_tensor(out=t2[:], in0=pr[:, :], in1=STb[:, tsl], op=ALU.mult), reads=[T_ps[4 + k], T_tl], writes=[T_t2])
            P.op("vector", lambda e: e.tensor_tensor(out=dst, in0=t1[:], in1=t2[:], op=ALU.add), reads=[T_t1, T_t2], writes=[T_dst])

        n = len(q)
        if n == 0:
            return
        S1(0)
        for i in range(n):
            S2(i)
            if i + 1 < n:
                S1(i + 1)
            S3(i)

    def proj_tmp(es):
        tm = {}
        for nm, dtp in (("xn", BF16), ("xsq", BF16), ("sd", F32)):
            tm[nm] = [sbuf(es, "pj_%s%d" % (nm, k), [128, 512], dtp) for k in range(2)]
            tm["T_" + nm] = [Trk(), Trk()]
        for nm, dtp in (("t1", F32), ("t2", F32)):
            buf = sbuf(es, "pj_%s" % nm, [128, 512], dtp)
            tk = Trk()
            tm[nm] = [buf, buf]
            tm["T_" + nm] = [tk, tk]
        tm["CT"] = sbuf(es, "pj_CT", [128, S], BF16)
        tm["ST"] = sbuf(es, "pj_ST", [128, S], BF16)
        tm["T_tl"] = Trk()
        P.dma("sync", tm["CT"][:], tab_d[0], writes=[tm["T_tl"]])
        P.dma("sync", tm["ST"][:], tab_d[1], writes=[tm["T_tl"]])
        proj_fm.tmp = tm

    def load_w_in(Wt, T_W, l, segs):
        wv = win_d[l].rearrange("(c p) f -> p c f", p=128)
        for (d0, s0, n) in segs:
            P.dma("gpsimd", Wt[:, :, d0:d0 + n], wv[:, :, s0:s0 + n], writes=[T_W])

    def proj_tm(HT, T_HT, Wv, T_Wv, ncol, t, pbank):
        pv = PSB[pbank]
        P.group("tensor", [(lambda e, c=c: e.matmul(pv[:, 0:ncol], lhsT=HT[:, c, t * 128:(t + 1) * 128], rhs=Wv[:, c, 0:ncol], start=(c == 0), stop=(c == 7))) for c in range(8)],
                reads=[T_Wv, T_HT[t]], writes=[T_ps[pbank]])
        return pv

    def transpose_out(o_tile, T_o, ncol, OT, T_OT, chunk0, t):
        n = ncol // 128
        psT = PSH[:, 0:n * 128].rearrange("p (c t) -> p c t", c=n)
        P.group("tensor", [(lambda e, j=j: e.transpose(out=psT[:, j, :], in_=o_tile[:, j * 128:(j + 1) * 128], identity=identb[:])) for j in range(n)],
                reads=[T_o, T_cst], writes=[T_psh])
        P.op("scalar", lambda e: e.copy(out=OT[:, chunk0:chunk0 + n, t * 128:(t + 1) * 128], in_=psT), reads=[T_psh], writes=[T_OT[t]])

    def mixer_A(l, HT, T_HT, OT, T_OT):
        with contextlib.ExitStack() as es:
            QA = sbuf(es, "a_qa", [128, 2, S], BF16)
            KA = sbuf(es, "a_ka", [128, S], BF16)
            QI = sbuf(es, "a_qi", [128, 4, S], BF16)
            KI = sbuf(es, "a_ki", [128, S], BF16)
            VA = sbuf(es, "a_va", [128, NT, 65], BF16)
            WI = sbuf(es, "a_wi", [128, NT, 8], F32)
            T_Q = [Trk() for _ in range(4)]
            T_V = [Trk() for _ in range(NT)]
            with contextlib.ExitStack() as es1:
                W = sbuf(es1, "a_w", [128, 8, 1024], BF16)
                Wv = sbuf(es1, "a_wv", [128, 8, 72], BF16)
                T_W = Trk()
                T_Wv = Trk()
                proj_tmp(es1)
                load_w_in(W, T_W, l, [(0, OFF["qa"], 256), (256, OFF["ka"], 64), (320, OFF["ka"], 64),
                                      (384, OFF["qi"], 512), (896, OFF["ki"], 64), (960, OFF["ki"], 64)])
                load_w_in(Wv, T_Wv, l, [(0, OFF["va"], 64), (64, OFF["wi"], 8)])
                P.op("vector", lambda e: e.memset(VA[:, :, 64:65], 1.0), writes=T_V)
                gq = qkgT[:, l * 6 + 0:l * 6 + 1]
                gk = qkgT[:, l * 6 + 1:l * 6 + 2]
                for tg in range(4):
                    tsl = slice(tg * 512, (tg + 1) * 512)
                    for p in range(2):
                        proj_fm(es1, HT, T_HT, W, T_W, p * 128, tg, QA[:, p, tsl], T_Q[tg], "norm", gq)
                    proj_fm(es1, HT, T_HT, W, T_W, 256, tg, KA[:, tsl], T_Q[tg], "norm", gk)
                    for p in range(4):
                        proj_fm(es1, HT, T_HT, W, T_W, 384 + p * 128, tg, QI[:, p, tsl], T_Q[tg], "rope", None)
                    proj_fm(es1, HT, T_HT, W, T_W, 896, tg, KI[:, tsl], T_Q[tg], "rope", None)
                proj_flush()
                for t in range(NT):
                    pv = proj_tm(HT, T_HT, Wv, T_Wv, 72, t, 6)
                    P.op("scalar", lambda e, t=t, pv=pv: e.copy(out=VA[:, t, 0:64], in_=pv[:, 0:64]), reads=[T_ps[6]], writes=[T_V[t]])
                    P.op("vector", lambda e, t=t, pv=pv: e.tensor_copy(out=WI[:, t, :], in_=pv[:, 64:72]), reads=[T_ps[6]], writes=[T_V[t]])
                P.barrier()
                P.flush()
            if SUB == 1:
                return
            with contextlib.ExitStack() as es2:
                ISC = [sbuf(es2, "a_isc%d" % k, [128, S], F32) for k in range(2)]
                M = sbuf(es2, "a_m", [128, S], BF16)
                MT = sbuf(es2, "a_mt", [128, NT, 128], BF16)
                RL = [sbuf(es2, "a_rl%d" % k, [128, 512], F32) for k in range(2)]
                bis = sbuf(es2, "a_bis", [128, 32], F32)
                stp = sbuf(es2, "a_stp", [128, NBIS], F32)
                oa = sbuf(es2, "a_oa", [128, 256], BF16)
                rc = sbuf(es2, "a_rc", [128, 4], F32)
                T_isc = [Trk(), Trk()]
                T_dead = [Trk(), Trk()]
                T_M, T_MT, T_bis, T_oa = Trk(), Trk(), Trk(), Trk()
                T_PTk = [Trk() for _ in range(NT)]
                T_RL = [Trk(), Trk()]
                itc = [0, 0]

                def indexer(qt):
                    ib = qt % 2
                    L = 128 * (qt + 1)
                    qsl = slice(qt * 128, (qt + 1) * 128)
                    tgq = qt // 4
                    nch = (L + 511) // 512
                    for ch in range(nch):
                        c0 = ch * 512
                        cw = min(512, L - c0)
                        for h in range(8):
                            k = itc[0] % 2
                            itc[0] += 1
                            hp = (h % 2) * 64
                            pl = PSB[k]
                            P.group("tensor", [lambda e: e.matmul(pl[:, 0:cw], lhsT=QI[hp:hp + 64, h // 2, qsl], rhs=KI[hp:hp + 64, c0:c0 + cw], start=True, stop=True)],
                                    reads=T_Q[0:tgq + 1], writes=[T_ps[k]])
                            if h == 0:
                                P.op("vector", lambda e: e.tensor_scalar(out=ISC[ib][:, c0:c0 + cw], in0=pl[:, 0:cw], scalar1=0.0, scalar2=WI[:, qt, h:h + 1], op0=ALU.max, op1=ALU.mult),
                                     reads=[T_ps[k], T_V[qt]], writes=[T_isc[ib]])
                            else:
                                P.op("vector", lambda e: e.tensor_scalar(out=RL[k][:, 0:cw], in0=pl[:, 0:cw], scalar1=0.0, scalar2=WI[:, qt, h:h + 1], op0=ALU.max, op1=ALU.mult),
                                     reads=[T_ps[k], T_V[qt]], writes=[T_RL[k]])
                                P.op("gpsimd", lambda e: e.tensor_tensor(out=ISC[ib][:, c0:c0 + cw], in0=ISC[ib][:, c0:c0 + cw], in1=RL[k][:, 0:cw], op=ALU.add),
                                     reads=[T_RL[k], T_isc[ib]], writes=[T_isc[ib]])
                            yield

                def rest(qt):
                    ib = qt % 2
                    L = 128 * (qt + 1)
                    qsl = slice(qt * 128, (qt + 1) * 128)
                    tgq = qt // 4
                    isc = ISC[ib]
                    PT = isc[:, :].bitcast(BF16).rearrange("p (k h q) -> p k h q", k=NT, h=2)
                    Vb = lambda fn: P.op("vector", fn, reads=[T_isc[ib], T_bis, T_cst], writes=[T_bis])
                    if qt >= 2:
                        Vb(lambda e: e.tensor_reduce(out=bis[:, 0:1], in_=isc[:, 0:L], axis=AX.X, op=ALU.min))
                        Vb(lambda e: e.tensor_reduce(out=bis[:, 1:2], in_=isc[:, 0:L], axis=AX.X, op=ALU.max))
                        yield
                    P.op("gpsimd", lambda e: e.affine_select(out=isc[:, L - 128:L], in_=isc[:, L - 128:L], pattern=[[-1, 128]], compare_op=ALU.is_ge, fill=NEG, base=0, channel_multiplier=1),
                         reads=[T_isc[ib], T_bis], writes=[T_isc[ib]])
                    if qt >= 2:
                        Vb(lambda e: e.tensor_tensor(out=bis[:, 2:3], in0=bis[:, 1:2], in1=bis[:, 0:1], op=ALU.subtract))
                        Vb(lambda e: e.tensor_scalar(out=bis[:, 2:3], in0=bis[:, 2:3], scalar1=1.0001, scalar2=1e-20, op0=ALU.mult, op1=ALU.add))
                        Vb(lambda e: e.tensor_scalar(out=stp[:, :], in0=cstf[:, C_POW:C_POW + NBIS], scalar1=bis[:, 2:3], scalar2=None, op0=ALU.mult))
                        for i in range(NBIS):
                            Vb(lambda e: e.scalar_tensor_tensor(out=bis[:, 3:4], in0=bis[:, 0:1], scalar=-1.0, in1=stp[:, i:i + 1], op0=ALU.mult, op1=ALU.subtract))
                            P.op("scalar", lambda e: e.activation(out=M[:, 0:L], in_=isc[:, 0:L], func=AF.Sign, bias=bis[:, 3:4], scale=1.0, accum_out=bis[:, 4:5]),
                                 reads=[T_isc[ib], T_bis], writes=[T_M, T_bis])
                            Vb(lambda e: e.tensor_scalar(out=bis[:, 5:6], in0=bis[:, 4:5], scalar1=float(511 - L), scalar2=stp[:, i:i + 1], op0=ALU.is_ge, op1=ALU.mult))
                            Vb(lambda e: e.tensor_tensor(out=bis[:, 0:1], in0=bis[:, 0:1], in1=bis[:, 5:6], op=ALU.add))
                            yield
                        P.op("vector", lambda e: e.tensor_scalar(out=M[:, 0:L], in0=isc[:, 0:L], scalar1=bis[:, 0:1], scalar2=None, op0=ALU.is_ge),
                             reads=[T_isc[ib], T_bis], writes=[T_M, T_dead[ib]])
                    else:
                        P.op("vector", lambda e: e.tensor_scalar(out=M[:, 0:L], in0=isc[:, 0:L], scalar1=-1.0e29, scalar2=None, op0=ALU.is_ge),
                             reads=[T_isc[ib], T_bis], writes=[T_M, T_dead[ib]])
                    yield
                    for k0 in range(0, qt + 1, 8):
                        n = min(8, qt + 1 - k0)
                        psT = PSH[:, 0:n * 128].rearrange("p (c t) -> p c t", c=n)
                        P.group("tensor", [(lambda e, j=j: e.transpose(out=psT[:, j, :], in_=M[:, (k0 + j) * 128:(k0 + j + 1) * 128], identity=identb[:])) for j in range(n)],
                                reads=[T_M, T_cst], writes=[T_psh])
                        P.op("scalar", lambda e: e.copy(out=MT[:, k0:k0 + n, :], in_=psT), reads=[T_psh], writes=[T_MT])
                        yield
                    po = PSB[6]
                    for pr in range(2):
                        for kt in range(qt + 1):
                            k = itc[1] % 2
                            itc[1] += 1
                            ksl = slice(kt * 128, (kt + 1) * 128)
                            for hh in range(2):
                                pb = 2 + 2 * hh + k
                                ps_s = PSB[pb]
                                P.group("tensor", [lambda e: e.matmul(ps_s[:, 0:128], lhsT=KA[hh * 64:hh * 64 + 64, ksl], rhs=QA[hh * 64:hh * 64 + 64, pr, qsl], start=True, stop=True)],
                                        reads=T_Q[0:tgq + 1], writes=[T_ps[pb]])
                                P.op("scalar", lambda e: e.activation(out=PT[:, kt, hh, :], in_=ps_s[:, 0:128], func=AF.Exp, scale=0.125), reads=[T_ps[pb], T_dead[ib]], writes=[T_PTk[kt]])
                            P.op("vector", lambda e: e.tensor_tensor(out=PT[:, kt, :, :], in0=PT[:, kt, :, :], in1=MT[:, kt:kt + 1, :].to_broadcast([128, 2, 128]), op=ALU.mult),
                                 reads=[T_PTk[kt], T_MT], writes=[T_PTk[kt]])
                            yield
                        pov = po[:, pr * 130:(pr + 1) * 130].rearrange("p (h e) -> p h e", h=2)
                        for hh in range(2):
                            P.group("tensor", [(lambda e, kt=kt: e.matmul(pov[:, hh, :], lhsT=PT[:, kt, hh, :], rhs=VA[:, kt, :], start=(kt == 0), stop=(kt == qt))) for kt in range(qt + 1)],
                                    reads=T_PTk[0:qt + 1] + T_V[0:qt + 1] + [T_isc[ib]], writes=[T_ps[6]])
                        P.op("vector", lambda e: e.reciprocal(out=rc[:, 2 * pr:2 * pr + 2], in_=pov[:, :, 64]), reads=[T_ps[6]], writes=[T_bis])
                        P.op("vector", lambda e: e.tensor_tensor(out=oa[:, pr * 128:(pr + 1) * 128].rearrange("p (h e) -> p h e", h=2), in0=pov[:, :, 0:64],
                                                                 in1=rc[:, 2 * pr:2 * pr + 2].unsqueeze(2).to_broadcast([128, 2, 64]), op=ALU.mult),
                             reads=[T_ps[6], T_bis], writes=[T_oa])
                        yield
                    transpose_out(oa, T_oa, 256, OT, T_OT, 0, qt)
                    yield

                def interleave(ga, gb):
                    da = db = False
                    while not (da and db):
                        if not da:
                            try:
                                next(ga)
                            except StopIteration:
                                da = True
                        if not db:
                            try:
                                next(gb)
                            except StopIteration:
                                db = True

                for _ in indexer(0):
                    pass
                for qt in range(NT):
                    interleave(rest(qt), indexer(qt + 1) if qt + 1 < NT else iter(()))
                P.barrier()
                P.flush()

    def mixer_B(l, HT, T_HT, OT, T_OT):
        with contextlib.ExitStack() as es:
            QB = sbuf(es, "b_q", [128, 2, S], BF16)
            KB = sbuf(es, "b_k", [128, 2, S], BF16)
            VB = sbuf(es, "b_v", [128, NT, 4, 65], BF16)
            KM = sbuf(es, "b_km", [128, 2, 8], BF16)
            T_Q = [Trk() for _ in range(4)]
            T_V = [Trk() for _ in range(NT)]
            T_KM = Trk()
            with contextlib.ExitStack() as es1:
                W = sbuf(es1, "b_w", [128, 8, 512], BF16)
                Wv = sbuf(es1, "b_wv", [128, 8, 256], BF16)
                kmf = sbuf(es1, "b_kmf", [128, 2, 8], F32)
                T_W, T_Wv = Trk(), Trk()
                proj_tmp(es1)
                load_w_in(W, T_W, l, [(0, OFF["qb"], 256), (256, OFF["kb"], 256)])
                load_w_in(Wv, T_Wv, l, [(0, OFF["vb"], 256)])
                P.op("vector", lambda e: e.memset(VB[:, :, :, 64:65], 1.0), writes=T_V)
                gq = qkgT[:, l * 6 + 2:l * 6 + 3]
                gk = qkgT[:, l * 6 + 3:l * 6 + 4]
                for tg in range(4):
                    tsl = slice(tg * 512, (tg + 1) * 512)
                    for p in range(2):
                        proj_fm(es1, HT, T_HT, W, T_W, p * 128, tg, QB[:, p, tsl], T_Q[tg], "norm", gq)
                        proj_fm(es1, HT, T_HT, W, T_W, 256 + p * 128, tg, KB[:, p, tsl], T_Q[tg], "norm", gk)
                proj_flush()
                for t in range(NT):
                    pv = proj_tm(HT, T_HT, Wv, T_Wv, 256, t, 6)
                    P.op("scalar", lambda e, t=t, pv=pv: e.copy(out=VB[:, t, :, 0:64], in_=pv[:, 0:256].rearrange("p (h e) -> p h e", h=4)), reads=[T_ps[6]], writes=[T_V[t]])
                for p in range(2):
                    P.op("vector", lambda e, p=p: e.tensor_reduce(out=kmf[:, p, :], in_=KB[:, p, :].rearrange("p (n k) -> p n k", n=8), axis=AX.X, op=ALU.add), reads=T_Q, writes=[T_KM])
                P.op("vector", lambda e: e.tensor_scalar(out=KM[:, :, :], in0=kmf[:, :, :], scalar1=1.0 / 256, scalar2=None, op0=ALU.mult), reads=[T_KM], writes=[T_KM])
                P.barrier()
                P.flush()
            with contextlib.ExitStack() as es2:
                PT = [sbuf(es2, "b_pt%d" % k, [128, 2, 256], BF16) for k in range(3)]
                T_PT = [Trk() for _ in range(3)]
                gate = sbuf(es2, "b_gate", [128, 2, 4, 8], F32)
                top8 = sbuf(es2, "b_top8", [128, 8], F32)
                BM = sbuf(es2, "b_bm", [128, 2, 4, 8], F32)
                acc = sbuf(es2, "b_acc", [128, 2, 4, 65], F32)
                ob = sbuf(es2, "b_ob", [128, 2, 256], BF16)
                rc = sbuf(es2, "b_rc", [128, 2, 4], F32)
                T_g, T_ob = Trk(), Trk()
                T_acc = [Trk() for _ in range(4)]
                itb = [0]
                for j in range(8):
                    tgq = j // 2
                    if j > 0:
                        for par in range(2):
                            pg = PSB[6]
                            pgv = pg[:, 0:64].rearrange("p (a h n) -> p a h n", a=2, h=4)
                            P.group("tensor", [(lambda e, a=a, h=h: e.matmul(pgv[:, a, h, :], lhsT=QB[(h % 2) * 64:(h % 2) * 64 + 64, h // 2, (2 * j + a) * 128:(2 * j + a + 1) * 128],
                                                                              rhs=KM[(h % 2) * 64:(h % 2) * 64 + 64, h // 2, :], start=True, stop=True)) for a in range(2) for h in (par, par + 2)],
                                    reads=[T_Q[tgq], T_KM], writes=[T_ps[6]])
                            for h in (par, par + 2):
                                P.op("vector", lambda e: e.tensor_copy(out=gate[:, :, h, :], in_=pgv[:, :, h, :]), reads=[T_ps[6]], writes=[T_g])
                        P.op("vector", lambda e: e.memset(gate[:, :, :, j:8], NEG), reads=[T_g], writes=[T_g])
                        for a in range(2):
                            for h in range(4):
                                P.op("vector", lambda e: e.max(out=top8[:, :], in_=gate[:, a, h, :]), reads=[T_g], writes=[T_g])
                                P.op("vector", lambda e: e.tensor_scalar(out=BM[:, a, h, :], in0=gate[:, a, h, :], scalar1=top8[:, 2:3], scalar2=None, op0=ALU.is_ge), reads=[T_g], writes=[T_g])
                    pend = []

                    def stage_a(h, n):
                        hp = (h % 2) * 64
                        pp = h // 2
                        qs_all = slice((2 * j) * 128, (2 * j + 2) * 128)
                        k = itb[0] % 3
                        itb[0] += 1
                        ps_s = PSB[k]
                        psv = ps_s[:, 0:512].rearrange("p (c q) -> p c q", c=2)
                        nb = j if n is None else n
                        P.group("tensor", [(lambda e, c=c: e.matmul(psv[:, c, :], lhsT=KB[hp:hp + 64, pp, (2 * nb + c) * 128:(2 * nb + c + 1) * 128], rhs=QB[hp:hp + 64, pp, qs_all], start=True, stop=True)) for c in range(2)],
                                reads=[T_Q[tgq], T_Q[nb // 2]], writes=[T_ps[k]])
                        P.op("scalar", lambda e: e.activation(out=PT[k][:, :, :], in_=psv, func=AF.Exp, scale=0.125), reads=[T_ps[k]], writes=[T_PT[k]])
                        if n is None:
                            P.op("vector", lambda e: e.tensor_tensor(out=PT[k][:, 0, 0:128], in0=PT[k][:, 0, 0:128], in1=trib[:], op=ALU.mult), reads=[T_PT[k], T_cst], writes=[T_PT[k]])
                            P.op("vector", lambda e: e.tensor_tensor(out=PT[k][:, 1, 128:256], in0=PT[k][:, 1, 128:256], in1=trib[:], op=ALU.mult), reads=[T_PT[k], T_cst], writes=[T_PT[k]])
                        pend.append((h, n, k))

                    def stage_b():
                        h, n, k = pend.pop(0)
                        po = PSB[3 + k]
                        pov = po[:, 0:130].rearrange("p (a e) -> p a e", a=2)
                        if n is None:
                            P.group("tensor", [lambda e: e.matmul(pov[:, 0, :], lhsT=PT[k][:, 0, 0:128], rhs=VB[:, 2 * j, h, :], start=True, stop=True),
                                               lambda e: e.matmul(pov[:, 1, :], lhsT=PT[k][:, 0, 128:256], rhs=VB[:, 2 * j, h, :], start=True, stop=False),
                                               lambda e: e.matmul(pov[:, 1, :], lhsT=PT[k][:, 1, 128:256], rhs=VB[:, 2 * j + 1, h, :], start=False, stop=True)],
                                    reads=[T_PT[k], T_V[2 * j], T_V[2 * j + 1]], writes=[T_ps[3 + k]])
                            P.op("vector", lambda e: e.tensor_copy(out=acc[:, :, h, :], in_=pov), reads=[T_ps[3 + k]], writes=[T_acc[h]])
                        else:
                            P.group("tensor", [(lambda e, c=c, a=a: e.matmul(pov[:, a, :], lhsT=PT[k][:, c, a * 128:(a + 1) * 128], rhs=VB[:, 2 * n + c, h, :], start=(c == 0), stop=(c == 1))) for a in range(2) for c in range(2)],
                                    reads=[T_PT[k], T_V[2 * n], T_V[2 * n + 1]], writes=[T_ps[3 + k]])
                            for a in range(2):
                                P.op("vector", lambda e: e.scalar_tensor_tensor(out=acc[:, a, h, :], in0=pov[:, a, :], scalar=BM[:, a, h, n:n + 1], in1=acc[:, a, h, :], op0=ALU.mult, op1=ALU.add),
                                     reads=[T_ps[3 + k], T_g, T_acc[h]], writes=[T_acc[h]])

                    for h in range(4):
                        for n in [None] + list(range(j)):
                            stage_a(h, n)
                            if len(pend) > 2:
                                stage_b()
                    while pend:
                        stage_b()
                    P.op("vector", lambda e: e.reciprocal(out=rc[:, :, :], in_=acc[:, :, :, 64]), reads=T_acc, writes=[T_g])
                    for a in range(2):
                        P.op("vector", lambda e, a=a: e.tensor_tensor(out=ob[:, a, :].rearrange("p (h e) -> p h e", h=4), in0=acc[:, a, :, 0:64], in1=rc[:, a, :].unsqueeze(2).to_broadcast([128, 4, 64]), op=ALU.mult),
                             reads=T_acc + [T_g], writes=[T_ob])
                        transpose_out(ob[:, a, :], T_ob, 256, OT, T_OT, 2, 2 * j + a)
                P.barrier()
                P.flush()

    def mixer_C(l, HT, T_HT, OT, T_OT, half):
        with contextlib.ExitStack() as es:
            QC = sbuf(es, "c_q", [128, 2, S], BF16)
            KC = sbuf(es, "c_k", [128, 2, S], BF16)
            VC = sbuf(es, "c_v", [128, NT, 2, 129], BF16)
            T_Q = [Trk() for _ in range(4)]
            T_V = [Trk() for _ in range(NT)]
            with contextlib.ExitStack() as es1:
                W = sbuf(es1, "c_w", [128, 8, 512], BF16)
                Wv = sbuf(es1, "c_wv", [128, 8, 256], BF16)
                T_W, T_Wv = Trk(), Trk()
                proj_tmp(es1)
                load_w_in(W, T_W, l, [(0, OFF["qc"] + half * 256, 256), (256, OFF["kc"] + half * 256, 256)])
                load_w_in(Wv, T_Wv, l, [(0, OFF["vc"] + half * 256, 256)])
                P.op("vector", lambda e: e.memset(VC[:, :, :, 128:129], 1.0), writes=T_V)
                gq = qkgT[:, l * 6 + 4:l * 6 + 5]
                gk = qkgT[:, l * 6 + 5:l * 6 + 6]
                for tg in range(4):
                    tsl = slice(tg * 512, (tg + 1) * 512)
                    for p in range(2):
                        proj_fm(es1, HT, T_HT, W, T_W, p * 128, tg, QC[:, p, tsl], T_Q[tg], "norm", gq)
                        proj_fm(es1, HT, T_HT, W, T_W, 256 + p * 128, tg, KC[:, p, tsl], T_Q[tg], "norm", gk)
                proj_flush()
                for t in range(NT):
                    pv = proj_tm(HT, T_HT, Wv, T_Wv, 256, t, 6)
                    P.op("scalar", lambda e, t=t, pv=pv: e.copy(out=VC[:, t, :, 0:128], in_=pv[:, 0:256].rearrange("p (h e) -> p h e", h=2)), reads=[T_ps[6]], writes=[T_V[t]])
                P.barrier()
                P.flush()
            with contextlib.ExitStack() as es2:
                PT = [sbuf(es2, "c_pt%d" % k, [128, NT, 512], BF16) for k in range(2)]
                T_PTk = [[Trk() for _ in range(NT)] for _ in range(2)]
                t0 = sbuf(es2, "c_t0", [128, 4, 128], F32)
                o32 = sbuf(es2, "c_o32", [128, 2, 4, 128], F32)
                oc = sbuf(es2, "c_oc", [128, 4, 256], BF16)
                sq = sbuf(es2, "c_sq", [128, 128], F32)
                st = sbuf(es2, "c_st", [128, 32], F32)
                T_t0, T_o32, T_oc, T_st, T_ss = Trk(), Trk(), Trk(), Trk(), Trk()
                itc = [0, 0]
                units = [(G, hh, c) for G in range(4) for hh in range(2) for c in range(2)]

                def st_gen(u):
                    G, hh, c = units[u]
                    pbf = u % 2
                    cp = c * 64
                    nkt = 4 * G + 4
                    for kt in range(nkt):
                        k = itc[0] % 3
                        itc[0] += 1
                        ps_s = PSB[k]
                        qs0 = max(kt - 4 * G, 0)
                        q0 = (4 * G + qs0) * 128
                        nq = (4 - qs0) * 128
                        P.group("tensor", [lambda e: e.matmul(ps_s[:, 0:nq], lhsT=KC[cp:cp + 64, hh, kt * 128:(kt + 1) * 128], rhs=QC[cp:cp + 64, hh, q0:q0 + nq], start=True, stop=True)],
                                reads=[T_Q[G], T_Q[kt // 4]], writes=[T_ps[k]])
                        P.op("scalar", lambda e: e.activation(out=PT[pbf][:, kt, qs0 * 128:qs0 * 128 + nq], in_=ps_s[:, 0:nq], func=AF.Exp, scale=0.125), reads=[T_ps[k]], writes=[T_PTk[pbf][kt]])
                        if kt >= 4 * G:
                            P.op("vector", lambda e: e.tensor_tensor(out=PT[pbf][:, kt, qs0 * 128:(qs0 + 1) * 128], in0=PT[pbf][:, kt, qs0 * 128:(qs0 + 1) * 128], in1=trib[:], op=ALU.mult),
                                 reads=[T_PTk[pbf][kt], T_cst], writes=[T_PTk[pbf][kt]])
                        yield

                def pv_gen(u):
                    G, hh, c = units[u]
                    pbf = u % 2
                    for qs in range(4):
                        pb = 3 + (itc[1] % 3)
                        itc[1] += 1
                        po = PSB[pb]
                        nk = 4 * G + qs + 1
                        P.group("tensor", [(lambda e, kt=kt: e.matmul(po[:, 0:129], lhsT=PT[pbf][:, kt, qs * 128:(qs + 1) * 128], rhs=VC[:, kt, hh, :], start=(kt == 0), stop=(kt == nk - 1))) for kt in range(nk)],
                                reads=T_PTk[pbf][0:nk] + T_V[0:nk], writes=[T_ps[pb]])
                        P.op("vector", lambda e: e.reciprocal(out=st[:, qs * 2 + c:qs * 2 + c + 1], in_=po[:, 128:129]), reads=[T_ps[pb]], writes=[T_st])
                        if c == 0:
                            P.op("vector", lambda e: e.tensor_scalar(out=t0[:, qs, :], in0=po[:, 0:128], scalar1=st[:, qs * 2:qs * 2 + 1], scalar2=None, op0=ALU.mult),
                                 reads=[T_ps[pb], T_st], writes=[T_t0])
                        else:
                            P.op("vector", lambda e: e.tensor_tensor(out=st[:, 8 + qs:9 + qs], in0=st[:, qs * 2 + 1:qs * 2 + 2], in1=lamv[:, l:l + 1], op=ALU.mult), reads=[T_st, T_lam], writes=[T_st])
                            P.op("vector", lambda e: e.scalar_tensor_tensor(out=o32[:, hh, qs, :], in0=po[:, 0:128], scalar=st[:, 8 + qs:9 + qs], in1=t0[:, qs, :], op0=ALU.mult, op1=ALU.add),
                                 reads=[T_ps[pb], T_st, T_t0], writes=[T_o32])
                            P.op("vector", lambda e: e.tensor_tensor(out=sq[:], in0=o32[:, hh, qs, :], in1=o32[:, hh, qs, :], op=ALU.mult), reads=[T_o32], writes=[T_st])
                            P.op("vector", lambda e: e.reduce_sum(out=st[:, 16 + hh * 4 + qs:17 + hh * 4 + qs], in_=sq[:], axis=AX.X), reads=[T_st], writes=[T_st, T_ss])
                        yield
                    if hh == 1 and c == 1:
                        P.op("scalar", lambda e: e.activation(out=st[:, 24:32], in_=st[:, 16:24], func=AF.Sqrt, scale=1.0 / 128, bias=EPS), reads=[T_ss, T_st], writes=[T_ss])
                        P.op("vector", lambda e: e.reciprocal(out=st[:, 24:32], in_=st[:, 24:32]), reads=[T_ss], writes=[T_ss])
                        P.op("vector", lambda e: e.tensor_scalar(out=st[:, 24:32], in0=st[:, 24:32], scalar1=float(1.0 - lam_init(l)), scalar2=None, op0=ALU.mult), reads=[T_ss], writes=[T_ss])
                        for h2 in range(2):
                            for qs in range(4):
                                P.op("vector", lambda e: e.scalar_tensor_tensor(out=oc[:, qs, h2 * 128:(h2 + 1) * 128], in0=o32[:, h2, qs, :], scalar=st[:, 24 + h2 * 4 + qs:25 + h2 * 4 + qs],
                                                                              in1=subg[:, l * 128:(l + 1) * 128], op0=ALU.mult, op1=ALU.mult),
                                     reads=[T_o32, T_ss, T_cst], writes=[T_oc])
                        yield
                        for qs in range(4):
                            transpose_out(oc[:, qs, :], T_oc, 256, OT, T_OT, 4 + 2 * half, 4 * G + qs)
                        yield

                for _ in st_gen(0):
                    pass
                for u in range(len(units)):
                    ga = pv_gen(u)
                    gb = st_gen(u + 1) if u + 1 < len(units) else iter(())
                    nb = (4 * units[u + 1][0] + 4) if u + 1 < len(units) else 0
                    per = max(1, (nb + 3) // 4)
                    da = db = False
                    while not (da and db):
                        if not db:
                            for _ in range(per):
                                try:
                                    next(gb)
                                except StopIteration:
                                    db = True
                                    break
                        if not da:
                            try:
                                next(ga)
                            except StopIteration:
                                da = True
                P.barrier()
                P.flush()

    def mixer_out(l, HT, T_HT, OT, T_OT):
        with contextlib.ExitStack() as es:
            MG = sbuf(es, "o_mg", [128, 8, S], BF16)
            T_MG = [Trk() for _ in range(4)]
            es_a = contextlib.ExitStack()
            WBR = [sbuf(es_a, "o_wbr%d" % k, [128, 8, 128], BF16) for k in range(2)]
            WGT = [sbuf(es_a, "o_wgt%d" % k, [128, 8, 384], BF16) for k in range(2)]
            sg = [sbuf(es_a, "o_sg%d" % k, [128, 512], F32) for k in range(2)]
            mg32 = [sbuf(es_a, "o_m32%d" % k, [128, 512], F32) for k in range(2)]
            T_W = [Trk(), Trk()]
            T_sg = [Trk(), Trk()]
            T_m32 = [Trk(), Trk()]
            wbv = wbr_d[l].rearrange("(c p) f -> p c f", p=128)
            wiv = win_d[l].rearrange("(c p) f -> p c f", p=128)
            feat = [(0, 2), (2, 2), (4, 4)]
            it = 0
            for dc in range(8):
                wb = dc % 2
                P.dma("gpsimd", WBR[wb][:, :, :], wbv[:, :, dc * 128:(dc + 1) * 128], writes=[T_W[wb]])
                for br, nm in enumerate(("ga", "gb", "gc")):
                    P.dma("gpsimd", WGT[wb][:, :, br * 128:(br + 1) * 128], wiv[:, :, OFF[nm] + dc * 128:OFF[nm] + (dc + 1) * 128], writes=[T_W[wb]])
                for tg in range(4):
                    tsl = slice(tg * 512, (tg + 1) * 512)
                    mk = it % 2
                    it += 1
                    for br in range(3):
                        k = (it + br) % 2
                        pgt = PSB[k]
                        py = PSB[2 + k]
                        c0, ncn = feat[br]
                        P.group("tensor", [(lambda e, c=c, br=br, pgt=pgt, wb=wb: e.matmul(pgt[:, :], lhsT=WGT[wb][:, c, br * 128:(br + 1) * 128], rhs=HT[:, c, tsl], start=(c == 0), stop=(c == 7))) for c in range(8)],
                                reads=[T_W[wb]] + T_HT[tg * 4:tg * 4 + 4], writes=[T_ps[k]])
                        P.group("tensor", [(lambda e, c=c, c0=c0, ncn=ncn, py=py, wb=wb: e.matmul(py[:, :], lhsT=WBR[wb][:, c0 + c, :], rhs=OT[:, c0 + c, tsl], start=(c == 0), stop=(c == ncn - 1))) for c in range(ncn)],
                                reads=[T_W[wb]] + T_OT[tg * 4:tg * 4 + 4], writes=[T_ps[2 + k]])
                        P.op("scalar", lambda e, k=k, pgt=pgt: e.activation(out=sg[k][:], in_=pgt[:, :], func=AF.Sigmoid), reads=[T_ps[k]], writes=[T_sg[k]])
                        if br == 0:
                            P.op("vector", lambda e, k=k, py=py, mk=mk: e.tensor_tensor(out=mg32[mk][:], in0=py[:, :], in1=sg[k][:], op=ALU.mult), reads=[T_ps[2 + k], T_sg[k]], writes=[T_m32[mk]])
                        else:
                            P.op("vector", lambda e, k=k, py=py: e.tensor_tensor(out=sg[k][:], in0=py[:, :], in1=sg[k][:], op=ALU.mult), reads=[T_ps[2 + k], T_sg[k]], writes=[T_sg[k]])
                            if br == 1:
                                P.op("gpsimd", lambda e, k=k, mk=mk: e.tensor_tensor(out=mg32[mk][:], in0=mg32[mk][:], in1=sg[k][:], op=ALU.add), reads=[T_sg[k], T_m32[mk]], writes=[T_m32[mk]])
                            else:
                                P.op("gpsimd", lambda e, k=k, mk=mk, dc=dc: e.tensor_tensor(out=MG[:, dc, tsl], in0=mg32[mk][:], in1=sg[k][:], op=ALU.add), reads=[T_sg[k], T_m32[mk]], writes=[T_MG[tg]])
            P.barrier()
            P.flush()
            es_a.close()
            WO = sbuf(es, "o_wo", [128, 8, D], BF16)
            T_WO = Trk()
            wov = wout_d[l].rearrange("(c p) f -> p c f", p=128)
            for hf in range(2):
                P.dma("gpsimd", WO[:, :, hf * 512:(hf + 1) * 512], wov[:, :, hf * 512:(hf + 1) * 512], writes=[T_WO])
            for t in range(NT):
                for hf in range(2):
                    pb = 4 + (t * 2 + hf) % 2
                    pd = PSB[pb]
                    P.group("tensor", [(lambda e, c=c, t=t, hf=hf, pd=pd: e.matmul(pd[:, :], lhsT=MG[:, c, t * 128:(t + 1) * 128], rhs=WO[:, c, hf * 512:(hf + 1) * 512], start=(c == 0), stop=(c == 7))) for c in range(8)],
                            reads=[T_MG[t // 4], T_WO], writes=[T_ps[pb]])
                    P.op("vector", lambda e, t=t, hf=hf, pd=pd: e.tensor_tensor(out=X[:, t, hf * 512:(hf + 1) * 512], in0=pd[:, :], in1=X[:, t, hf * 512:(hf + 1) * 512], op=ALU.add),
                         reads=[T_ps[pb], T_X[t]], writes=[T_X[t]])
            P.barrier()
            P.flush()

    def mixer(l, b):
        with contextlib.ExitStack() as es:
            HT = sbuf(es, "m_HT", [128, 8, S], BF16)
            OT = sbuf(es, "m_OT", [128, 8, S], BF16)
            T_HT = [Trk() for _ in range(NT)]
            T_OT = [Trk() for _ in range(NT)]
            with contextlib.ExitStack() as esn:
                norm_T(esn, l, 1, HT, T_HT)
                P.barrier()
                P.flush()
            if stage >= 2:
                mixer_A(l, HT, T_HT, OT, T_OT)
            if stage >= 3:
                mixer_B(l, HT, T_HT, OT, T_OT)
            if stage >= 4:
                mixer_C(l, HT, T_HT, OT, T_OT, 0)
                mixer_C(l, HT, T_HT, OT, T_OT, 1)
            if dbg and l == layers[0] and b == 0:
                nch = {2: 2, 3: 4}.get(stage, 8)
                P.dma("sync", dbg_ot[:, 0:nch, :], OT[:, 0:nch, :], reads=T_OT, writes=[T_out])
            if stage >= 5:
                mixer_out(l, HT, T_HT, OT, T_OT)
            P.barrier()
            P.flush()

    for b in range(n_seq):
        for t in range(NT):
            P.dma("sync", X[:, t, :], x_d[b, t * 128:(t + 1) * 128, :], writes=[T_X[t]])
        rope_tables(b)
        for l in layers:
            if stage >= 1:
                ffn(l, 0)
            if stage >= 2:
                mixer(l, b)
            if stage >= 6:
                ffn(l, 1)
        for t in range(NT):
            P.dma("sync", out_d[b, t * 128:(t + 1) * 128, :], X[:, t, :], reads=[T_X[t]], writes=[Trk()])
    P.barrier()
    P.flush()
    es0.close()
    P.close()
    build.nins = P.nins
    return nc


def prep_shared(inputs):
    norm_g = np.asarray(inputs["norm_g"], np.float32)
    qk = np.asarray(inputs["qk_norm_g"], np.float32)
    ngT = np.ascontiguousarray(norm_g.reshape(2, 3, 8, 128).transpose(3, 0, 1, 2).reshape(128, 48))
    qkT = qk.reshape(12, 64).T
    qkgT = np.ascontiguousarray(np.concatenate([qkT, qkT], axis=0))
    return {
        "w_in": np.ascontiguousarray(inputs["w_in"], np.float32),
        "w_branch": np.ascontiguousarray(inputs["w_branch"], np.float32),
        "w_out": np.ascontiguousarray(inputs["w_out"], np.float32),
        "ffn_w_gate": np.ascontiguousarray(inputs["ffn_w_gate"], np.float32),
        "ffn_w_up": np.ascontiguousarray(inputs["ffn_w_up"], np.float32),
        "ffn_w_down": np.ascontiguousarray(inputs["ffn_w_down"], np.float32),
        "cst": make_consts(),
        "ngT": ngT,
        "qkgT": qkgT,
        "lamp": np.ascontiguousarray(np.asarray(inputs["lambda_params"], np.float32).reshape(512)),
        "subg": np.ascontiguousarray(np.asarray(inputs["diff_subln_g"], np.float32).reshape(256)),
    }


def kernel(**inputs):
    x = np.asarray(inputs["x"], np.float32)
    pos = np.asarray(inputs["positions"], np.int32)
    shared = prep_shared(inputs)
    nc = build(n_seq=2, layers=(0, 1))
    in_maps = []
    for c in range(NCORES):
        m = dict(shared)
        m["x"] = np.ascontiguousarray(x[2 * c:2 * c + 2])
        m["pos"] = np.ascontiguousarray(pos[2 * c:2 * c + 2])
        in_maps.append(m)
    res = run_bass_kernel_spmd(nc, in_maps, core_ids=list(range(NCORES)))
    out = np.concatenate([np.asarray(r["out"]) for r in res.results], axis=0)
    return out.astype(np.float32)
```

```python
import contextlib
import math
import numpy as np
import concourse.bass as bass
import concourse.mybir as mybir
from concourse.bass_utils import run_bass_kernel_spmd

F32 = mybir.dt.float32
BF16 = mybir.dt.bfloat16
I32 = mybir.dt.int32
AF = mybir.ActivationFunctionType
ALU = mybir.AluOpType
AX = mybir.AxisListType

S = 2048
D = 1024
NT = 16
DFF = 2816
EPS = 1e-6
NCORES = 8
OFF = dict(qa=0, ka=256, va=320, qi=384, ki=896, wi=960, qb=968, kb=1224, vb=1480,
           qc=1736, kc=2248, vc=2760, ga=3272, gb=4296, gc=5320)
NBIS = 10
C_ID, C_BD, C_RM, C_TRI, C_INVF, C_POW = 0, 128, 256, 384, 512, 513
NCST = C_POW + NBIS
NEG = -1.0e30
import os
SUB = int(os.environ.get('SUB', '9'))
PI = math.pi


class Trk:
    __slots__ = ("name", "w", "r")

    def __init__(self, name=""):
        self.name = name
        self.w = None
        self.r = []


class _Rec:
    def __init__(self):
        self.calls = []

    def __getattr__(self, name):
        def f(*a, **k):
            self.calls.append((name, a, k))
            return self
        return f


class Prog:
    ENG = ("tensor", "vector", "scalar", "gpsimd", "sync")

    def __init__(self, nc, n_dma_sems=24):
        self.nc = nc
        self.stack = []
        self.ops = {e: [] for e in self.ENG}
        self.sems = {}
        self.cnt = {}
        self.wm = {e: {} for e in self.ENG}
        for e in self.ENG:
            self._newsem(e)
        self.dma_keys = {}
        self.dma_rr = {}
        for q in ("sync", "gpsimd", "scalar"):
            self.dma_keys[q] = []
            self.dma_rr[q] = 0
            for i in range(n_dma_sems if q != "scalar" else 4):
                k = "dma_%s%d" % (q, i)
                self._newsem(k)
                self.dma_keys[q].append(k)
        self.nins = 0
        self.fill_reg = None

    def _newsem(self, key):
        cm = self.nc.semaphore(key)
        h = cm.__enter__()
        self.stack.append(cm)
        self.sems[key] = h
        self.cnt[key] = 0

    def _waits(self, eng, reads, writes):
        need = {}
        for t in reads:
            if t.w is not None:
                k, v = t.w
                if need.get(k, 0) < v:
                    need[k] = v
        for t in writes:
            if t.w is not None:
                k, v = t.w
                if need.get(k, 0) < v:
                    need[k] = v
            for (k, v) in t.r:
                if need.get(k, 0) < v:
                    need[k] = v
        out = []
        wm = self.wm[eng]
        for k, v in need.items():
            if wm.get(k, 0) < v:
                wm[k] = v
                out.append((k, v))
        return out

    def _mark(self, dep, reads, writes):
        for t in reads:
            t.r.append(dep)
        for t in writes:
            t.w = dep
            t.r = []

    def op(self, eng, fn, reads=(), writes=()):
        return self.group(eng, [fn], reads, writes)

    def group(self, eng, fns, reads=(), writes=()):
        waits = self._waits(eng, reads, writes)
        self.cnt[eng] += 1
        dep = (eng, self.cnt[eng])
        self._mark(dep, reads, writes)
        sem = self.sems[eng]
        sems = self.sems
        self.nins += len(fns)
        rec = _Rec()
        for f in fns:
            f(rec)
        calls = rec.calls

        def run(e, calls=calls, waits=waits, sem=sem):
            for (k, val) in waits:
                e.wait_ge(sems[k], val)
            last = None
            for (name, a, kw) in calls:
                if name == "affine_select":
                    kw = dict(kw)
                    if self.fill_reg is None:
                        self.fill_reg = e.to_reg(kw["fill"])
                    kw["fill"] = self.fill_reg
                last = getattr(e, name)(*a, **kw)
            last.then_inc(sem, 1)
        self.ops[eng].append(run)
        return dep

    def dma(self, q, out, in_, reads=(), writes=(), **kw):
        waits = self._waits(q, reads, writes)
        k = self.dma_keys[q][self.dma_rr[q] % len(self.dma_keys[q])]
        self.dma_rr[q] += 1
        prev = self.cnt[k]
        if prev > 0 and self.wm[q].get(k, 0) < prev:
            self.wm[q][k] = prev
            waits.append((k, prev))
        self.cnt[k] += 16
        dep = (k, self.cnt[k])
        self._mark(dep, reads, writes)
        sems = self.sems
        sem = sems[k]
        self.nins += 1

        def run(e, waits=waits, sem=sem, out=out, in_=in_, kw=kw):
            for (kk, val) in waits:
                e.wait_ge(sems[kk], val)
            e.dma_start(out=out, in_=in_, **kw).then_inc(sem, 16)
        self.ops[q].append(run)
        return dep

    def barrier(self):
        sems = self.sems
        snap = [(k, v) for k, v in self.cnt.items() if v > 0]
        for eng in self.ENG:
            waits = []
            for (k, v) in snap:
                if k == eng:
                    continue
                if self.wm[eng].get(k, 0) < v:
                    self.wm[eng][k] = v
                    waits.append((k, v))

            def run(e, waits=waits):
                for (k, val) in waits:
                    e.wait_ge(sems[k], val)
            self.ops[eng].append(run)

    def flush(self):
        nc = self.nc
        ops = self.ops
        with nc.Block() as block:
            @block.tensor
            def _(e):
                for f in ops["tensor"]:
                    f(e)

            @block.vector
            def _(e):
                for f in ops["vector"]:
                    f(e)

            @block.scalar
            def _(e):
                for f in ops["scalar"]:
                    f(e)

            @block.gpsimd
            def _(e):
                for f in ops["gpsimd"]:
                    f(e)

            @block.sync
            def _(e):
                for f in ops["sync"]:
                    f(e)
        self.ops = {e: [] for e in self.ENG}

    def close(self):
        for cm in reversed(self.stack):
            cm.__exit__(None, None, None)


def make_consts():
    c = np.zeros((128, NCST), np.float32)
    c[:, C_ID:C_ID + 128] = np.eye(128, dtype=np.float32)
    for p in range(128):
        for f in range(128):
            if p // 64 == f // 64:
                c[p, C_BD + f] = 1.0
    for f in range(128):
        r = f % 64
        if r < 8:
            c[f + 8, C_RM + f] = -1.0
        elif r < 16:
            c[f - 8, C_RM + f] = 1.0
    for k in range(128):
        c[k, C_TRI + k:C_TRI + 128] = 1.0
    inv = np.power(np.float32(500000.0), -np.arange(0, 16, 2, dtype=np.float32) / np.float32(16.0)).astype(np.float32)
    for p in range(128):
        r = p % 64
        c[p, C_INVF] = inv[r % 8] if r < 16 else 0.0
    for i in range(NBIS):
        c[:, C_POW + i] = 3.0 ** (-(i + 1))
    return c


def build(n_seq=2, layers=(0, 1), stage=99, dbg=False):
    nc = bass.Bass("TRN2", target_bir_lowering=False)
    dt = nc.dram_tensor
    x_d = dt("x", [n_seq, S, D], F32, kind="ExternalInput").ap()
    pos_d = dt("pos", [n_seq, S], I32, kind="ExternalInput").ap()
    win_d = dt("w_in", [2, D, 6344], F32, kind="ExternalInput").ap()
    wbr_d = dt("w_branch", [2, D, D], F32, kind="ExternalInput").ap()
    wout_d = dt("w_out", [2, D, D], F32, kind="ExternalInput").ap()
    wg_d = dt("ffn_w_gate", [2, 2, D, DFF], F32, kind="ExternalInput").ap()
    wu_d = dt("ffn_w_up", [2, 2, D, DFF], F32, kind="ExternalInput").ap()
    wd_d = dt("ffn_w_down", [2, 2, DFF, D], F32, kind="ExternalInput").ap()
    cst_d = dt("cst", [128, NCST], F32, kind="ExternalInput").ap()
    ngT_d = dt("ngT", [128, 48], F32, kind="ExternalInput").ap()
    qkgT_d = dt("qkgT", [128, 12], F32, kind="ExternalInput").ap()
    lam_d = dt("lamp", [512], F32, kind="ExternalInput").ap()
    sub_d = dt("subg", [256], F32, kind="ExternalInput").ap()
    out_d = dt("out", [n_seq, S, D], F32, kind="ExternalOutput").ap()
    if dbg:
        dbg_ot = dt("dbg_ot", [128, 8, S], BF16, kind="ExternalOutput").ap()

    P = Prog(nc)
    es0 = contextlib.ExitStack()

    uid = [0]

    def sbuf(es, name, shape, dtype):
        uid[0] += 1
        return es.enter_context(nc.sbuf_tensor("s%d_%s" % (uid[0], name), shape, dtype))

    X = sbuf(es0, "X", [128, NT, D], F32)
    tab_d = dt("tabs", [2, 128, S], BF16, kind="ExternalOutput").ap()
    cstf = sbuf(es0, "cstf", [128, NCST], F32)
    identb = sbuf(es0, "identb", [128, 128], BF16)
    BDb = sbuf(es0, "BDb", [128, 128], BF16)
    RMb = sbuf(es0, "RMb", [128, 128], BF16)
    trib = sbuf(es0, "trib", [128, 128], BF16)
    onesb = sbuf(es0, "onesb", [128, 128], BF16)
    ngT = sbuf(es0, "ngT", [128, 48], F32)
    qkgT = sbuf(es0, "qkgT", [128, 12], F32)
    subg = sbuf(es0, "subg", [128, 256], F32)
    lamv = sbuf(es0, "lamv", [128, 8], F32)
    rs_x = sbuf(es0, "rs_x", [128, 2 * NT], F32)
    PSB = [es0.enter_context(nc.psum_tensor("psb%d" % i, [128, 512], F32)) for i in range(7)]
    PSH = es0.enter_context(nc.psum_tensor("psh", [128, 1024], BF16))
    T_ps = [Trk("ps%d" % i) for i in range(7)]
    T_psh = Trk("psh")
    T_X = [Trk("X%d" % i) for i in range(NT)]
    T_cst = Trk("cst")
    T_tab = Trk("tab")
    T_out = Trk("out")
    T_lam = Trk("lam")

    def lam_init(l):
        return 0.8 - 0.6 * math.exp(-0.3 * l)

    P.dma("sync", cstf[:], cst_d, writes=[T_cst])
    P.dma("gpsimd", identb[:], cst_d[:, C_ID:C_ID + 128], writes=[T_cst])
    P.dma("gpsimd", BDb[:], cst_d[:, C_BD:C_BD + 128], writes=[T_cst])
    P.dma("gpsimd", RMb[:], cst_d[:, C_RM:C_RM + 128], writes=[T_cst])
    P.dma("gpsimd", trib[:], cst_d[:, C_TRI:C_TRI + 128], writes=[T_cst])
    P.dma("sync", ngT[:], ngT_d, writes=[T_cst])
    P.dma("sync", qkgT[:], qkgT_d, writes=[T_cst])
    P.dma("sync", subg[:], sub_d.partition_broadcast(128), writes=[T_cst])
    P.op("vector", lambda e: e.memset(onesb[:], 1.0), writes=[T_cst])
    with contextlib.ExitStack() as es:
        lamp = sbuf(es, "lamp", [128, 512], F32)
        tmp = sbuf(es, "lamtmp", [128, 512], F32)
        sums = sbuf(es, "lamsum", [128, 8], F32)
        T_t = Trk()
        P.dma("sync", lamp[:], lam_d.partition_broadcast(128), writes=[T_lam])
        for l in range(2):
            for j in range(2):
                a0 = l * 256 + (2 * j) * 64
                P.op("vector", lambda e, a0=a0: e.tensor_tensor(out=tmp[:, a0:a0 + 64], in0=lamp[:, a0:a0 + 64], in1=lamp[:, a0 + 64:a0 + 128], op=ALU.mult),
                     reads=[T_lam], writes=[T_t])
                P.op("vector", lambda e, a0=a0, l=l, j=j: e.reduce_sum(out=sums[:, 2 * l + j:2 * l + j + 1], in_=tmp[:, a0:a0 + 64], axis=AX.X),
                     reads=[T_t], writes=[T_t])
        P.op("scalar", lambda e: e.activation(out=sums[:, 4:8], in_=sums[:, 0:4], func=AF.Exp), reads=[T_t], writes=[T_t])
        for l in range(2):
            P.op("vector", lambda e, l=l: e.scalar_tensor_tensor(out=lamv[:, l:l + 1], in0=sums[:, 5 + 2 * l:6 + 2 * l], scalar=-lam_init(l),
                                                               in1=sums[:, 4 + 2 * l:5 + 2 * l], op0=ALU.add, op1=ALU.subtract),
                 reads=[T_t], writes=[T_lam])
        P.barrier()
        P.flush()

    def rope_tables(b):
        with contextlib.ExitStack() as es:
            CT = sbuf(es, "CT", [128, S], BF16)
            STb = sbuf(es, "ST", [128, S], BF16)
            posi = sbuf(es, "posi", [128, S], I32)
            a = sbuf(es, "ta", [128, S], F32)
            r = sbuf(es, "tr", [128, S], F32)
            ki = sbuf(es, "tki", [128, S], I32)
            kf = sbuf(es, "tkf", [128, S], F32)
            T = Trk()
            P.dma("sync", posi[:], pos_d[b].partition_broadcast(128), writes=[T])
            V = lambda fn: P.op("vector", fn, reads=[T, T_cst], writes=[T, T_tab])
            V(lambda e: e.tensor_copy(out=a[:], in_=posi[:]))
            V(lambda e: e.tensor_scalar(out=a[:], in0=a[:], scalar1=cstf[:, C_INVF:C_INVF + 1], scalar2=None, op0=ALU.mult))
            V(lambda e: e.tensor_scalar(out=r[:], in0=a[:], scalar1=float(1.0 / (2 * PI)), scalar2=None, op0=ALU.mult))
            V(lambda e: e.tensor_copy(out=ki[:], in_=r[:]))
            V(lambda e: e.tensor_copy(out=kf[:], in_=ki[:]))
            V(lambda e: e.scalar_tensor_tensor(out=r[:], in0=kf[:], scalar=-6.28125, in1=a[:], op0=ALU.mult, op1=ALU.add))
            V(lambda e: e.scalar_tensor_tensor(out=r[:], in0=kf[:], scalar=-(2 * PI - 6.28125), in1=r[:], op0=ALU.mult, op1=ALU.add))

            def wrap(t):
                V(lambda e: e.tensor_scalar(out=kf[:], in0=t[:], scalar1=PI, scalar2=-2 * PI, op0=ALU.is_gt, op1=ALU.mult))
                V(lambda e: e.tensor_tensor(out=t[:], in0=t[:], in1=kf[:], op=ALU.add))
                V(lambda e: e.tensor_scalar(out=kf[:], in0=t[:], scalar1=-PI, scalar2=2 * PI, op0=ALU.is_lt, op1=ALU.mult))
                V(lambda e: e.tensor_tensor(out=t[:], in0=t[:], in1=kf[:], op=ALU.add))
                V(lambda e: e.tensor_scalar(out=t[:], in0=t[:], scalar1=3.1415925, scalar2=-3.1415925, op0=ALU.min, op1=ALU.max))
            wrap(r)
            P.op("scalar", lambda e: e.activation(out=STb[:], in_=r[:], func=AF.Sin), reads=[T], writes=[T_tab, T])
            V(lambda e: e.tensor_scalar(out=a[:], in0=r[:], scalar1=float(PI / 2), scalar2=None, op0=ALU.add))
            wrap(a)
            P.op("scalar", lambda e: e.activation(out=CT[:], in_=a[:], func=AF.Sin), reads=[T], writes=[T_tab, T])
            P.dma("sync", tab_d[0], CT[:], reads=[T_tab], writes=[Trk()])
            P.dma("sync", tab_d[1], STb[:], reads=[T_tab], writes=[Trk()])
            P.barrier()
            P.flush()

    def norm_T(es, l, i, HT, T_HT):
        xsq = sbuf(es, "n_xsq", [128, D], BF16)
        xn = [sbuf(es, "n_xn%d" % k, [128, D], BF16) for k in range(2)]
        T_sq = Trk()
        T_xn = [Trk(), Trk()]
        T_rs = Trk()
        for t in range(NT):
            P.op("scalar", lambda e, t=t: e.activation(out=xsq[:], in_=X[:, t, :], func=AF.Square, accum_out=rs_x[:, t:t + 1]),
                 reads=[T_X[t]], writes=[T_sq, T_rs])
        P.op("scalar", lambda e: e.activation(out=rs_x[:, NT:2 * NT], in_=rs_x[:, 0:NT], func=AF.Sqrt, scale=1.0 / D, bias=EPS),
             reads=[T_rs], writes=[T_rs])
        P.op("vector", lambda e: e.reciprocal(out=rs_x[:, 0:NT], in_=rs_x[:, NT:2 * NT]), reads=[T_rs], writes=[T_rs])
        g0 = (l * 3 + i) * 8
        psT = PSH[:, :].rearrange("p (c t) -> p c t", c=8)
        for t in range(NT):
            k = t % 2
            P.op("vector", lambda e, t=t, k=k: e.tensor_scalar(out=xn[k][:], in0=X[:, t, :], scalar1=rs_x[:, t:t + 1], scalar2=None, op0=ALU.mult),
                 reads=[T_X[t], T_rs], writes=[T_xn[k]])
            P.group("tensor", [(lambda e, c=c, k=k: e.transpose(out=psT[:, c, :], in_=xn[k][:, c * 128:(c + 1) * 128], identity=identb[:])) for c in range(8)],
                    reads=[T_xn[k], T_cst], writes=[T_psh])
            P.op("vector", lambda e, t=t: e.tensor_tensor(out=HT[:, :, t * 128:(t + 1) * 128], in0=psT,
                                                         in1=ngT[:, g0:g0 + 8].unsqueeze(2).to_broadcast([128, 8, 128]), op=ALU.mult),
                 reads=[T_psh, T_cst], writes=[T_HT[t]])

    def ffn(l, i):
        with contextlib.ExitStack() as es:
            HT = sbuf(es, "f_HT", [128, 8, S], BF16)
            T_HT = [Trk() for _ in range(NT)]
            norm_T(es, l, i, HT, T_HT)
            WG = [sbuf(es, "f_wg%d" % k, [128, 8, 512], BF16) for k in range(2)]
            WU = [sbuf(es, "f_wu%d" % k, [128, 8, 512], BF16) for k in range(2)]
            WD = [sbuf(es, "f_wd%d" % k, [128, 4, D], BF16) for k in range(2)]
            AT = [sbuf(es, "f_at%d" % k, [128, 4, 512], BF16) for k in range(2)]
            SG = [sbuf(es, "f_sg%d" % k, [128, 512], F32) for k in range(2)]
            T_WG, T_WU, T_WD = [Trk(), Trk()], [Trk(), Trk()], [Trk(), Trk()]
            T_AT = [Trk(), Trk()]
            T_SG = [Trk(), Trk()]
            wgv = wg_d[l, i].rearrange("(c p) f -> p c f", p=128)
            wuv = wu_d[l, i].rearrange("(c p) f -> p c f", p=128)
            groups = [(f0, min(512, DFF - f0)) for f0 in range(0, DFF, 512)]
            itf = [0]

            def load(gi):
                f0, fw = groups[gi]
                wb = gi % 2
                nfb = fw // 128
                P.dma("gpsimd", WG[wb][:, :, 0:fw], wgv[:, :, f0:f0 + fw], writes=[T_WG[wb]])
                P.dma("gpsimd", WU[wb][:, :, 0:fw], wuv[:, :, f0:f0 + fw], writes=[T_WU[wb]])
                P.dma("gpsimd", WD[wb][:, 0:nfb, :], wd_d[l, i][f0:f0 + fw, :].rearrange("(c p) d -> p c d", p=128), writes=[T_WD[wb]])

            def gate_up(idx, gi, tg):
                f0, fw = groups[gi]
                wb = gi % 2
                ab = idx % 2
                for fb in range(fw // 128):
                    k = itf[0] % 2
                    itf[0] += 1
                    pg, pu = PSB[2 * k], PSB[2 * k + 1]
                    P.group("tensor", [(lambda e, c=c: e.matmul(pg[:, :], lhsT=WG[wb][:, c, fb * 128:(fb + 1) * 128], rhs=HT[:, c, tg * 512:(tg + 1) * 512], start=(c == 0), stop=(c == 7))) for c in range(8)],
                            reads=[T_WG[wb]] + T_HT[tg * 4:tg * 4 + 4], writes=[T_ps[2 * k]])
                    P.group("tensor", [(lambda e, c=c: e.matmul(pu[:, :], lhsT=WU[wb][:, c, fb * 128:(fb + 1) * 128], rhs=HT[:, c, tg * 512:(tg + 1) * 512], start=(c == 0), stop=(c == 7))) for c in range(8)],
                            reads=[T_WU[wb]] + T_HT[tg * 4:tg * 4 + 4], writes=[T_ps[2 * k + 1]])
                    P.op("scalar", lambda e: e.activation(out=SG[k][:], in_=pg[:, :], func=AF.Silu), reads=[T_ps[2 * k]], writes=[T_SG[k]])
                    P.op("vector", lambda e: e.tensor_tensor(out=AT[ab][:, fb, :], in0=pu[:, :], in1=SG[k][:], op=ALU.mult),
                         reads=[T_ps[2 * k + 1], T_SG[k]], writes=[T_AT[ab]])

            def down(idx, gi, tg):
                f0, fw = groups[gi]
                wb = gi % 2
                ab = idx % 2
                nfb = fw // 128
                for tt in range(4):
                    t = tg * 4 + tt
                    for hf in range(2):
                        pb = 4 + (t * 2 + hf) % 2
                        pd = PSB[pb]
                        P.group("tensor", [(lambda e, fb=fb: e.matmul(pd[:, :], lhsT=AT[ab][:, fb, tt * 128:(tt + 1) * 128], rhs=WD[wb][:, fb, hf * 512:(hf + 1) * 512], start=(fb == 0), stop=(fb == nfb - 1))) for fb in range(nfb)],
                                reads=[T_AT[ab], T_WD[wb]], writes=[T_ps[pb]])
                        P.op("vector", lambda e: e.scalar_tensor_tensor(out=X[:, t, hf * 512:(hf + 1) * 512], in0=pd[:, :], scalar=0.5, in1=X[:, t, hf * 512:(hf + 1) * 512], op0=ALU.mult, op1=ALU.add),
                             reads=[T_ps[pb], T_X[t]], writes=[T_X[t]])

            units = [(gi, tg) for gi in range(len(groups)) for tg in range(4)]
            load(0)
            load(1)
            for idx, (gi, tg) in enumerate(units):
                gate_up(idx, gi, tg)
                if idx >= 1:
                    pgi, ptg = units[idx - 1]
                    down(idx - 1, pgi, ptg)
                    if ptg == 3 and pgi + 2 < len(groups):
                        load(pgi + 2)
            down(len(units) - 1, *units[-1])
            P.barrier()
            P.flush()

    def proj_fm(es_tmp, HT, T_HT, W, T_W, col0, tg, dst, T_dst, mode, gcol):
        proj_fm.q.append((HT, T_HT, W, T_W, col0, tg, dst, T_dst, mode, gcol))
    proj_fm.q = []

    def proj_flush():
        q = proj_fm.q
        proj_fm.q = []
        tm = proj_fm.tmp
        CT, STb, T_tl = tm["CT"], tm["ST"], tm["T_tl"]

        def S1(i):
            HT, T_HT, W, T_W, col0, tg, dst, T_dst, mode, gcol = q[i]
            k = i % 2
            pq = PSB[k]
            tsl = slice(tg * 512, (tg + 1) * 512)
            P.group("tensor", [(lambda e, c=c: e.matmul(pq[:, :], lhsT=W[:, c, col0:col0 + 128], rhs=HT[:, c, tsl], start=(c == 0), stop=(c == 7))) for c in range(8)],
                    reads=T_W + T_HT[tg * 4:tg * 4 + 4], writes=[T_ps[k]])
            if mode == "norm":
                P.op("scalar", lambda e: e.activation(out=tm["xsq"][k][:], in_=pq[:, :], func=AF.Square), reads=[T_ps[k]], writes=[tm["T_xsq"][k]])

        def S2(i):
            HT, T_HT, W, T_W, col0, tg, dst, T_dst, mode, gcol = q[i]
            k = i % 2
            pq = PSB[k]
            xn, T_xn = tm["xn"][k], tm["T_xn"][k]
            if mode == "norm":
                xsq, T_xsq, sd, T_sd = tm["xsq"][k], tm["T_xsq"][k], tm["sd"][k], tm["T_sd"][k]
                pss = PSB[2 + k]
                P.group("tensor", [lambda e: e.matmul(pss[:, :], lhsT=BDb[:], rhs=xsq[:], start=True, stop=True)], reads=[T_xsq, T_cst], writes=[T_ps[2 + k]])
                P.op("scalar", lambda e: e.activation(out=sd[:], in_=pss[:, :], func=AF.Sqrt, scale=1.0 / 64, bias=EPS), reads=[T_ps[2 + k]], writes=[T_sd])
                P.op("vector", lambda e: e.reciprocal(out=sd[:], in_=sd[:]), reads=[T_sd], writes=[T_sd])
                P.op("vector", lambda e: e.scalar_tensor_tensor(out=xn[:], in0=pq[:, :], scalar=gcol, in1=sd[:], op0=ALU.mult, op1=ALU.mult),
                     reads=[T_ps[k], T_sd, T_cst], writes=[T_xn])
            else:
                P.op("scalar", lambda e: e.copy(out=xn[:], in_=pq[:, :]), reads=[T_ps[k]], writes=[T_xn])

        def S3(i):
            HT, T_HT, W, T_W, col0, tg, dst, T_dst, mode, gcol = q[i]
            k = i % 2
            tsl = slice(tg * 512, (tg + 1) * 512)
            xn, T_xn = tm["xn"][k], tm["T_xn"][k]
            pr = PSB[4 + k]
            t1, T_t1, t2, T_t2 = tm["t1"][k], tm["T_t1"][k], tm["t2"][k], tm["T_t2"][k]
            P.group("tensor", [lambda e: e.matmul(pr[:, :], lhsT=RMb[:], rhs=xn[:], start=True, stop=True)], reads=[T_xn, T_cst], writes=[T_ps[4 + k]])
            P.op("gpsimd", lambda e: e.tensor_tensor(out=t1[:], in0=xn[:], in1=CT[:, tsl], op=ALU.mult), reads=[T_xn, T_tl], writes=[T_t1])
            P.op("vector", lambda e: e.tensor_tensor(out=t2[:], in0=pr[:, :], in1=STb[:, tsl], op=ALU.mult), reads=[T_ps[4 + k], T_tl], writes=[T_t2])
            P.op("vector", lambda e: e.tensor_tensor(out=dst, in0=t1[:], in1=t2[:], op=ALU.add), reads=[T_t1, T_t2], writes=[T_dst])

        n = len(q)
        if n == 0:
            return
        S1(0)
        for i in range(n):
            S2(i)
            if i + 1 < n:
                S1(i + 1)
            S3(i)

    def proj_tmp(es):
        tm = {}
        for nm, dtp in (("xn", BF16), ("xsq", BF16), ("sd", F32)):
            tm[nm] = [sbuf(es, "pj_%s%d" % (nm, k), [128, 512], dtp) for k in range(2)]
            tm["T_" + nm] = [Trk(), Trk()]
        for nm, dtp in (("t1", F32), ("t2", F32)):
            buf = sbuf(es, "pj_%s" % nm, [128, 512], dtp)
            tk = Trk()
            tm[nm] = [buf, buf]
            tm["T_" + nm] = [tk, tk]
        tm["CT"] = sbuf(es, "pj_CT", [128, S], BF16)
        tm["ST"] = sbuf(es, "pj_ST", [128, S], BF16)
        tm["T_tl"] = Trk()
        P.dma("sync", tm["CT"][:], tab_d[0], writes=[tm["T_tl"]])
        P.dma("sync", tm["ST"][:], tab_d[1], writes=[tm["T_tl"]])
        proj_fm.tmp = tm

    def load_w_in(Wt, T_W, l, segs):
        wv = win_d[l].rearrange("(c p) f -> p c f", p=128)
        for (d0, s0, n) in segs:
            tk = Trk()
            T_W.append(tk)
            P.dma("gpsimd", Wt[:, :, d0:d0 + n], wv[:, :, s0:s0 + n], writes=[tk])

    def proj_tm(HT, T_HT, Wv, T_Wv, ncol, t, pbank):
        pv = PSB[pbank]
        P.group("tensor", [(lambda e, c=c: e.matmul(pv[:, 0:ncol], lhsT=HT[:, c, t * 128:(t + 1) * 128], rhs=Wv[:, c, 0:ncol], start=(c == 0), stop=(c == 7))) for c in range(8)],
                reads=T_Wv + [T_HT[t]], writes=[T_ps[pbank]])
        return pv

    def transpose_out(o_tile, T_o, ncol, OT, T_OT, chunk0, t):
        n = ncol // 128
        psT = PSH[:, 0:n * 128].rearrange("p (c t) -> p c t", c=n)
        P.group("tensor", [(lambda e, j=j: e.transpose(out=psT[:, j, :], in_=o_tile[:, j * 128:(j + 1) * 128], identity=identb[:])) for j in range(n)],
                reads=[T_o, T_cst], writes=[T_psh])
        P.op("scalar", lambda e: e.copy(out=OT[:, chunk0:chunk0 + n, t * 128:(t + 1) * 128], in_=psT), reads=[T_psh], writes=[T_OT[t]])

    def mixer_A(l, HT, T_HT, OT, T_OT):
        with contextlib.ExitStack() as es:
            QA = sbuf(es, "a_qa", [128, 2, S], BF16)
            KA = sbuf(es, "a_ka", [128, S], BF16)
            QI = sbuf(es, "a_qi", [128, 4, S], BF16)
            KI = sbuf(es, "a_ki", [128, S], BF16)
            VA = sbuf(es, "a_va", [128, NT, 65], BF16)
            WI = sbuf(es, "a_wi", [128, NT, 8], F32)
            T_Q = [Trk() for _ in range(4)]
            T_V = [Trk() for _ in range(NT)]
            with contextlib.ExitStack() as es1:
                W = sbuf(es1, "a_w", [128, 8, 1024], BF16)
                Wv = sbuf(es1, "a_wv", [128, 8, 72], BF16)
                T_W = []
                T_Wv = []
                proj_tmp(es1)
                load_w_in(W, T_W, l, [(0, OFF["qa"], 256), (256, OFF["ka"], 64), (320, OFF["ka"], 64),
                                      (384, OFF["qi"], 512), (896, OFF["ki"], 64), (960, OFF["ki"], 64)])
                load_w_in(Wv, T_Wv, l, [(0, OFF["va"], 64), (64, OFF["wi"], 8)])
                P.op("vector", lambda e: e.memset(VA[:, :, 64:65], 1.0), writes=T_V)
                gq = qkgT[:, l * 6 + 0:l * 6 + 1]
                gk = qkgT[:, l * 6 + 1:l * 6 + 2]
                for tg in range(4):
                    tsl = slice(tg * 512, (tg + 1) * 512)
                    for p in range(2):
                        proj_fm(es1, HT, T_HT, W, T_W, p * 128, tg, QA[:, p, tsl], T_Q[tg], "norm", gq)
                    proj_fm(es1, HT, T_HT, W, T_W, 256, tg, KA[:, tsl], T_Q[tg], "norm", gk)
                    for p in range(4):
                        proj_fm(es1, HT, T_HT, W, T_W, 384 + p * 128, tg, QI[:, p, tsl], T_Q[tg], "rope", None)
                    proj_fm(es1, HT, T_HT, W, T_W, 896, tg, KI[:, tsl], T_Q[tg], "rope", None)
                proj_flush()
                for t in range(NT):
                    pv = proj_tm(HT, T_HT, Wv, T_Wv, 72, t, 6)
                    P.op("scalar", lambda e, t=t, pv=pv: e.copy(out=VA[:, t, 0:64], in_=pv[:, 0:64]), reads=[T_ps[6]], writes=[T_V[t]])
                    P.op("vector", lambda e, t=t, pv=pv: e.tensor_copy(out=WI[:, t, :], in_=pv[:, 64:72]), reads=[T_ps[6]], writes=[T_V[t]])
                P.barrier()
                P.flush()
            if SUB == 1:
                return
            with contextlib.ExitStack() as es2:
                ISC = [sbuf(es2, "a_isc%d" % k, [128, S], F32) for k in range(2)]
                M = sbuf(es2, "a_m", [128, S], BF16)
                MT = sbuf(es2, "a_mt", [128, NT, 128], BF16)
                RL = [sbuf(es2, "a_rl%d" % k, [128, 512], F32) for k in range(2)]
                bis = sbuf(es2, "a_bis", [128, 32], F32)
                stp = sbuf(es2, "a_stp", [128, NBIS], F32)
                oa = sbuf(es2, "a_oa", [128, 256], BF16)
                rc = sbuf(es2, "a_rc", [128, 4], F32)
                J2 = sbuf(es2, "a_j2", [128, S], BF16)
                T_J2, T_bis2 = Trk(), Trk()
                T_isc = [Trk(), Trk()]
                T_dead = [Trk(), Trk()]
                T_M, T_MT, T_bis, T_oa = Trk(), Trk(), Trk(), Trk()
                T_PTk = [Trk() for _ in range(NT)]
                T_RL = [Trk(), Trk()]
                itc = [0, 0]

                def indexer(qt):
                    ib = qt % 2
                    L = 128 * (qt + 1)
                    qsl = slice(qt * 128, (qt + 1) * 128)
                    tgq = qt // 4
                    nch = (L + 511) // 512
                    for ch in range(nch):
                        c0 = ch * 512
                        cw = min(512, L - c0)
                        for h in range(8):
                            k = itc[0] % 2
                            itc[0] += 1
                            hp = (h % 2) * 64
                            pl = PSB[k]
                            P.group("tensor", [lambda e: e.matmul(pl[:, 0:cw], lhsT=QI[hp:hp + 64, h // 2, qsl], rhs=KI[hp:hp + 64, c0:c0 + cw], start=True, stop=True)],
                                    reads=T_Q[0:tgq + 1], writes=[T_ps[k]])
                            if h == 0:
                                P.op("vector", lambda e: e.tensor_scalar(out=ISC[ib][:, c0:c0 + cw], in0=pl[:, 0:cw], scalar1=0.0, scalar2=WI[:, qt, h:h + 1], op0=ALU.max, op1=ALU.mult),
                                     reads=[T_ps[k], T_V[qt]], writes=[T_isc[ib]])
                            else:
                                P.op("vector", lambda e: e.tensor_scalar(out=RL[k][:, 0:cw], in0=pl[:, 0:cw], scalar1=0.0, scalar2=WI[:, qt, h:h + 1], op0=ALU.max, op1=ALU.mult),
                                     reads=[T_ps[k], T_V[qt]], writes=[T_RL[k]])
                                P.op("gpsimd", lambda e: e.tensor_tensor(out=ISC[ib][:, c0:c0 + cw], in0=ISC[ib][:, c0:c0 + cw], in1=RL[k][:, 0:cw], op=ALU.add),
                                     reads=[T_RL[k], T_isc[ib]], writes=[T_isc[ib]])
                            yield

                def rest(qt):
                    ib = qt % 2
                    L = 128 * (qt + 1)
                    qsl = slice(qt * 128, (qt + 1) * 128)
                    tgq = qt // 4
                    isc = ISC[ib]
                    PT = isc[:, :].bitcast(BF16).rearrange("p (k h q) -> p k h q", k=NT, h=2)
                    Vb = lambda fn: P.op("vector", fn, reads=[T_isc[ib], T_bis, T_cst], writes=[T_bis])
                    if qt >= 2:
                        Vb(lambda e: e.tensor_reduce(out=bis[:, 0:1], in_=isc[:, 0:L], axis=AX.X, op=ALU.min))
                        Vb(lambda e: e.tensor_reduce(out=bis[:, 1:2], in_=isc[:, 0:L], axis=AX.X, op=ALU.max))
                        yield
                    P.op("gpsimd", lambda e: e.affine_select(out=isc[:, L - 128:L], in_=isc[:, L - 128:L], pattern=[[-1, 128]], compare_op=ALU.is_ge, fill=NEG, base=0, channel_multiplier=1),
                         reads=[T_isc[ib], T_bis], writes=[T_isc[ib]])
                    if qt >= 2:
                        Vb(lambda e: e.tensor_tensor(out=bis[:, 2:3], in0=bis[:, 1:2], in1=bis[:, 0:1], op=ALU.subtract))
                        Vb(lambda e: e.tensor_scalar(out=bis[:, 2:3], in0=bis[:, 2:3], scalar1=1.0001, scalar2=1e-20, op0=ALU.mult, op1=ALU.add))
                        Vb(lambda e: e.tensor_scalar(out=stp[:, :], in0=cstf[:, C_POW:C_POW + NBIS], scalar1=bis[:, 2:3], scalar2=None, op0=ALU.mult))
                        for i in range(NBIS):
                            Vb(lambda e: e.scalar_tensor_tensor(out=bis[:, 3:4], in0=bis[:, 0:1], scalar=-1.0, in1=stp[:, i:i + 1], op0=ALU.mult, op1=ALU.subtract))
                            P.op("scalar", lambda e: e.activation(out=M[:, 0:L], in_=isc[:, 0:L], func=AF.Sign, bias=bis[:, 3:4], scale=1.0, accum_out=bis[:, 4:5]),
                                 reads=[T_isc[ib], T_bis], writes=[T_M, T_bis])
                            P.op("vector", lambda e: e.scalar_tensor_tensor(out=bis[:, 6:7], in0=stp[:, i:i + 1], scalar=2.0, in1=bis[:, 0:1], op0=ALU.mult, op1=ALU.add),
                                 reads=[T_bis, T_bis2], writes=[T_bis2])
                            P.op("vector", lambda e: e.tensor_scalar(out=J2[:, 0:L], in0=isc[:, 0:L], scalar1=bis[:, 6:7], scalar2=0.0, op0=ALU.is_ge, op1=ALU.add, accum_out=bis[:, 7:8]),
                                 reads=[T_isc[ib], T_bis2], writes=[T_J2, T_bis2])
                            Vb(lambda e: e.tensor_scalar(out=bis[:, 5:6], in0=bis[:, 4:5], scalar1=float(511 - L), scalar2=None, op0=ALU.is_ge))
                            P.op("vector", lambda e: e.scalar_tensor_tensor(out=bis[:, 5:6], in0=bis[:, 7:8], scalar=255.5, in1=bis[:, 5:6], op0=ALU.is_ge, op1=ALU.add),
                                 reads=[T_bis, T_bis2], writes=[T_bis])
                            Vb(lambda e: e.scalar_tensor_tensor(out=bis[:, 0:1], in0=bis[:, 5:6], scalar=stp[:, i:i + 1], in1=bis[:, 0:1], op0=ALU.mult, op1=ALU.add))
                            yield
                        P.op("vector", lambda e: e.tensor_scalar(out=M[:, 0:L], in0=isc[:, 0:L], scalar1=bis[:, 0:1], scalar2=None, op0=ALU.is_ge),
                             reads=[T_isc[ib], T_bis], writes=[T_M, T_dead[ib]])
                    else:
                        P.op("vector", lambda e: e.tensor_scalar(out=M[:, 0:L], in0=isc[:, 0:L], scalar1=-1.0e29, scalar2=None, op0=ALU.is_ge),
                             reads=[T_isc[ib], T_bis], writes=[T_M, T_dead[ib]])
                    yield
                    for k0 in range(0, qt + 1, 8):
                        n = min(8, qt + 1 - k0)
                        psT = PSH[:, 0:n * 128].rearrange("p (c t) -> p c t", c=n)
                        P.group("tensor", [(lambda e, j=j: e.transpose(out=psT[:, j, :], in_=M[:, (k0 + j) * 128:(k0 + j + 1) * 128], identity=identb[:])) for j in range(n)],
                                reads=[T_M, T_cst], writes=[T_psh])
                        P.op("scalar", lambda e: e.copy(out=MT[:, k0:k0 + n, :], in_=psT), reads=[T_psh], writes=[T_MT])
                        yield
                    po = PSB[6]
                    for pr in range(2):
                        for kt in range(qt + 1):
                            k = itc[1] % 2
                            itc[1] += 1
                            ksl = slice(kt * 128, (kt + 1) * 128)
                            for hh in range(2):
                                pb = 2 + 2 * hh + k
                                ps_s = PSB[pb]
                                P.group("tensor", [lambda e: e.matmul(ps_s[:, 0:128], lhsT=KA[hh * 64:hh * 64 + 64, ksl], rhs=QA[hh * 64:hh * 64 + 64, pr, qsl], start=True, stop=True)],
                                        reads=T_Q[0:tgq + 1], writes=[T_ps[pb]])
                                P.op("scalar", lambda e: e.activation(out=PT[:, kt, hh, :], in_=ps_s[:, 0:128], func=AF.Exp, scale=0.125), reads=[T_ps[pb], T_dead[ib]], writes=[T_PTk[kt]])
                            P.op("vector", lambda e: e.tensor_tensor(out=PT[:, kt, :, :], in0=PT[:, kt, :, :], in1=MT[:, kt:kt + 1, :].to_broadcast([128, 2, 128]), op=ALU.mult),
                                 reads=[T_PTk[kt], T_MT], writes=[T_PTk[kt]])
                            yield
                        pov = po[:, pr * 130:(pr + 1) * 130].rearrange("p (h e) -> p h e", h=2)
                        for hh in range(2):
                            P.group("tensor", [(lambda e, kt=kt: e.matmul(pov[:, hh, :], lhsT=PT[:, kt, hh, :], rhs=VA[:, kt, :], start=(kt == 0), stop=(kt == qt))) for kt in range(qt + 1)],
                                    reads=T_PTk[0:qt + 1] + T_V[0:qt + 1] + [T_isc[ib]], writes=[T_ps[6]])
                        P.op("vector", lambda e: e.reciprocal(out=rc[:, 2 * pr:2 * pr + 2], in_=pov[:, :, 64]), reads=[T_ps[6]], writes=[T_bis])
                        P.op("vector", lambda e: e.tensor_tensor(out=oa[:, pr * 128:(pr + 1) * 128].rearrange("p (h e) -> p h e", h=2), in0=pov[:, :, 0:64],
                                                                 in1=rc[:, 2 * pr:2 * pr + 2].unsqueeze(2).to_broadcast([128, 2, 64]), op=ALU.mult),
                             reads=[T_ps[6], T_bis], writes=[T_oa])
                        yield
                    transpose_out(oa, T_oa, 256, OT, T_OT, 0, qt)
                    yield

                def interleave(ga, gb):
                    da = db = False
                    while not (da and db):
                        if not da:
                            try:
                                next(ga)
                            except StopIteration:
                                da = True
                        if not db:
                            try:
                                next(gb)
                            except StopIteration:
                                db = True

                for _ in indexer(0):
                    pass
                for qt in range(NT):
                    interleave(rest(qt), indexer(qt + 1) if qt + 1 < NT else iter(()))
                P.barrier()
                P.flush()

    def mixer_B(l, HT, T_HT, OT, T_OT):
        with contextlib.ExitStack() as es:
            QB = sbuf(es, "b_q", [128, 2, S], BF16)
            KB = sbuf(es, "b_k", [128, 2, S], BF16)
            VB = sbuf(es, "b_v", [128, NT, 4, 65], BF16)
            KM = sbuf(es, "b_km", [128, 2, 8], BF16)
            T_Q = [Trk() for _ in range(4)]
            T_V = [Trk() for _ in range(NT)]
            T_KM = Trk()
            with contextlib.ExitStack() as es1:
                W = sbuf(es1, "b_w", [128, 8, 512], BF16)
                Wv = sbuf(es1, "b_wv", [128, 8, 256], BF16)
                kmf = sbuf(es1, "b_kmf", [128, 2, 8], F32)
                T_W, T_Wv = [], []
                proj_tmp(es1)
                load_w_in(W, T_W, l, [(0, OFF["qb"], 256), (256, OFF["kb"], 256)])
                load_w_in(Wv, T_Wv, l, [(0, OFF["vb"], 256)])
                P.op("vector", lambda e: e.memset(VB[:, :, :, 64:65], 1.0), writes=T_V)
                gq = qkgT[:, l * 6 + 2:l * 6 + 3]
                gk = qkgT[:, l * 6 + 3:l * 6 + 4]
                for tg in range(4):
                    tsl = slice(tg * 512, (tg + 1) * 512)
                    for p in range(2):
                        proj_fm(es1, HT, T_HT, W, T_W, p * 128, tg, QB[:, p, tsl], T_Q[tg], "norm", gq)
                        proj_fm(es1, HT, T_HT, W, T_W, 256 + p * 128, tg, KB[:, p, tsl], T_Q[tg], "norm", gk)
                proj_flush()
                for t in range(NT):
                    pv = proj_tm(HT, T_HT, Wv, T_Wv, 256, t, 6)
                    P.op("scalar", lambda e, t=t, pv=pv: e.copy(out=VB[:, t, :, 0:64], in_=pv[:, 0:256].rearrange("p (h e) -> p h e", h=4)), reads=[T_ps[6]], writes=[T_V[t]])
                for p in range(2):
                    P.op("vector", lambda e, p=p: e.tensor_reduce(out=kmf[:, p, :], in_=KB[:, p, :].rearrange("p (n k) -> p n k", n=8), axis=AX.X, op=ALU.add), reads=T_Q, writes=[T_KM])
                P.op("vector", lambda e: e.tensor_scalar(out=KM[:, :, :], in0=kmf[:, :, :], scalar1=1.0 / 256, scalar2=None, op0=ALU.mult), reads=[T_KM], writes=[T_KM])
                P.barrier()
                P.flush()
            with contextlib.ExitStack() as es2:
                PT = [sbuf(es2, "b_pt%d" % k, [128, 2, 256], BF16) for k in range(3)]
                T_PT = [Trk() for _ in range(3)]
                gate = sbuf(es2, "b_gate", [128, 2, 4, 8], F32)
                top8 = sbuf(es2, "b_top8", [128, 8], F32)
                BM = sbuf(es2, "b_bm", [128, 2, 4, 8], F32)
                acc = sbuf(es2, "b_acc", [128, 2, 4, 65], F32)
                ob = sbuf(es2, "b_ob", [128, 2, 256], BF16)
                rc = sbuf(es2, "b_rc", [128, 2, 4], F32)
                T_g, T_ob = Trk(), Trk()
                T_acc = [Trk() for _ in range(4)]
                itb = [0]
                for j in range(8):
                    tgq = j // 2
                    if j > 0:
                        for par in range(2):
                            pg = PSB[6]
                            pgv = pg[:, 0:64].rearrange("p (a h n) -> p a h n", a=2, h=4)
                            P.group("tensor", [(lambda e, a=a, h=h: e.matmul(pgv[:, a, h, :], lhsT=QB[(h % 2) * 64:(h % 2) * 64 + 64, h // 2, (2 * j + a) * 128:(2 * j + a + 1) * 128],
                                                                              rhs=KM[(h % 2) * 64:(h % 2) * 64 + 64, h // 2, :], start=True, stop=True)) for a in range(2) for h in (par, par + 2)],
                                    reads=[T_Q[tgq], T_KM], writes=[T_ps[6]])
                            for h in (par, par + 2):
                                P.op("vector", lambda e: e.tensor_copy(out=gate[:, :, h, :], in_=pgv[:, :, h, :]), reads=[T_ps[6]], writes=[T_g])
                        P.op("vector", lambda e: e.memset(gate[:, :, :, j:8], NEG), reads=[T_g], writes=[T_g])
                        for a in range(2):
                            for h in range(4):
                                P.op("vector", lambda e: e.max(out=top8[:, :], in_=gate[:, a, h, :]), reads=[T_g], writes=[T_g])
                                P.op("vector", lambda e: e.tensor_scalar(out=BM[:, a, h, :], in0=gate[:, a, h, :], scalar1=top8[:, 2:3], scalar2=None, op0=ALU.is_ge), reads=[T_g], writes=[T_g])
                    pend = []

                    def stage_a(h, n):
                        hp = (h % 2) * 64
                        pp = h // 2
                        qs_all = slice((2 * j) * 128, (2 * j + 2) * 128)
                        k = itb[0] % 3
                        itb[0] += 1
                        ps_s = PSB[k]
                        psv = ps_s[:, 0:512].rearrange("p (c q) -> p c q", c=2)
                        nb = j if n is None else n
                        P.group("tensor", [(lambda e, c=c: e.matmul(psv[:, c, :], lhsT=KB[hp:hp + 64, pp, (2 * nb + c) * 128:(2 * nb + c + 1) * 128], rhs=QB[hp:hp + 64, pp, qs_all], start=True, stop=True)) for c in range(2)],
                                reads=[T_Q[tgq], T_Q[nb // 2]], writes=[T_ps[k]])
                        P.op("scalar", lambda e: e.activation(out=PT[k][:, :, :], in_=psv, func=AF.Exp, scale=0.125), reads=[T_ps[k]], writes=[T_PT[k]])
                        if n is None:
                            P.op("vector", lambda e: e.tensor_tensor(out=PT[k][:, 0, 0:128], in0=PT[k][:, 0, 0:128], in1=trib[:], op=ALU.mult), reads=[T_PT[k], T_cst], writes=[T_PT[k]])
                            P.op("vector", lambda e: e.tensor_tensor(out=PT[k][:, 1, 128:256], in0=PT[k][:, 1, 128:256], in1=trib[:], op=ALU.mult), reads=[T_PT[k], T_cst], writes=[T_PT[k]])
                        pend.append((h, n, k))

                    def stage_b():
                        h, n, k = pend.pop(0)
                        po = PSB[3 + k]
                        pov = po[:, 0:130].rearrange("p (a e) -> p a e", a=2)
                        if n is None:
                            P.group("tensor", [lambda e: e.matmul(pov[:, 0, :], lhsT=PT[k][:, 0, 0:128], rhs=VB[:, 2 * j, h, :], start=True, stop=True),
                                               lambda e: e.matmul(pov[:, 1, :], lhsT=PT[k][:, 0, 128:256], rhs=VB[:, 2 * j, h, :], start=True, stop=False),
                                               lambda e: e.matmul(pov[:, 1, :], lhsT=PT[k][:, 1, 128:256], rhs=VB[:, 2 * j + 1, h, :], start=False, stop=True)],
                                    reads=[T_PT[k], T_V[2 * j], T_V[2 * j + 1]], writes=[T_ps[3 + k]])
                            P.op("vector", lambda e: e.tensor_copy(out=acc[:, :, h, :], in_=pov), reads=[T_ps[3 + k]], writes=[T_acc[h]])
                        else:
                            P.group("tensor", [(lambda e, c=c, a=a: e.matmul(pov[:, a, :], lhsT=PT[k][:, c, a * 128:(a + 1) * 128], rhs=VB[:, 2 * n + c, h, :], start=(c == 0), stop=(c == 1))) for a in range(2) for c in range(2)],
                                    reads=[T_PT[k], T_V[2 * n], T_V[2 * n + 1]], writes=[T_ps[3 + k]])
                            for a in range(2):
                                P.op("vector", lambda e: e.scalar_tensor_tensor(out=acc[:, a, h, :], in0=pov[:, a, :], scalar=BM[:, a, h, n:n + 1], in1=acc[:, a, h, :], op0=ALU.mult, op1=ALU.add),
                                     reads=[T_ps[3 + k], T_g, T_acc[h]], writes=[T_acc[h]])

                    for h in range(4):
                        for n in [None] + list(range(j)):
                            stage_a(h, n)
                            if len(pend) > 2:
                                stage_b()
                    while pend:
                        stage_b()
                    P.op("vector", lambda e: e.reciprocal(out=rc[:, :, :], in_=acc[:, :, :, 64]), reads=T_acc, writes=[T_g])
                    for a in range(2):
                        P.op("vector", lambda e, a=a: e.tensor_tensor(out=ob[:, a, :].rearrange("p (h e) -> p h e", h=4), in0=acc[:, a, :, 0:64], in1=rc[:, a, :].unsqueeze(2).to_broadcast([128, 4, 64]), op=ALU.mult),
                             reads=T_acc + [T_g], writes=[T_ob])
                        transpose_out(ob[:, a, :], T_ob, 256, OT, T_OT, 2, 2 * j + a)
                P.barrier()
                P.flush()

    def mixer_C(l, HT, T_HT, OT, T_OT, half):
        with contextlib.ExitStack() as es:
            QC = sbuf(es, "c_q", [128, 2, S], BF16)
            KC = sbuf(es, "c_k", [128, 2, S], BF16)
            VC = sbuf(es, "c_v", [128, NT, 2, 129], BF16)
            T_Q = [Trk() for _ in range(4)]
            T_V = [Trk() for _ in range(NT)]
            with contextlib.ExitStack() as es1:
                W = sbuf(es1, "c_w", [128, 8, 512], BF16)
                Wv = sbuf(es1, "c_wv", [128, 8, 256], BF16)
                T_W, T_Wv = [], []
                proj_tmp(es1)
                load_w_in(W, T_W, l, [(0, OFF["qc"] + half * 256, 256), (256, OFF["kc"] + half * 256, 256)])
                load_w_in(Wv, T_Wv, l, [(0, OFF["vc"] + half * 256, 256)])
                P.op("vector", lambda e: e.memset(VC[:, :, :, 128:129], 1.0), writes=T_V)
                gq = qkgT[:, l * 6 + 4:l * 6 + 5]
                gk = qkgT[:, l * 6 + 5:l * 6 + 6]
                for tg in range(4):
                    tsl = slice(tg * 512, (tg + 1) * 512)
                    for p in range(2):
                        proj_fm(es1, HT, T_HT, W, T_W, p * 128, tg, QC[:, p, tsl], T_Q[tg], "norm", gq)
                        proj_fm(es1, HT, T_HT, W, T_W, 256 + p * 128, tg, KC[:, p, tsl], T_Q[tg], "norm", gk)
                proj_flush()
                for t in range(NT):
                    pv = proj_tm(HT, T_HT, Wv, T_Wv, 256, t, 6)
                    P.op("scalar", lambda e, t=t, pv=pv: e.copy(out=VC[:, t, :, 0:128], in_=pv[:, 0:256].rearrange("p (h e) -> p h e", h=2)), reads=[T_ps[6]], writes=[T_V[t]])
                P.barrier()
                P.flush()
            with contextlib.ExitStack() as es2:
                PT = [sbuf(es2, "c_pt%d" % k, [128, NT, 512], BF16) for k in range(2)]
                T_PTk = [[Trk() for _ in range(NT)] for _ in range(2)]
                t0 = sbuf(es2, "c_t0", [128, 4, 128], F32)
                o32 = sbuf(es2, "c_o32", [128, 2, 4, 128], F32)
                oc = sbuf(es2, "c_oc", [128, 4, 256], BF16)
                sq = sbuf(es2, "c_sq", [128, 128], F32)
                st = sbuf(es2, "c_st", [128, 32], F32)
                T_t0, T_o32, T_oc, T_st, T_ss = Trk(), Trk(), Trk(), Trk(), Trk()
                itc = [0, 0]
                units = [(G, hh, c) for G in range(4) for hh in range(2) for c in range(2)]

                def st_gen(u):
                    G, hh, c = units[u]
                    pbf = u % 2
                    cp = c * 64
                    nkt = 4 * G + 4
                    for kt in range(nkt):
                        k = itc[0] % 3
                        itc[0] += 1
                        ps_s = PSB[k]
                        qs0 = max(kt - 4 * G, 0)
                        q0 = (4 * G + qs0) * 128
                        nq = (4 - qs0) * 128
                        P.group("tensor", [lambda e: e.matmul(ps_s[:, 0:nq], lhsT=KC[cp:cp + 64, hh, kt * 128:(kt + 1) * 128], rhs=QC[cp:cp + 64, hh, q0:q0 + nq], start=True, stop=True)],
                                reads=[T_Q[G], T_Q[kt // 4]], writes=[T_ps[k]])
                        P.op("scalar", lambda e: e.activation(out=PT[pbf][:, kt, qs0 * 128:qs0 * 128 + nq], in_=ps_s[:, 0:nq], func=AF.Exp, scale=0.125), reads=[T_ps[k]], writes=[T_PTk[pbf][kt]])
                        if kt >= 4 * G:
                            P.op("vector", lambda e: e.tensor_tensor(out=PT[pbf][:, kt, qs0 * 128:(qs0 + 1) * 128], in0=PT[pbf][:, kt, qs0 * 128:(qs0 + 1) * 128], in1=trib[:], op=ALU.mult),
                                 reads=[T_PTk[pbf][kt], T_cst], writes=[T_PTk[pbf][kt]])
                        yield

                def pv_gen(u):
                    G, hh, c = units[u]
                    pbf = u % 2
                    for qs in range(4):
                        pb = 3 + (itc[1] % 3)
                        itc[1] += 1
                        po = PSB[pb]
                        nk = 4 * G + qs + 1
                        P.group("tensor", [(lambda e, kt=kt: e.matmul(po[:, 0:129], lhsT=PT[pbf][:, kt, qs * 128:(qs + 1) * 128], rhs=VC[:, kt, hh, :], start=(kt == 0), stop=(kt == nk - 1))) for kt in range(nk)],
                                reads=T_PTk[pbf][0:nk] + T_V[0:nk], writes=[T_ps[pb]])
                        P.op("vector", lambda e: e.reciprocal(out=st[:, qs * 2 + c:qs * 2 + c + 1], in_=po[:, 128:129]), reads=[T_ps[pb]], writes=[T_st])
                        if c == 0:
                            P.op("vector", lambda e: e.tensor_scalar(out=t0[:, qs, :], in0=po[:, 0:128], scalar1=st[:, qs * 2:qs * 2 + 1], scalar2=None, op0=ALU.mult),
                                 reads=[T_ps[pb], T_st], writes=[T_t0])
                        else:
                            P.op("vector", lambda e: e.tensor_tensor(out=st[:, 8 + qs:9 + qs], in0=st[:, qs * 2 + 1:qs * 2 + 2], in1=lamv[:, l:l + 1], op=ALU.mult), reads=[T_st, T_lam], writes=[T_st])
                            P.op("vector", lambda e: e.scalar_tensor_tensor(out=o32[:, hh, qs, :], in0=po[:, 0:128], scalar=st[:, 8 + qs:9 + qs], in1=t0[:, qs, :], op0=ALU.mult, op1=ALU.add),
                                 reads=[T_ps[pb], T_st, T_t0], writes=[T_o32])
                            P.op("vector", lambda e: e.tensor_tensor(out=sq[:], in0=o32[:, hh, qs, :], in1=o32[:, hh, qs, :], op=ALU.mult), reads=[T_o32], writes=[T_st])
                            P.op("vector", lambda e: e.reduce_sum(out=st[:, 16 + hh * 4 + qs:17 + hh * 4 + qs], in_=sq[:], axis=AX.X), reads=[T_st], writes=[T_st, T_ss])
                        yield
                    if hh == 1 and c == 1:
                        P.op("scalar", lambda e: e.activation(out=st[:, 24:32], in_=st[:, 16:24], func=AF.Sqrt, scale=1.0 / 128, bias=EPS), reads=[T_ss, T_st], writes=[T_ss])
                        P.op("vector", lambda e: e.reciprocal(out=st[:, 24:32], in_=st[:, 24:32]), reads=[T_ss], writes=[T_ss])
                        P.op("vector", lambda e: e.tensor_scalar(out=st[:, 24:32], in0=st[:, 24:32], scalar1=float(1.0 - lam_init(l)), scalar2=None, op0=ALU.mult), reads=[T_ss], writes=[T_ss])
                        for h2 in range(2):
                            for qs in range(4):
                                P.op("vector", lambda e: e.scalar_tensor_tensor(out=oc[:, qs, h2 * 128:(h2 + 1) * 128], in0=o32[:, h2, qs, :], scalar=st[:, 24 + h2 * 4 + qs:25 + h2 * 4 + qs],
                                                                              in1=subg[:, l * 128:(l + 1) * 128], op0=ALU.mult, op1=ALU.mult),
                                     reads=[T_o32, T_ss, T_cst], writes=[T_oc])
                        yield
                        for qs in range(4):
                            transpose_out(oc[:, qs, :], T_oc, 256, OT, T_OT, 4 + 2 * half, 4 * G + qs)
                        yield

                for _ in st_gen(0):
                    pass
                for u in range(len(units)):
                    ga = pv_gen(u)
                    gb = st_gen(u + 1) if u + 1 < len(units) else iter(())
                    nb = (4 * units[u + 1][0] + 4) if u + 1 < len(units) else 0
                    per = max(1, (nb + 3) // 4)
                    da = db = False
                    while not (da and db):
                        if not db:
                            for _ in range(per):
                                try:
                                    next(gb)
                                except StopIteration:
                                    db = True
                                    break
                        if not da:
                            try:
                                next(ga)
                            except StopIteration:
                                da = True
                P.barrier()
                P.flush()

    def mixer_out(l, HT, T_HT, OT, T_OT):
        with contextlib.ExitStack() as es:
            MG = sbuf(es, "o_mg", [128, 8, S], BF16)
            T_MG = [Trk() for _ in range(4)]
            es_a = contextlib.ExitStack()
            WBR = [sbuf(es_a, "o_wbr%d" % k, [128, 8, 128], BF16) for k in range(2)]
            WGT = [sbuf(es_a, "o_wgt%d" % k, [128, 8, 384], BF16) for k in range(2)]
            sg = [sbuf(es_a, "o_sg%d" % k, [128, 512], F32) for k in range(2)]
            mg32 = [sbuf(es_a, "o_m32%d" % k, [128, 512], F32) for k in range(2)]
            T_Wb = [Trk(), Trk()]
            T_Wg = [[Trk() for _ in range(3)] for _ in range(2)]
            T_sg = [Trk(), Trk()]
            T_m32 = [Trk(), Trk()]
            wbv = wbr_d[l].rearrange("(c p) f -> p c f", p=128)
            wiv = win_d[l].rearrange("(c p) f -> p c f", p=128)
            feat = [(0, 2), (2, 2), (4, 4)]
            it = 0
            for dc in range(8):
                wb = dc % 2
                P.dma("gpsimd", WBR[wb][:, :, :], wbv[:, :, dc * 128:(dc + 1) * 128], writes=[T_Wb[wb]])
                for br, nm in enumerate(("ga", "gb", "gc")):
                    P.dma("gpsimd", WGT[wb][:, :, br * 128:(br + 1) * 128], wiv[:, :, OFF[nm] + dc * 128:OFF[nm] + (dc + 1) * 128], writes=[T_Wg[wb][br]])
                for tg in range(4):
                    tsl = slice(tg * 512, (tg + 1) * 512)
                    mk = it % 2
                    it += 1
                    for br in range(3):
                        k = (it + br) % 2
                        pgt = PSB[k]
                        py = PSB[2 + k]
                        c0, ncn = feat[br]
                        P.group("tensor", [(lambda e, c=c, br=br, pgt=pgt, wb=wb: e.matmul(pgt[:, :], lhsT=WGT[wb][:, c, br * 128:(br + 1) * 128], rhs=HT[:, c, tsl], start=(c == 0), stop=(c == 7))) for c in range(8)],
                                reads=[T_Wg[wb][br]] + T_HT[tg * 4:tg * 4 + 4], writes=[T_ps[k]])
                        P.group("tensor", [(lambda e, c=c, c0=c0, ncn=ncn, py=py, wb=wb: e.matmul(py[:, :], lhsT=WBR[wb][:, c0 + c, :], rhs=OT[:, c0 + c, tsl], start=(c == 0), stop=(c == ncn - 1))) for c in range(ncn)],
                                reads=[T_Wb[wb]] + T_OT[tg * 4:tg * 4 + 4], writes=[T_ps[2 + k]])
                        P.op("scalar", lambda e, k=k, pgt=pgt: e.activation(out=sg[k][:], in_=pgt[:, :], func=AF.Sigmoid), reads=[T_ps[k]], writes=[T_sg[k]])
                        if br == 0:
                            P.op("vector", lambda e, k=k, py=py, mk=mk: e.tensor_tensor(out=mg32[mk][:], in0=py[:, :], in1=sg[k][:], op=ALU.mult), reads=[T_ps[2 + k], T_sg[k]], writes=[T_m32[mk]])
                        else:
                            P.op("vector", lambda e, k=k, py=py: e.tensor_tensor(out=sg[k][:], in0=py[:, :], in1=sg[k][:], op=ALU.mult), reads=[T_ps[2 + k], T_sg[k]], writes=[T_sg[k]])
                            if br == 1:
                                P.op("gpsimd", lambda e, k=k, mk=mk: e.tensor_tensor(out=mg32[mk][:], in0=mg32[mk][:], in1=sg[k][:], op=ALU.add), reads=[T_sg[k], T_m32[mk]], writes=[T_m32[mk]])
                            else:
                                P.op("gpsimd", lambda e, k=k, mk=mk, dc=dc: e.tensor_tensor(out=MG[:, dc, tsl], in0=mg32[mk][:], in1=sg[k][:], op=ALU.add), reads=[T_sg[k], T_m32[mk]], writes=[T_MG[tg]])
            P.barrier()
            P.flush()
            es_a.close()
            WO = sbuf(es, "o_wo", [128, 8, D], BF16)
            T_WO = Trk()
            wov = wout_d[l].rearrange("(c p) f -> p c f", p=128)
            for hf in range(2):
                P.dma("gpsimd", WO[:, :, hf * 512:(hf + 1) * 512], wov[:, :, hf * 512:(hf + 1) * 512], writes=[T_WO])
            for t in range(NT):
                for hf in range(2):
                    pb = 4 + (t * 2 + hf) % 2
                    pd = PSB[pb]
                    P.group("tensor", [(lambda e, c=c, t=t, hf=hf, pd=pd: e.matmul(pd[:, :], lhsT=MG[:, c, t * 128:(t + 1) * 128], rhs=WO[:, c, hf * 512:(hf + 1) * 512], start=(c == 0), stop=(c == 7))) for c in range(8)],
                            reads=[T_MG[t // 4], T_WO], writes=[T_ps[pb]])
                    P.op("vector", lambda e, t=t, hf=hf, pd=pd: e.tensor_tensor(out=X[:, t, hf * 512:(hf + 1) * 512], in0=pd[:, :], in1=X[:, t, hf * 512:(hf + 1) * 512], op=ALU.add),
                         reads=[T_ps[pb], T_X[t]], writes=[T_X[t]])
            P.barrier()
            P.flush()

    def mixer(l, b):
        with contextlib.ExitStack() as es:
            HT = sbuf(es, "m_HT", [128, 8, S], BF16)
            OT = sbuf(es, "m_OT", [128, 8, S], BF16)
            T_HT = [Trk() for _ in range(NT)]
            T_OT = [Trk() for _ in range(NT)]
            with contextlib.ExitStack() as esn:
                norm_T(esn, l, 1, HT, T_HT)
                P.barrier()
                P.flush()
            if stage >= 2:
                mixer_A(l, HT, T_HT, OT, T_OT)
            if stage >= 3:
                mixer_B(l, HT, T_HT, OT, T_OT)
            if stage >= 4:
                mixer_C(l, HT, T_HT, OT, T_OT, 0)
                mixer_C(l, HT, T_HT, OT, T_OT, 1)
            if dbg and l == layers[0] and b == 0:
                nch = {2: 2, 3: 4}.get(stage, 8)
                P.dma("sync", dbg_ot[:, 0:nch, :], OT[:, 0:nch, :], reads=T_OT, writes=[T_out])
            if stage >= 5:
                mixer_out(l, HT, T_HT, OT, T_OT)
            P.barrier()
            P.flush()

    for b in range(n_seq):
        for t in range(NT):
            P.dma("sync", X[:, t, :], x_d[b, t * 128:(t + 1) * 128, :], writes=[T_X[t]])
        rope_tables(b)
        for l in layers:
            if stage >= 1:
                ffn(l, 0)
            if stage >= 2:
                mixer(l, b)
            if stage >= 6:
                ffn(l, 1)
        for t in range(NT):
            P.dma("sync", out_d[b, t * 128:(t + 1) * 128, :], X[:, t, :], reads=[T_X[t]], writes=[Trk()])
    P.barrier()
    P.flush()
    es0.close()
    P.close()
    build.nins = P.nins
    return nc


def prep_shared(inputs):
    norm_g = np.asarray(inputs["norm_g"], np.float32)
    qk = np.asarray(inputs["qk_norm_g"], np.float32)
    ngT = np.ascontiguousarray(norm_g.reshape(2, 3, 8, 128).transpose(3, 0, 1, 2).reshape(128, 48))
    qkT = qk.reshape(12, 64).T
    qkgT = np.ascontiguousarray(np.concatenate([qkT, qkT], axis=0))
    return {
        "w_in": np.ascontiguousarray(inputs["w_in"], np.float32),
        "w_branch": np.ascontiguousarray(inputs["w_branch"], np.float32),
        "w_out": np.ascontiguousarray(inputs["w_out"], np.float32),
        "ffn_w_gate": np.ascontiguousarray(inputs["ffn_w_gate"], np.float32),
        "ffn_w_up": np.ascontiguousarray(inputs["ffn_w_up"], np.float32),
        "ffn_w_down": np.ascontiguousarray(inputs["ffn_w_down"], np.float32),
        "cst": make_consts(),
        "ngT": ngT,
        "qkgT": qkgT,
        "lamp": np.ascontiguousarray(np.asarray(inputs["lambda_params"], np.float32).reshape(512)),
        "subg": np.ascontiguousarray(np.asarray(inputs["diff_subln_g"], np.float32).reshape(256)),
    }


def kernel(**inputs):
    x = np.asarray(inputs["x"], np.float32)
    pos = np.asarray(inputs["positions"], np.int32)
    shared = prep_shared(inputs)
    nc = build(n_seq=2, layers=(0, 1))
    in_maps = []
    for c in range(NCORES):
        m = dict(shared)
        m["x"] = np.ascontiguousarray(x[2 * c:2 * c + 2])
        m["pos"] = np.ascontiguousarray(pos[2 * c:2 * c + 2])
        in_maps.append(m)
    res = run_bass_kernel_spmd(nc, in_maps, core_ids=list(range(NCORES)))
    out = np.concatenate([np.asarray(r["out"]) for r in res.results], axis=0)
    return out.astype(np.float32)
```

```python
import contextlib
import math
import numpy as np
import concourse.bass as bass
import concourse.mybir as mybir
from concourse.bass_utils import run_bass_kernel_spmd

F32 = mybir.dt.float32
BF16 = mybir.dt.bfloat16
I32 = mybir.dt.int32
AF = mybir.ActivationFunctionType
ALU = mybir.AluOpType
AX = mybir.AxisListType

S = 2048
D = 1024
NT = 16
DFF = 2816
EPS = 1e-6
NCORES = 8
OFF = dict(qa=0, ka=256, va=320, qi=384, ki=896, wi=960, qb=968, kb=1224, vb=1480,
           qc=1736, kc=2248, vc=2760, ga=3272, gb=4296, gc=5320)
NBIS = 10
C_ID, C_BD, C_RM, C_TRI, C_INVF, C_POW = 0, 128, 256, 384, 512, 513
NCST = C_POW + NBIS
NEG = -1.0e30
import os
SUB = int(os.environ.get('SUB', '9'))
PI = math.pi


class Trk:
    __slots__ = ("name", "w", "r")

    def __init__(self, name=""):
        self.name = name
        self.w = None
        self.r = []


class _Rec:
    def __init__(self):
        self.calls = []

    def __getattr__(self, name):
        def f(*a, **k):
            self.calls.append((name, a, k))
            return self
        return f


class Prog:
    ENG = ("tensor", "vector", "scalar", "gpsimd", "sync")

    def __init__(self, nc, n_dma_sems=24):
        self.nc = nc
        self.stack = []
        self.ops = {e: [] for e in self.ENG}
        self.sems = {}
        self.cnt = {}
        self.wm = {e: {} for e in self.ENG}
        for e in self.ENG:
            self._newsem(e)
        self.dma_keys = {}
        self.dma_rr = {}
        for q in ("sync", "gpsimd", "scalar"):
            self.dma_keys[q] = []
            self.dma_rr[q] = 0
            for i in range(n_dma_sems if q != "scalar" else 4):
                k = "dma_%s%d" % (q, i)
                self._newsem(k)
                self.dma_keys[q].append(k)
        self.nins = 0
        self.fill_reg = None

    def _newsem(self, key):
        cm = self.nc.semaphore(key)
        h = cm.__enter__()
        self.stack.append(cm)
        self.sems[key] = h
        self.cnt[key] = 0

    def _waits(self, eng, reads, writes):
        need = {}
        for t in reads:
            if t.w is not None:
                k, v = t.w
                if need.get(k, 0) < v:
                    need[k] = v
        for t in writes:
            if t.w is not None:
                k, v = t.w
                if need.get(k, 0) < v:
                    need[k] = v
            for (k, v) in t.r:
                if need.get(k, 0) < v:
                    need[k] = v
        out = []
        wm = self.wm[eng]
        for k, v in need.items():
            if wm.get(k, 0) < v:
                wm[k] = v
                out.append((k, v))
        return out

    def _mark(self, dep, reads, writes):
        for t in reads:
            t.r.append(dep)
        for t in writes:
            t.w = dep
            t.r = []

    def op(self, eng, fn, reads=(), writes=()):
        return self.group(eng, [fn], reads, writes)

    def group(self, eng, fns, reads=(), writes=()):
        waits = self._waits(eng, reads, writes)
        self.cnt[eng] += 1
        dep = (eng, self.cnt[eng])
        self._mark(dep, reads, writes)
        sem = self.sems[eng]
        sems = self.sems
        self.nins += len(fns)
        rec = _Rec()
        for f in fns:
            f(rec)
        calls = rec.calls

        def run(e, calls=calls, waits=waits, sem=sem):
            for (k, val) in waits:
                e.wait_ge(sems[k], val)
            last = None
            for (name, a, kw) in calls:
                if name == "affine_select":
                    kw = dict(kw)
                    if self.fill_reg is None:
                        self.fill_reg = e.to_reg(kw["fill"])
                    kw["fill"] = self.fill_reg
                last = getattr(e, name)(*a, **kw)
            last.then_inc(sem, 1)
        self.ops[eng].append(run)
        return dep

    def dma(self, q, out, in_, reads=(), writes=(), **kw):
        waits = self._waits(q, reads, writes)
        k = self.dma_keys[q][self.dma_rr[q] % len(self.dma_keys[q])]
        self.dma_rr[q] += 1
        prev = self.cnt[k]
        if prev > 0 and self.wm[q].get(k, 0) < prev:
            self.wm[q][k] = prev
            waits.append((k, prev))
        self.cnt[k] += 16
        dep = (k, self.cnt[k])
        self._mark(dep, reads, writes)
        sems = self.sems
        sem = sems[k]
        self.nins += 1

        def run(e, waits=waits, sem=sem, out=out, in_=in_, kw=kw):
            for (kk, val) in waits:
                e.wait_ge(sems[kk], val)
            e.dma_start(out=out, in_=in_, **kw).then_inc(sem, 16)
        self.ops[q].append(run)
        return dep

    def barrier(self):
        sems = self.sems
        snap = [(k, v) for k, v in self.cnt.items() if v > 0]
        for eng in self.ENG:
            waits = []
            for (k, v) in snap:
                if k == eng:
                    continue
                if self.wm[eng].get(k, 0) < v:
                    self.wm[eng][k] = v
                    waits.append((k, v))

            def run(e, waits=waits):
                for (k, val) in waits:
                    e.wait_ge(sems[k], val)
            self.ops[eng].append(run)

    def flush(self):
        nc = self.nc
        ops = self.ops
        with nc.Block() as block:
            @block.tensor
            def _(e):
                for f in ops["tensor"]:
                    f(e)

            @block.vector
            def _(e):
                for f in ops["vector"]:
                    f(e)

            @block.scalar
            def _(e):
                for f in ops["scalar"]:
                    f(e)

            @block.gpsimd
            def _(e):
                for f in ops["gpsimd"]:
                    f(e)

            @block.sync
            def _(e):
                for f in ops["sync"]:
                    f(e)
        self.ops = {e: [] for e in self.ENG}

    def close(self):
        for cm in reversed(self.stack):
            cm.__exit__(None, None, None)


def make_consts():
    c = np.zeros((128, NCST), np.float32)
    c[:, C_ID:C_ID + 128] = np.eye(128, dtype=np.float32)
    for p in range(128):
        for f in range(128):
            if p // 64 == f // 64:
                c[p, C_BD + f] = 1.0
    for f in range(128):
        r = f % 64
        if r < 8:
            c[f + 8, C_RM + f] = -1.0
        elif r < 16:
            c[f - 8, C_RM + f] = 1.0
    for k in range(128):
        c[k, C_TRI + k:C_TRI + 128] = 1.0
    inv = np.power(np.float32(500000.0), -np.arange(0, 16, 2, dtype=np.float32) / np.float32(16.0)).astype(np.float32)
    for p in range(128):
        r = p % 64
        c[p, C_INVF] = inv[r % 8] if r < 16 else 0.0
    for i in range(NBIS):
        c[:, C_POW + i] = 3.0 ** (-(i + 1))
    return c


def build(n_seq=2, layers=(0, 1), stage=99, dbg=False):
    nc = bass.Bass("TRN2", target_bir_lowering=False)
    dt = nc.dram_tensor
    x_d = dt("x", [n_seq, S, D], F32, kind="ExternalInput").ap()
    pos_d = dt("pos", [n_seq, S], I32, kind="ExternalInput").ap()
    win_d = dt("w_in", [2, D, 6344], F32, kind="ExternalInput").ap()
    wbr_d = dt("w_branch", [2, D, D], F32, kind="ExternalInput").ap()
    wout_d = dt("w_out", [2, D, D], F32, kind="ExternalInput").ap()
    wg_d = dt("ffn_w_gate", [2, 2, D, DFF], F32, kind="ExternalInput").ap()
    wu_d = dt("ffn_w_up", [2, 2, D, DFF], F32, kind="ExternalInput").ap()
    wd_d = dt("ffn_w_down", [2, 2, DFF, D], F32, kind="ExternalInput").ap()
    cst_d = dt("cst", [128, NCST], F32, kind="ExternalInput").ap()
    ngT_d = dt("ngT", [128, 48], F32, kind="ExternalInput").ap()
    qkgT_d = dt("qkgT", [128, 12], F32, kind="ExternalInput").ap()
    lam_d = dt("lamp", [512], F32, kind="ExternalInput").ap()
    sub_d = dt("subg", [256], F32, kind="ExternalInput").ap()
    out_d = dt("out", [n_seq, S, D], F32, kind="ExternalOutput").ap()
    if dbg:
        dbg_ot = dt("dbg_ot", [128, 8, S], BF16, kind="ExternalOutput").ap()

    P = Prog(nc)
    es0 = contextlib.ExitStack()

    uid = [0]

    def sbuf(es, name, shape, dtype):
        uid[0] += 1
        return es.enter_context(nc.sbuf_tensor("s%d_%s" % (uid[0], name), shape, dtype))

    X = sbuf(es0, "X", [128, NT, D], F32)
    tab_d = dt("tabs", [2, 128, S], BF16, kind="ExternalOutput").ap()
    cstf = sbuf(es0, "cstf", [128, NCST], F32)
    identb = sbuf(es0, "identb", [128, 128], BF16)
    BDb = sbuf(es0, "BDb", [128, 128], BF16)
    RMb = sbuf(es0, "RMb", [128, 128], BF16)
    trib = sbuf(es0, "trib", [128, 128], BF16)
    onesb = sbuf(es0, "onesb", [128, 128], BF16)
    ngT = sbuf(es0, "ngT", [128, 48], F32)
    qkgT = sbuf(es0, "qkgT", [128, 12], F32)
    subg = sbuf(es0, "subg", [128, 256], F32)
    lamv = sbuf(es0, "lamv", [128, 8], F32)
    rs_x = sbuf(es0, "rs_x", [128, 2 * NT], F32)
    PSB = [es0.enter_context(nc.psum_tensor("psb%d" % i, [128, 512], F32)) for i in range(7)]
    PSH = es0.enter_context(nc.psum_tensor("psh", [128, 1024], BF16))
    T_ps = [Trk("ps%d" % i) for i in range(7)]
    T_psh = Trk("psh")
    T_X = [Trk("X%d" % i) for i in range(NT)]
    T_cst = Trk("cst")
    T_tab = Trk("tab")
    T_out = Trk("out")
    T_lam = Trk("lam")

    def lam_init(l):
        return 0.8 - 0.6 * math.exp(-0.3 * l)

    P.dma("sync", cstf[:], cst_d, writes=[T_cst])
    P.dma("gpsimd", identb[:], cst_d[:, C_ID:C_ID + 128], writes=[T_cst])
    P.dma("gpsimd", BDb[:], cst_d[:, C_BD:C_BD + 128], writes=[T_cst])
    P.dma("gpsimd", RMb[:], cst_d[:, C_RM:C_RM + 128], writes=[T_cst])
    P.dma("gpsimd", trib[:], cst_d[:, C_TRI:C_TRI + 128], writes=[T_cst])
    P.dma("sync", ngT[:], ngT_d, writes=[T_cst])
    P.dma("sync", qkgT[:], qkgT_d, writes=[T_cst])
    P.dma("sync", subg[:], sub_d.partition_broadcast(128), writes=[T_cst])
    P.op("vector", lambda e: e.memset(onesb[:], 1.0), writes=[T_cst])
    with contextlib.ExitStack() as es:
        lamp = sbuf(es, "lamp", [128, 512], F32)
        tmp = sbuf(es, "lamtmp", [128, 512], F32)
        sums = sbuf(es, "lamsum", [128, 8], F32)
        T_t = Trk()
        P.dma("sync", lamp[:], lam_d.partition_broadcast(128), writes=[T_lam])
        for l in range(2):
            for j in range(2):
                a0 = l * 256 + (2 * j) * 64
                P.op("vector", lambda e, a0=a0: e.tensor_tensor(out=tmp[:, a0:a0 + 64], in0=lamp[:, a0:a0 + 64], in1=lamp[:, a0 + 64:a0 + 128], op=ALU.mult),
                     reads=[T_lam], writes=[T_t])
                P.op("vector", lambda e, a0=a0, l=l, j=j: e.reduce_sum(out=sums[:, 2 * l + j:2 * l + j + 1], in_=tmp[:, a0:a0 + 64], axis=AX.X),
                     reads=[T_t], writes=[T_t])
        P.op("scalar", lambda e: e.activation(out=sums[:, 4:8], in_=sums[:, 0:4], func=AF.Exp), reads=[T_t], writes=[T_t])
        for l in range(2):
            P.op("vector", lambda e, l=l: e.scalar_tensor_tensor(out=lamv[:, l:l + 1], in0=sums[:, 5 + 2 * l:6 + 2 * l], scalar=-lam_init(l),
                                                               in1=sums[:, 4 + 2 * l:5 + 2 * l], op0=ALU.add, op1=ALU.subtract),
                 reads=[T_t], writes=[T_lam])
        P.barrier()
        P.flush()

    def rope_tables(b):
        with contextlib.ExitStack() as es:
            CT = sbuf(es, "CT", [128, S], BF16)
            STb = sbuf(es, "ST", [128, S], BF16)
            posi = sbuf(es, "posi", [128, S], I32)
            a = sbuf(es, "ta", [128, S], F32)
            r = sbuf(es, "tr", [128, S], F32)
            ki = sbuf(es, "tki", [128, S], I32)
            kf = sbuf(es, "tkf", [128, S], F32)
            T = Trk()
            P.dma("sync", posi[:], pos_d[b].partition_broadcast(128), writes=[T])
            V = lambda fn: P.op("vector", fn, reads=[T, T_cst], writes=[T, T_tab])
            V(lambda e: e.tensor_copy(out=a[:], in_=posi[:]))
            V(lambda e: e.tensor_scalar(out=a[:], in0=a[:], scalar1=cstf[:, C_INVF:C_INVF + 1], scalar2=None, op0=ALU.mult))
            V(lambda e: e.tensor_scalar(out=r[:], in0=a[:], scalar1=float(1.0 / (2 * PI)), scalar2=None, op0=ALU.mult))
            V(lambda e: e.tensor_copy(out=ki[:], in_=r[:]))
            V(lambda e: e.tensor_copy(out=kf[:], in_=ki[:]))
            V(lambda e: e.scalar_tensor_tensor(out=r[:], in0=kf[:], scalar=-6.28125, in1=a[:], op0=ALU.mult, op1=ALU.add))
            V(lambda e: e.scalar_tensor_tensor(out=r[:], in0=kf[:], scalar=-(2 * PI - 6.28125), in1=r[:], op0=ALU.mult, op1=ALU.add))

            def wrap(t):
                V(lambda e: e.tensor_scalar(out=kf[:], in0=t[:], scalar1=PI, scalar2=-2 * PI, op0=ALU.is_gt, op1=ALU.mult))
                V(lambda e: e.tensor_tensor(out=t[:], in0=t[:], in1=kf[:], op=ALU.add))
                V(lambda e: e.tensor_scalar(out=kf[:], in0=t[:], scalar1=-PI, scalar2=2 * PI, op0=ALU.is_lt, op1=ALU.mult))
                V(lambda e: e.tensor_tensor(out=t[:], in0=t[:], in1=kf[:], op=ALU.add))
                V(lambda e: e.tensor_scalar(out=t[:], in0=t[:], scalar1=3.1415925, scalar2=-3.1415925, op0=ALU.min, op1=ALU.max))
            wrap(r)
            P.op("scalar", lambda e: e.activation(out=STb[:], in_=r[:], func=AF.Sin), reads=[T], writes=[T_tab, T])
            V(lambda e: e.tensor_scalar(out=a[:], in0=r[:], scalar1=float(PI / 2), scalar2=None, op0=ALU.add))
            wrap(a)
            P.op("scalar", lambda e: e.activation(out=CT[:], in_=a[:], func=AF.Sin), reads=[T], writes=[T_tab, T])
            P.dma("sync", tab_d[0], CT[:], reads=[T_tab], writes=[Trk()])
            P.dma("sync", tab_d[1], STb[:], reads=[T_tab], writes=[Trk()])
            P.barrier()
            P.flush()

    def norm_T(es, l, i, HT, T_HT):
        xsq = sbuf(es, "n_xsq", [128, D], BF16)
        xn = [sbuf(es, "n_xn%d" % k, [128, D], BF16) for k in range(2)]
        T_sq = Trk()
        T_xn = [Trk(), Trk()]
        T_rs = Trk()
        for t in range(NT):
            P.op("scalar", lambda e, t=t: e.activation(out=xsq[:], in_=X[:, t, :], func=AF.Square, accum_out=rs_x[:, t:t + 1]),
                 reads=[T_X[t]], writes=[T_sq, T_rs])
        P.op("scalar", lambda e: e.activation(out=rs_x[:, NT:2 * NT], in_=rs_x[:, 0:NT], func=AF.Sqrt, scale=1.0 / D, bias=EPS),
             reads=[T_rs], writes=[T_rs])
        P.op("vector", lambda e: e.reciprocal(out=rs_x[:, 0:NT], in_=rs_x[:, NT:2 * NT]), reads=[T_rs], writes=[T_rs])
        g0 = (l * 3 + i) * 8
        psT = PSH[:, :].rearrange("p (c t) -> p c t", c=8)
        for t in range(NT):
            k = t % 2
            P.op("vector", lambda e, t=t, k=k: e.tensor_scalar(out=xn[k][:], in0=X[:, t, :], scalar1=rs_x[:, t:t + 1], scalar2=None, op0=ALU.mult),
                 reads=[T_X[t], T_rs], writes=[T_xn[k]])
            P.group("tensor", [(lambda e, c=c, k=k: e.transpose(out=psT[:, c, :], in_=xn[k][:, c * 128:(c + 1) * 128], identity=identb[:])) for c in range(8)],
                    reads=[T_xn[k], T_cst], writes=[T_psh])
            P.op("vector", lambda e, t=t: e.tensor_tensor(out=HT[:, :, t * 128:(t + 1) * 128], in0=psT,
                                                         in1=ngT[:, g0:g0 + 8].unsqueeze(2).to_broadcast([128, 8, 128]), op=ALU.mult),
                 reads=[T_psh, T_cst], writes=[T_HT[t]])

    def ffn(l, i):
        with contextlib.ExitStack() as es:
            HT = sbuf(es, "f_HT", [128, 8, S], BF16)
            T_HT = [Trk() for _ in range(NT)]
            norm_T(es, l, i, HT, T_HT)
            WG = [sbuf(es, "f_wg%d" % k, [128, 8, 512], BF16) for k in range(2)]
            WU = [sbuf(es, "f_wu%d" % k, [128, 8, 512], BF16) for k in range(2)]
            WD = [sbuf(es, "f_wd%d" % k, [128, 4, D], BF16) for k in range(2)]
            AT = [sbuf(es, "f_at%d" % k, [128, 4, 512], BF16) for k in range(2)]
            SG = [sbuf(es, "f_sg%d" % k, [128, 512], F32) for k in range(2)]
            T_WG, T_WU, T_WD = [Trk(), Trk()], [Trk(), Trk()], [Trk(), Trk()]
            T_AT = [Trk(), Trk()]
            T_SG = [Trk(), Trk()]
            wgv = wg_d[l, i].rearrange("(c p) f -> p c f", p=128)
            wuv = wu_d[l, i].rearrange("(c p) f -> p c f", p=128)
            groups = [(f0, min(512, DFF - f0)) for f0 in range(0, DFF, 512)]
            itf = [0]

            def load(gi):
                f0, fw = groups[gi]
                wb = gi % 2
                nfb = fw // 128
                P.dma("gpsimd", WG[wb][:, :, 0:fw], wgv[:, :, f0:f0 + fw], writes=[T_WG[wb]])
                P.dma("gpsimd", WU[wb][:, :, 0:fw], wuv[:, :, f0:f0 + fw], writes=[T_WU[wb]])
                P.dma("gpsimd", WD[wb][:, 0:nfb, :], wd_d[l, i][f0:f0 + fw, :].rearrange("(c p) d -> p c d", p=128), writes=[T_WD[wb]])

            def gate_up(idx, gi, tg):
                f0, fw = groups[gi]
                wb = gi % 2
                ab = idx % 2
                for fb in range(fw // 128):
                    k = itf[0] % 2
                    itf[0] += 1
                    pg, pu = PSB[2 * k], PSB[2 * k + 1]
                    P.group("tensor", [(lambda e, c=c: e.matmul(pg[:, :], lhsT=WG[wb][:, c, fb * 128:(fb + 1) * 128], rhs=HT[:, c, tg * 512:(tg + 1) * 512], start=(c == 0), stop=(c == 7))) for c in range(8)],
                            reads=[T_WG[wb]] + T_HT[tg * 4:tg * 4 + 4], writes=[T_ps[2 * k]])
                    P.group("tensor", [(lambda e, c=c: e.matmul(pu[:, :], lhsT=WU[wb][:, c, fb * 128:(fb + 1) * 128], rhs=HT[:, c, tg * 512:(tg + 1) * 512], start=(c == 0), stop=(c == 7))) for c in range(8)],
                            reads=[T_WU[wb]] + T_HT[tg * 4:tg * 4 + 4], writes=[T_ps[2 * k + 1]])
                    P.op("scalar", lambda e: e.activation(out=SG[k][:], in_=pg[:, :], func=AF.Silu), reads=[T_ps[2 * k]], writes=[T_SG[k]])
                    P.op("vector", lambda e: e.tensor_tensor(out=AT[ab][:, fb, :], in0=pu[:, :], in1=SG[k][:], op=ALU.mult),
                         reads=[T_ps[2 * k + 1], T_SG[k]], writes=[T_AT[ab]])

            def down(idx, gi, tg):
                f0, fw = groups[gi]
                wb = gi % 2
                ab = idx % 2
                nfb = fw // 128
                for tt in range(4):
                    t = tg * 4 + tt
                    for hf in range(2):
                        pb = 4 + (t * 2 + hf) % 2
                        pd = PSB[pb]
                        P.group("tensor", [(lambda e, fb=fb: e.matmul(pd[:, :], lhsT=AT[ab][:, fb, tt * 128:(tt + 1) * 128], rhs=WD[wb][:, fb, hf * 512:(hf + 1) * 512], start=(fb == 0), stop=(fb == nfb - 1))) for fb in range(nfb)],
                                reads=[T_AT[ab], T_WD[wb]], writes=[T_ps[pb]])
                        P.op("vector", lambda e: e.scalar_tensor_tensor(out=X[:, t, hf * 512:(hf + 1) * 512], in0=pd[:, :], scalar=0.5, in1=X[:, t, hf * 512:(hf + 1) * 512], op0=ALU.mult, op1=ALU.add),
                             reads=[T_ps[pb], T_X[t]], writes=[T_X[t]])

            units = [(gi, tg) for gi in range(len(groups)) for tg in range(4)]
            load(0)
            load(1)
            for idx, (gi, tg) in enumerate(units):
                gate_up(idx, gi, tg)
                if idx >= 1:
                    pgi, ptg = units[idx - 1]
                    down(idx - 1, pgi, ptg)
                    if ptg == 3 and pgi + 2 < len(groups):
                        load(pgi + 2)
            down(len(units) - 1, *units[-1])
            P.barrier()
            P.flush()

    def proj_fm(es_tmp, HT, T_HT, W, T_W, col0, tg, dst, T_dst, mode, gcol):
        proj_fm.q.append((HT, T_HT, W, T_W, col0, tg, dst, T_dst, mode, gcol))
    proj_fm.q = []

    def proj_flush():
        q = proj_fm.q
        proj_fm.q = []
        tm = proj_fm.tmp
        CT, STb, T_tl = tm["CT"], tm["ST"], tm["T_tl"]

        def S1(i):
            HT, T_HT, W, T_W, col0, tg, dst, T_dst, mode, gcol = q[i]
            k = i % 2
            pq = PSB[k]
            tsl = slice(tg * 512, (tg + 1) * 512)
            P.group("tensor", [(lambda e, c=c: e.matmul(pq[:, :], lhsT=W[:, c, col0:col0 + 128], rhs=HT[:, c, tsl], start=(c == 0), stop=(c == 7))) for c in range(8)],
                    reads=T_W + T_HT[tg * 4:tg * 4 + 4], writes=[T_ps[k]])
            if mode == "norm":
                P.op("scalar", lambda e: e.activation(out=tm["xsq"][k][:], in_=pq[:, :], func=AF.Square), reads=[T_ps[k]], writes=[tm["T_xsq"][k]])

        def S2(i):
            HT, T_HT, W, T_W, col0, tg, dst, T_dst, mode, gcol = q[i]
            k = i % 2
            pq = PSB[k]
            xn, T_xn = tm["xn"][k], tm["T_xn"][k]
            if mode == "norm":
                xsq, T_xsq, sd, T_sd = tm["xsq"][k], tm["T_xsq"][k], tm["sd"][k], tm["T_sd"][k]
                pss = PSB[2 + k]
                P.group("tensor", [lambda e: e.matmul(pss[:, :], lhsT=BDb[:], rhs=xsq[:], start=True, stop=True)], reads=[T_xsq, T_cst], writes=[T_ps[2 + k]])
                P.op("scalar", lambda e: e.activation(out=sd[:], in_=pss[:, :], func=AF.Sqrt, scale=1.0 / 64, bias=EPS), reads=[T_ps[2 + k]], writes=[T_sd])
                P.op("vector", lambda e: e.reciprocal(out=sd[:], in_=sd[:]), reads=[T_sd], writes=[T_sd])
                P.op("vector", lambda e: e.scalar_tensor_tensor(out=xn[:], in0=pq[:, :], scalar=gcol, in1=sd[:], op0=ALU.mult, op1=ALU.mult),
                     reads=[T_ps[k], T_sd, T_cst], writes=[T_xn])
            else:
                P.op("scalar", lambda e: e.copy(out=xn[:], in_=pq[:, :]), reads=[T_ps[k]], writes=[T_xn])

        def S3(i):
            HT, T_HT, W, T_W, col0, tg, dst, T_dst, mode, gcol = q[i]
            k = i % 2
            tsl = slice(tg * 512, (tg + 1) * 512)
            xn, T_xn = tm["xn"][k], tm["T_xn"][k]
            pr = PSB[4 + k]
            t1, T_t1, t2, T_t2 = tm["t1"][k], tm["T_t1"][k], tm["t2"][k], tm["T_t2"][k]
            P.group("tensor", [lambda e: e.matmul(pr[:, :], lhsT=RMb[:], rhs=xn[:], start=True, stop=True)], reads=[T_xn, T_cst], writes=[T_ps[4 + k]])
            P.op("gpsimd", lambda e: e.tensor_tensor(out=t1[:], in0=xn[:], in1=CT[:, tsl], op=ALU.mult), reads=[T_xn, T_tl], writes=[T_t1])
            P.op("vector", lambda e: e.tensor_tensor(out=t2[:], in0=pr[:, :], in1=STb[:, tsl], op=ALU.mult), reads=[T_ps[4 + k], T_tl], writes=[T_t2])
            P.op("vector", lambda e: e.tensor_tensor(out=dst, in0=t1[:], in1=t2[:], op=ALU.add), reads=[T_t1, T_t2], writes=[T_dst])

        n = len(q)
        if n == 0:
            return
        S1(0)
        for i in range(n):
            S2(i)
            if i + 1 < n:
                S1(i + 1)
            S3(i)

    def proj_tmp(es):
        tm = {}
        for nm, dtp in (("xn", BF16), ("xsq", BF16), ("sd", F32)):
            tm[nm] = [sbuf(es, "pj_%s%d" % (nm, k), [128, 512], dtp) for k in range(2)]
            tm["T_" + nm] = [Trk(), Trk()]
        for nm, dtp in (("t1", F32), ("t2", F32)):
            buf = sbuf(es, "pj_%s" % nm, [128, 512], dtp)
            tk = Trk()
            tm[nm] = [buf, buf]
            tm["T_" + nm] = [tk, tk]
        tm["CT"] = sbuf(es, "pj_CT", [128, S], BF16)
        tm["ST"] = sbuf(es, "pj_ST", [128, S], BF16)
        tm["T_tl"] = Trk()
        P.dma("sync", tm["CT"][:], tab_d[0], writes=[tm["T_tl"]])
        P.dma("sync", tm["ST"][:], tab_d[1], writes=[tm["T_tl"]])
        proj_fm.tmp = tm

    def load_w_in(Wt, T_W, l, segs):
        wv = win_d[l].rearrange("(c p) f -> p c f", p=128)
        for (d0, s0, n) in segs:
            tk = Trk()
            T_W.append(tk)
            P.dma("gpsimd", Wt[:, :, d0:d0 + n], wv[:, :, s0:s0 + n], writes=[tk])

    def proj_tm(HT, T_HT, Wv, T_Wv, ncol, t, pbank):
        pv = PSB[pbank]
        P.group("tensor", [(lambda e, c=c: e.matmul(pv[:, 0:ncol], lhsT=HT[:, c, t * 128:(t + 1) * 128], rhs=Wv[:, c, 0:ncol], start=(c == 0), stop=(c == 7))) for c in range(8)],
                reads=T_Wv + [T_HT[t]], writes=[T_ps[pbank]])
        return pv

    def transpose_out(o_tile, T_o, ncol, OT, T_OT, chunk0, t):
        n = ncol // 128
        psT = PSH[:, 0:n * 128].rearrange("p (c t) -> p c t", c=n)
        P.group("tensor", [(lambda e, j=j: e.transpose(out=psT[:, j, :], in_=o_tile[:, j * 128:(j + 1) * 128], identity=identb[:])) for j in range(n)],
                reads=[T_o, T_cst], writes=[T_psh])
        P.op("scalar", lambda e: e.copy(out=OT[:, chunk0:chunk0 + n, t * 128:(t + 1) * 128], in_=psT), reads=[T_psh], writes=[T_OT[t]])

    def mixer_A(l, HT, T_HT, OT, T_OT):
        with contextlib.ExitStack() as es:
            QA = sbuf(es, "a_qa", [128, 2, S], BF16)
            KA = sbuf(es, "a_ka", [128, S], BF16)
            QI = sbuf(es, "a_qi", [128, 4, S], BF16)
            KI = sbuf(es, "a_ki", [128, S], BF16)
            VA = sbuf(es, "a_va", [128, NT, 65], BF16)
            WI = sbuf(es, "a_wi", [128, NT, 8], F32)
            T_Q = [Trk() for _ in range(4)]
            T_V = [Trk() for _ in range(NT)]
            with contextlib.ExitStack() as es1:
                W = sbuf(es1, "a_w", [128, 8, 1024], BF16)
                Wv = sbuf(es1, "a_wv", [128, 8, 72], BF16)
                T_W = []
                T_Wv = []
                proj_tmp(es1)
                load_w_in(W, T_W, l, [(0, OFF["qa"], 256), (256, OFF["ka"], 64), (320, OFF["ka"], 64),
                                      (384, OFF["qi"], 512), (896, OFF["ki"], 64), (960, OFF["ki"], 64)])
                load_w_in(Wv, T_Wv, l, [(0, OFF["va"], 64), (64, OFF["wi"], 8)])
                P.op("vector", lambda e: e.memset(VA[:, :, 64:65], 1.0), writes=T_V)
                gq = qkgT[:, l * 6 + 0:l * 6 + 1]
                gk = qkgT[:, l * 6 + 1:l * 6 + 2]
                for tg in range(4):
                    tsl = slice(tg * 512, (tg + 1) * 512)
                    for p in range(2):
                        proj_fm(es1, HT, T_HT, W, T_W, p * 128, tg, QA[:, p, tsl], T_Q[tg], "norm", gq)
                    proj_fm(es1, HT, T_HT, W, T_W, 256, tg, KA[:, tsl], T_Q[tg], "norm", gk)
                    for p in range(4):
                        proj_fm(es1, HT, T_HT, W, T_W, 384 + p * 128, tg, QI[:, p, tsl], T_Q[tg], "rope", None)
                    proj_fm(es1, HT, T_HT, W, T_W, 896, tg, KI[:, tsl], T_Q[tg], "rope", None)
                proj_flush()
                for t in range(NT):
                    pv = proj_tm(HT, T_HT, Wv, T_Wv, 72, t, 6)
                    P.op("scalar", lambda e, t=t, pv=pv: e.copy(out=VA[:, t, 0:64], in_=pv[:, 0:64]), reads=[T_ps[6]], writes=[T_V[t]])
                    P.op("vector", lambda e, t=t, pv=pv: e.tensor_copy(out=WI[:, t, :], in_=pv[:, 64:72]), reads=[T_ps[6]], writes=[T_V[t]])
                P.barrier()
                P.flush()
            if SUB == 1:
                return
            with contextlib.ExitStack() as es2:
                ISC = [sbuf(es2, "a_isc%d" % k, [128, S], F32) for k in range(2)]
                M = sbuf(es2, "a_m", [128, S], BF16)
                MT = sbuf(es2, "a_mt", [128, NT, 128], BF16)
                RL = [sbuf(es2, "a_rl%d" % k, [128, 512], F32) for k in range(2)]
                bis = sbuf(es2, "a_bis", [128, 32], F32)
                stp = sbuf(es2, "a_stp", [128, NBIS], F32)
                oa = sbuf(es2, "a_oa", [128, 256], BF16)
                rc = sbuf(es2, "a_rc", [128, 4], F32)
                J2 = sbuf(es2, "a_j2", [128, S], BF16)
                T_J2, T_bis2 = Trk(), Trk()
                T_isc = [Trk(), Trk()]
                T_dead = [Trk(), Trk()]
                T_M, T_MT, T_bis, T_oa = Trk(), Trk(), Trk(), Trk()
                T_PTk = [Trk() for _ in range(NT)]
                T_RL = [Trk(), Trk()]
                itc = [0, 0]

                def indexer(qt):
                    ib = qt % 2
                    L = 128 * (qt + 1)
                    qsl = slice(qt * 128, (qt + 1) * 128)
                    tgq = qt // 4
                    nch = (L + 511) // 512
                    for ch in range(nch):
                        c0 = ch * 512
                        cw = min(512, L - c0)
                        for h in range(8):
                            k = itc[0] % 2
                            itc[0] += 1
                            hp = (h % 2) * 64
                            pl = PSB[k]
                            P.group("tensor", [lambda e: e.matmul(pl[:, 0:cw], lhsT=QI[hp:hp + 64, h // 2, qsl], rhs=KI[hp:hp + 64, c0:c0 + cw], start=True, stop=True)],
                                    reads=T_Q[0:tgq + 1], writes=[T_ps[k]])
                            if h == 0:
                                P.op("vector", lambda e: e.tensor_scalar(out=ISC[ib][:, c0:c0 + cw], in0=pl[:, 0:cw], scalar1=0.0, scalar2=WI[:, qt, h:h + 1], op0=ALU.max, op1=ALU.mult),
                                     reads=[T_ps[k], T_V[qt]], writes=[T_isc[ib]])
                            else:
                                P.op("vector", lambda e: e.tensor_scalar(out=RL[k][:, 0:cw], in0=pl[:, 0:cw], scalar1=0.0, scalar2=WI[:, qt, h:h + 1], op0=ALU.max, op1=ALU.mult),
                                     reads=[T_ps[k], T_V[qt]], writes=[T_RL[k]])
                                P.op("gpsimd", lambda e: e.tensor_tensor(out=ISC[ib][:, c0:c0 + cw], in0=ISC[ib][:, c0:c0 + cw], in1=RL[k][:, 0:cw], op=ALU.add),
                                     reads=[T_RL[k], T_isc[ib]], writes=[T_isc[ib]])
                            yield

                def rest(qt):
                    ib = qt % 2
                    L = 128 * (qt + 1)
                    qsl = slice(qt * 128, (qt + 1) * 128)
                    tgq = qt // 4
                    isc = ISC[ib]
                    PT = isc[:, :].bitcast(BF16).rearrange("p (k h q) -> p k h q", k=NT, h=2)
                    Vb = lambda fn: P.op("vector", fn, reads=[T_isc[ib], T_bis, T_cst], writes=[T_bis])
                    if qt >= 2:
                        Vb(lambda e: e.tensor_reduce(out=bis[:, 0:1], in_=isc[:, 0:L], axis=AX.X, op=ALU.min))
                        Vb(lambda e: e.tensor_reduce(out=bis[:, 1:2], in_=isc[:, 0:L], axis=AX.X, op=ALU.max))
                        yield
                    P.op("gpsimd", lambda e: e.affine_select(out=isc[:, L - 128:L], in_=isc[:, L - 128:L], pattern=[[-1, 128]], compare_op=ALU.is_ge, fill=NEG, base=0, channel_multiplier=1),
                         reads=[T_isc[ib], T_bis], writes=[T_isc[ib]])
                    if qt >= 2:
                        Vb(lambda e: e.tensor_tensor(out=bis[:, 2:3], in0=bis[:, 1:2], in1=bis[:, 0:1], op=ALU.subtract))
                        Vb(lambda e: e.tensor_scalar(out=bis[:, 2:3], in0=bis[:, 2:3], scalar1=1.0001, scalar2=1e-20, op0=ALU.mult, op1=ALU.add))
                        Vb(lambda e: e.tensor_scalar(out=stp[:, :], in0=cstf[:, C_POW:C_POW + NBIS], scalar1=bis[:, 2:3], scalar2=None, op0=ALU.mult))
                        for i in range(NBIS):
                            Vb(lambda e: e.scalar_tensor_tensor(out=bis[:, 3:4], in0=bis[:, 0:1], scalar=-1.0, in1=stp[:, i:i + 1], op0=ALU.mult, op1=ALU.subtract))
                            P.op("scalar", lambda e: e.activation(out=M[:, 0:L], in_=isc[:, 0:L], func=AF.Sign, bias=bis[:, 3:4], scale=1.0, accum_out=bis[:, 4:5]),
                                 reads=[T_isc[ib], T_bis], writes=[T_M, T_bis])
                            P.op("vector", lambda e: e.scalar_tensor_tensor(out=bis[:, 6:7], in0=stp[:, i:i + 1], scalar=2.0, in1=bis[:, 0:1], op0=ALU.mult, op1=ALU.add),
                                 reads=[T_bis, T_bis2], writes=[T_bis2])
                            P.op("vector", lambda e: e.tensor_scalar(out=J2[:, 0:L], in0=isc[:, 0:L], scalar1=bis[:, 6:7], scalar2=0.0, op0=ALU.is_ge, op1=ALU.add, accum_out=bis[:, 7:8]),
                                 reads=[T_isc[ib], T_bis2], writes=[T_J2, T_bis2])
                            Vb(lambda e: e.tensor_scalar(out=bis[:, 5:6], in0=bis[:, 4:5], scalar1=float(511 - L), scalar2=None, op0=ALU.is_ge))
                            P.op("vector", lambda e: e.scalar_tensor_tensor(out=bis[:, 5:6], in0=bis[:, 7:8], scalar=255.5, in1=bis[:, 5:6], op0=ALU.is_ge, op1=ALU.add),
                                 reads=[T_bis, T_bis2], writes=[T_bis])
                            Vb(lambda e: e.scalar_tensor_tensor(out=bis[:, 0:1], in0=bis[:, 5:6], scalar=stp[:, i:i + 1], in1=bis[:, 0:1], op0=ALU.mult, op1=ALU.add))
                            yield
                        P.op("vector", lambda e: e.tensor_scalar(out=M[:, 0:L], in0=isc[:, 0:L], scalar1=bis[:, 0:1], scalar2=None, op0=ALU.is_ge),
                             reads=[T_isc[ib], T_bis], writes=[T_M, T_dead[ib]])
                    else:
                        P.op("vector", lambda e: e.tensor_scalar(out=M[:, 0:L], in0=isc[:, 0:L], scalar1=-1.0e29, scalar2=None, op0=ALU.is_ge),
                             reads=[T_isc[ib], T_bis], writes=[T_M, T_dead[ib]])
                    yield
                    for k0 in range(0, qt + 1, 8):
                        n = min(8, qt + 1 - k0)
                        psT = PSH[:, 0:n * 128].rearrange("p (c t) -> p c t", c=n)
                        P.group("tensor", [(lambda e, j=j: e.transpose(out=psT[:, j, :], in_=M[:, (k0 + j) * 128:(k0 + j + 1) * 128], identity=identb[:])) for j in range(n)],
                                reads=[T_M, T_cst], writes=[T_psh])
                        P.op("scalar", lambda e: e.copy(out=MT[:, k0:k0 + n, :], in_=psT), reads=[T_psh], writes=[T_MT])
                        yield
                    po = PSB[6]
                    for pr in range(2):
                        for kt in range(qt + 1):
                            k = itc[1] % 2
                            itc[1] += 1
                            ksl = slice(kt * 128, (kt + 1) * 128)
                            for hh in range(2):
                                pb = 2 + 2 * hh + k
                                ps_s = PSB[pb]
                                P.group("tensor", [lambda e: e.matmul(ps_s[:, 0:128], lhsT=KA[hh * 64:hh * 64 + 64, ksl], rhs=QA[hh * 64:hh * 64 + 64, pr, qsl], start=True, stop=True)],
                                        reads=T_Q[0:tgq + 1], writes=[T_ps[pb]])
                                P.op("scalar", lambda e: e.activation(out=PT[:, kt, hh, :], in_=ps_s[:, 0:128], func=AF.Exp, scale=0.125), reads=[T_ps[pb], T_dead[ib]], writes=[T_PTk[kt]])
                            P.op("vector", lambda e: e.tensor_tensor(out=PT[:, kt, :, :], in0=PT[:, kt, :, :], in1=MT[:, kt:kt + 1, :].to_broadcast([128, 2, 128]), op=ALU.mult),
                                 reads=[T_PTk[kt], T_MT], writes=[T_PTk[kt]])
                            yield
                        pov = po[:, pr * 130:(pr + 1) * 130].rearrange("p (h e) -> p h e", h=2)
                        for hh in range(2):
                            P.group("tensor", [(lambda e, kt=kt: e.matmul(pov[:, hh, :], lhsT=PT[:, kt, hh, :], rhs=VA[:, kt, :], start=(kt == 0), stop=(kt == qt))) for kt in range(qt + 1)],
                                    reads=T_PTk[0:qt + 1] + T_V[0:qt + 1] + [T_isc[ib]], writes=[T_ps[6]])
                        P.op("vector", lambda e: e.reciprocal(out=rc[:, 2 * pr:2 * pr + 2], in_=pov[:, :, 64]), reads=[T_ps[6]], writes=[T_bis])
                        P.op("vector", lambda e: e.tensor_tensor(out=oa[:, pr * 128:(pr + 1) * 128].rearrange("p (h e) -> p h e", h=2), in0=pov[:, :, 0:64],
                                                                 in1=rc[:, 2 * pr:2 * pr + 2].unsqueeze(2).to_broadcast([128, 2, 64]), op=ALU.mult),
                             reads=[T_ps[6], T_bis], writes=[T_oa])
                        yield
                    transpose_out(oa, T_oa, 256, OT, T_OT, 0, qt)
                    yield

                def interleave(ga, gb):
                    da = db = False
                    while not (da and db):
                        if not da:
                            try:
                                next(ga)
                            except StopIteration:
                                da = True
                        if not db:
                            try:
                                next(gb)
                            except StopIteration:
                                db = True

                for _ in indexer(0):
                    pass
                for qt in range(NT):
                    interleave(rest(qt), indexer(qt + 1) if qt + 1 < NT else iter(()))
                P.barrier()
                P.flush()

    def mixer_B(l, HT, T_HT, OT, T_OT):
        with contextlib.ExitStack() as es:
            QB = sbuf(es, "b_q", [128, 2, S], BF16)
            KB = sbuf(es, "b_k", [128, 2, S], BF16)
            VB = sbuf(es, "b_v", [128, NT, 4, 65], BF16)
            KM = sbuf(es, "b_km", [128, 2, 8], BF16)
            T_Q = [Trk() for _ in range(4)]
            T_V = [Trk() for _ in range(NT)]
            T_KM = Trk()
            with contextlib.ExitStack() as es1:
                W = sbuf(es1, "b_w", [128, 8, 512], BF16)
                Wv = sbuf(es1, "b_wv", [128, 8, 256], BF16)
                kmf = sbuf(es1, "b_kmf", [128, 2, 8], F32)
                T_W, T_Wv = [], []
                proj_tmp(es1)
                load_w_in(W, T_W, l, [(0, OFF["qb"], 256), (256, OFF["kb"], 256)])
                load_w_in(Wv, T_Wv, l, [(0, OFF["vb"], 256)])
                P.op("vector", lambda e: e.memset(VB[:, :, :, 64:65], 1.0), writes=T_V)
                gq = qkgT[:, l * 6 + 2:l * 6 + 3]
                gk = qkgT[:, l * 6 + 3:l * 6 + 4]
                for tg in range(4):
                    tsl = slice(tg * 512, (tg + 1) * 512)
                    for p in range(2):
                        proj_fm(es1, HT, T_HT, W, T_W, p * 128, tg, QB[:, p, tsl], T_Q[tg], "norm", gq)
                        proj_fm(es1, HT, T_HT, W, T_W, 256 + p * 128, tg, KB[:, p, tsl], T_Q[tg], "norm", gk)
                proj_flush()
                for t in range(NT):
                    pv = proj_tm(HT, T_HT, Wv, T_Wv, 256, t, 6)
                    P.op("scalar", lambda e, t=t, pv=pv: e.copy(out=VB[:, t, :, 0:64], in_=pv[:, 0:256].rearrange("p (h e) -> p h e", h=4)), reads=[T_ps[6]], writes=[T_V[t]])
                for p in range(2):
                    P.op("vector", lambda e, p=p: e.tensor_reduce(out=kmf[:, p, :], in_=KB[:, p, :].rearrange("p (n k) -> p n k", n=8), axis=AX.X, op=ALU.add), reads=T_Q, writes=[T_KM])
                P.op("vector", lambda e: e.tensor_scalar(out=KM[:, :, :], in0=kmf[:, :, :], scalar1=1.0 / 256, scalar2=None, op0=ALU.mult), reads=[T_KM], writes=[T_KM])
                P.barrier()
                P.flush()
            with contextlib.ExitStack() as es2:
                PT = [sbuf(es2, "b_pt%d" % k, [128, 2, 256], BF16) for k in range(3)]
                T_PT = [Trk() for _ in range(3)]
                gate = sbuf(es2, "b_gate", [128, 2, 4, 8], F32)
                top8 = sbuf(es2, "b_top8", [128, 8], F32)
                BM = sbuf(es2, "b_bm", [128, 2, 4, 8], F32)
                acc = sbuf(es2, "b_acc", [128, 2, 4, 65], F32)
                ob = sbuf(es2, "b_ob", [128, 2, 256], BF16)
                rc = sbuf(es2, "b_rc", [128, 2, 4], F32)
                T_g, T_ob = Trk(), Trk()
                T_acc = [Trk() for _ in range(4)]
                itb = [0]
                for j in range(8):
                    tgq = j // 2
                    if j > 0:
                        for par in range(2):
                            pg = PSB[6]
                            pgv = pg[:, 0:64].rearrange("p (a h n) -> p a h n", a=2, h=4)
                            P.group("tensor", [(lambda e, a=a, h=h: e.matmul(pgv[:, a, h, :], lhsT=QB[(h % 2) * 64:(h % 2) * 64 + 64, h // 2, (2 * j + a) * 128:(2 * j + a + 1) * 128],
                                                                              rhs=KM[(h % 2) * 64:(h % 2) * 64 + 64, h // 2, :], start=True, stop=True)) for a in range(2) for h in (par, par + 2)],
                                    reads=[T_Q[tgq], T_KM], writes=[T_ps[6]])
                            for h in (par, par + 2):
                                P.op("vector", lambda e: e.tensor_copy(out=gate[:, :, h, :], in_=pgv[:, :, h, :]), reads=[T_ps[6]], writes=[T_g])
                        P.op("vector", lambda e: e.memset(gate[:, :, :, j:8], NEG), reads=[T_g], writes=[T_g])
                        for a in range(2):
                            for h in range(4):
                                P.op("vector", lambda e: e.max(out=top8[:, :], in_=gate[:, a, h, :]), reads=[T_g], writes=[T_g])
                                P.op("vector", lambda e: e.tensor_scalar(out=BM[:, a, h, :], in0=gate[:, a, h, :], scalar1=top8[:, 2:3], scalar2=None, op0=ALU.is_ge), reads=[T_g], writes=[T_g])
                    pend = []

                    def stage_a(h, n):
                        hp = (h % 2) * 64
                        pp = h // 2
                        qs_all = slice((2 * j) * 128, (2 * j + 2) * 128)
                        k = itb[0] % 3
                        itb[0] += 1
                        ps_s = PSB[k]
                        psv = ps_s[:, 0:512].rearrange("p (c q) -> p c q", c=2)
                        nb = j if n is None else n
                        P.group("tensor", [(lambda e, c=c: e.matmul(psv[:, c, :], lhsT=KB[hp:hp + 64, pp, (2 * nb + c) * 128:(2 * nb + c + 1) * 128], rhs=QB[hp:hp + 64, pp, qs_all], start=True, stop=True)) for c in range(2)],
                                reads=[T_Q[tgq], T_Q[nb // 2]], writes=[T_ps[k]])
                        P.op("scalar", lambda e: e.activation(out=PT[k][:, :, :], in_=psv, func=AF.Exp, scale=0.125), reads=[T_ps[k]], writes=[T_PT[k]])
                        if n is None:
                            P.op("vector", lambda e: e.tensor_tensor(out=PT[k][:, 0, 0:128], in0=PT[k][:, 0, 0:128], in1=trib[:], op=ALU.mult), reads=[T_PT[k], T_cst], writes=[T_PT[k]])
                            P.op("vector", lambda e: e.tensor_tensor(out=PT[k][:, 1, 128:256], in0=PT[k][:, 1, 128:256], in1=trib[:], op=ALU.mult), reads=[T_PT[k], T_cst], writes=[T_PT[k]])
                        pend.append((h, n, k))

                    def stage_b():
                        h, n, k = pend.pop(0)
                        po = PSB[3 + k]
                        pov = po[:, 0:130].rearrange("p (a e) -> p a e", a=2)
                        if n is None:
                            P.group("tensor", [lambda e: e.matmul(pov[:, 0, :], lhsT=PT[k][:, 0, 0:128], rhs=VB[:, 2 * j, h, :], start=True, stop=True),
                                               lambda e: e.matmul(pov[:, 1, :], lhsT=PT[k][:, 0, 128:256], rhs=VB[:, 2 * j, h, :], start=True, stop=False),
                                               lambda e: e.matmul(pov[:, 1, :], lhsT=PT[k][:, 1, 128:256], rhs=VB[:, 2 * j + 1, h, :], start=False, stop=True)],
                                    reads=[T_PT[k], T_V[2 * j], T_V[2 * j + 1]], writes=[T_ps[3 + k]])
                            P.op("vector", lambda e: e.tensor_copy(out=acc[:, :, h, :], in_=pov), reads=[T_ps[3 + k]], writes=[T_acc[h]])
                        else:
                            P.group("tensor", [(lambda e, c=c, a=a: e.matmul(pov[:, a, :], lhsT=PT[k][:, c, a * 128:(a + 1) * 128], rhs=VB[:, 2 * n + c, h, :], start=(c == 0), stop=(c == 1))) for a in range(2) for c in range(2)],
                                    reads=[T_PT[k], T_V[2 * n], T_V[2 * n + 1]], writes=[T_ps[3 + k]])
                            for a in range(2):
                                P.op("vector", lambda e: e.scalar_tensor_tensor(out=acc[:, a, h, :], in0=pov[:, a, :], scalar=BM[:, a, h, n:n + 1], in1=acc[:, a, h, :], op0=ALU.mult, op1=ALU.add),
                                     reads=[T_ps[3 + k], T_g, T_acc[h]], writes=[T_acc[h]])

                    for h in range(4):
                        for n in [None] + list(range(j)):
                            stage_a(h, n)
                            if len(pend) > 2:
                                stage_b()
                    while pend:
                        stage_b()
                    P.op("vector", lambda e: e.reciprocal(out=rc[:, :, :], in_=acc[:, :, :, 64]), reads=T_acc, writes=[T_g])
                    for a in range(2):
                        P.op("vector", lambda e, a=a: e.tensor_tensor(out=ob[:, a, :].rearrange("p (h e) -> p h e", h=4), in0=acc[:, a, :, 0:64], in1=rc[:, a, :].unsqueeze(2).to_broadcast([128, 4, 64]), op=ALU.mult),
                             reads=T_acc + [T_g], writes=[T_ob])
                        transpose_out(ob[:, a, :], T_ob, 256, OT, T_OT, 2, 2 * j + a)
                P.barrier()
                P.flush()

    def mixer_C(l, HT, T_HT, OT, T_OT, half):
        with contextlib.ExitStack() as es:
            QC = sbuf(es, "c_q", [128, 2, S], BF16)
            KC = sbuf(es, "c_k", [128, 2, S], BF16)
            VC = sbuf(es, "c_v", [128, NT, 2, 129], BF16)
            T_Q = [Trk() for _ in range(4)]
            T_V = [Trk() for _ in range(NT)]
            with contextlib.ExitStack() as es1:
                W = sbuf(es1, "c_w", [128, 8, 512], BF16)
                Wv = sbuf(es1, "c_wv", [128, 8, 256], BF16)
                T_W, T_Wv = [], []
                proj_tmp(es1)
                load_w_in(W, T_W, l, [(0, OFF["qc"] + half * 256, 256), (256, OFF["kc"] + half * 256, 256)])
                load_w_in(Wv, T_Wv, l, [(0, OFF["vc"] + half * 256, 256)])
                P.op("vector", lambda e: e.memset(VC[:, :, :, 128:129], 1.0), writes=T_V)
                gq = qkgT[:, l * 6 + 4:l * 6 + 5]
                gk = qkgT[:, l * 6 + 5:l * 6 + 6]
                for tg in range(4):
                    tsl = slice(tg * 512, (tg + 1) * 512)
                    for p in range(2):
                        proj_fm(es1, HT, T_HT, W, T_W, p * 128, tg, QC[:, p, tsl], T_Q[tg], "norm", gq)
                        proj_fm(es1, HT, T_HT, W, T_W, 256 + p * 128, tg, KC[:, p, tsl], T_Q[tg], "norm", gk)
                proj_flush()
                for t in range(NT):
                    pv = proj_tm(HT, T_HT, Wv, T_Wv, 256, t, 6)
                    P.op("scalar", lambda e, t=t, pv=pv: e.copy(out=VC[:, t, :, 0:128], in_=pv[:, 0:256].rearrange("p (h e) -> p h e", h=2)), reads=[T_ps[6]], writes=[T_V[t]])
                P.barrier()
                P.flush()
            with contextlib.ExitStack() as es2:
                PT = [sbuf(es2, "c_pt%d" % k, [128, NT, 512], BF16) for k in range(2)]
                T_PTk = [[Trk() for _ in range(NT)] for _ in range(2)]
                t0 = sbuf(es2, "c_t0", [128, 4, 128], F32)
                o32 = sbuf(es2, "c_o32", [128, 2, 4, 128], F32)
                oc = sbuf(es2, "c_oc", [128, 4, 256], BF16)
                sq = sbuf(es2, "c_sq", [128, 128], F32)
                st = sbuf(es2, "c_st", [128, 32], F32)
                T_t0, T_o32, T_oc, T_st, T_ss = Trk(), Trk(), Trk(), Trk(), Trk()
                itc = [0, 0]
                units = [(G, hh, c) for G in range(4) for hh in range(2) for c in range(2)]

                def st_gen(u):
                    G, hh, c = units[u]
                    pbf = u % 2
                    cp = c * 64
                    nkt = 4 * G + 4
                    for kt in range(nkt):
                        k = itc[0] % 3
                        itc[0] += 1
                        ps_s = PSB[k]
                        qs0 = max(kt - 4 * G, 0)
                        q0 = (4 * G + qs0) * 128
                        nq = (4 - qs0) * 128
                        P.group("tensor", [lambda e: e.matmul(ps_s[:, 0:nq], lhsT=KC[cp:cp + 64, hh, kt * 128:(kt + 1) * 128], rhs=QC[cp:cp + 64, hh, q0:q0 + nq], start=True, stop=True)],
                                reads=[T_Q[G], T_Q[kt // 4]], writes=[T_ps[k]])
                        P.op("scalar", lambda e: e.activation(out=PT[pbf][:, kt, qs0 * 128:qs0 * 128 + nq], in_=ps_s[:, 0:nq], func=AF.Exp, scale=0.125), reads=[T_ps[k]], writes=[T_PTk[pbf][kt]])
                        if kt >= 4 * G:
                            P.op("vector", lambda e: e.tensor_tensor(out=PT[pbf][:, kt, qs0 * 128:(qs0 + 1) * 128], in0=PT[pbf][:, kt, qs0 * 128:(qs0 + 1) * 128], in1=trib[:], op=ALU.mult),
                                 reads=[T_PTk[pbf][kt], T_cst], writes=[T_PTk[pbf][kt]])
                        yield

                def pv_gen(u):
                    G, hh, c = units[u]
                    pbf = u % 2
                    for qs in range(4):
                        pb = 3 + (itc[1] % 3)
                        itc[1] += 1
                        po = PSB[pb]
                        nk = 4 * G + qs + 1
                        P.group("tensor", [(lambda e, kt=kt: e.matmul(po[:, 0:129], lhsT=PT[pbf][:, kt, qs * 128:(qs + 1) * 128], rhs=VC[:, kt, hh, :], start=(kt == 0), stop=(kt == nk - 1))) for kt in range(nk)],
                                reads=T_PTk[pbf][0:nk] + T_V[0:nk], writes=[T_ps[pb]])
                        P.op("vector", lambda e: e.reciprocal(out=st[:, qs * 2 + c:qs * 2 + c + 1], in_=po[:, 128:129]), reads=[T_ps[pb]], writes=[T_st])
                        if c == 0:
                            P.op("vector", lambda e: e.tensor_scalar(out=t0[:, qs, :], in0=po[:, 0:128], scalar1=st[:, qs * 2:qs * 2 + 1], scalar2=None, op0=ALU.mult),
                                 reads=[T_ps[pb], T_st], writes=[T_t0])
                        else:
                            P.op("vector", lambda e: e.tensor_tensor(out=st[:, 8 + qs:9 + qs], in0=st[:, qs * 2 + 1:qs * 2 + 2], in1=lamv[:, l:l + 1], op=ALU.mult), reads=[T_st, T_lam], writes=[T_st])
                            P.op("vector", lambda e: e.scalar_tensor_tensor(out=o32[:, hh, qs, :], in0=po[:, 0:128], scalar=st[:, 8 + qs:9 + qs], in1=t0[:, qs, :], op0=ALU.mult, op1=ALU.add),
                                 reads=[T_ps[pb], T_st, T_t0], writes=[T_o32])
                            P.op("vector", lambda e: e.tensor_tensor(out=sq[:], in0=o32[:, hh, qs, :], in1=o32[:, hh, qs, :], op=ALU.mult), reads=[T_o32], writes=[T_st])
                            P.op("vector", lambda e: e.reduce_sum(out=st[:, 16 + hh * 4 + qs:17 + hh * 4 + qs], in_=sq[:], axis=AX.X), reads=[T_st], writes=[T_st, T_ss])
                        yield
                    if hh == 1 and c == 1:
                        P.op("scalar", lambda e: e.activation(out=st[:, 24:32], in_=st[:, 16:24], func=AF.Sqrt, scale=1.0 / 128, bias=EPS), reads=[T_ss, T_st], writes=[T_ss])
                        P.op("vector", lambda e: e.reciprocal(out=st[:, 24:32], in_=st[:, 24:32]), reads=[T_ss], writes=[T_ss])
                        P.op("vector", lambda e: e.tensor_scalar(out=st[:, 24:32], in0=st[:, 24:32], scalar1=float(1.0 - lam_init(l)), scalar2=None, op0=ALU.mult), reads=[T_ss], writes=[T_ss])
                        for h2 in range(2):
                            for qs in range(4):
                                P.op("vector", lambda e: e.scalar_tensor_tensor(out=oc[:, qs, h2 * 128:(h2 + 1) * 128], in0=o32[:, h2, qs, :], scalar=st[:, 24 + h2 * 4 + qs:25 + h2 * 4 + qs],
                                                                              in1=subg[:, l * 128:(l + 1) * 128], op0=ALU.mult, op1=ALU.mult),
                                     reads=[T_o32, T_ss, T_cst], writes=[T_oc])
                        yield
                        for qs in range(4):
                            transpose_out(oc[:, qs, :], T_oc, 256, OT, T_OT, 4 + 2 * half, 4 * G + qs)
                        yield

                for _ in st_gen(0):
                    pass
                for u in range(len(units)):
                    ga = pv_gen(u)
                    gb = st_gen(u + 1) if u + 1 < len(units) else iter(())
                    nb = (4 * units[u + 1][0] + 4) if u + 1 < len(units) else 0
                    per = max(1, (nb + 3) // 4)
                    da = db = False
                    while not (da and db):
                        if not db:
                            for _ in range(per):
                                try:
                                    next(gb)
                                except StopIteration:
                                    db = True
                                    break
                        if not da:
                            try:
                                next(ga)
                            except StopIteration:
                                da = True
                P.barrier()
                P.flush()

    def mixer_out(l, HT, T_HT, OT, T_OT):
        with contextlib.ExitStack() as es:
            MG = sbuf(es, "o_mg", [128, 8, S], BF16)
            T_MG = [Trk() for _ in range(4)]
            WO = sbuf(es, "o_wo", [128, 8, D], BF16)
            T_WO = [Trk(), Trk()]
            wov = wout_d[l].rearrange("(c p) f -> p c f", p=128)
            es_a = contextlib.ExitStack()
            WBR = [sbuf(es_a, "o_wbr%d" % k, [128, 8, 128], BF16) for k in range(2)]
            WGT = [sbuf(es_a, "o_wgt%d" % k, [128, 8, 384], BF16) for k in range(2)]
            sg = [sbuf(es_a, "o_sg%d" % k, [128, 512], F32) for k in range(2)]
            mg32 = [sbuf(es_a, "o_m32%d" % k, [128, 512], F32) for k in range(2)]
            T_Wb = [Trk(), Trk()]
            T_Wg = [[Trk() for _ in range(3)] for _ in range(2)]
            T_sg = [Trk(), Trk()]
            T_m32 = [Trk(), Trk()]
            wbv = wbr_d[l].rearrange("(c p) f -> p c f", p=128)
            wiv = win_d[l].rearrange("(c p) f -> p c f", p=128)
            feat = [(0, 2), (2, 2), (4, 4)]
            it = 0
            for dc in range(8):
                wb = dc % 2
                P.dma("gpsimd", WBR[wb][:, :, :], wbv[:, :, dc * 128:(dc + 1) * 128], writes=[T_Wb[wb]])
                for br, nm in enumerate(("ga", "gb", "gc")):
                    P.dma("gpsimd", WGT[wb][:, :, br * 128:(br + 1) * 128], wiv[:, :, OFF[nm] + dc * 128:OFF[nm] + (dc + 1) * 128], writes=[T_Wg[wb][br]])
                if dc == 1:
                    for hf in range(2):
                        P.dma("gpsimd", WO[:, :, hf * 512:(hf + 1) * 512], wov[:, :, hf * 512:(hf + 1) * 512], writes=[T_WO[hf]])
                for tg in range(4):
                    tsl = slice(tg * 512, (tg + 1) * 512)
                    mk = it % 2
                    it += 1
                    for br in range(3):
                        k = (it + br) % 2
                        pgt = PSB[k]
                        py = PSB[2 + k]
                        c0, ncn = feat[br]
                        P.group("tensor", [(lambda e, c=c, br=br, pgt=pgt, wb=wb: e.matmul(pgt[:, :], lhsT=WGT[wb][:, c, br * 128:(br + 1) * 128], rhs=HT[:, c, tsl], start=(c == 0), stop=(c == 7))) for c in range(8)],
                                reads=[T_Wg[wb][br]] + T_HT[tg * 4:tg * 4 + 4], writes=[T_ps[k]])
                        P.group("tensor", [(lambda e, c=c, c0=c0, ncn=ncn, py=py, wb=wb: e.matmul(py[:, :], lhsT=WBR[wb][:, c0 + c, :], rhs=OT[:, c0 + c, tsl], start=(c == 0), stop=(c == ncn - 1))) for c in range(ncn)],
                                reads=[T_Wb[wb]] + T_OT[tg * 4:tg * 4 + 4], writes=[T_ps[2 + k]])
                        P.op("scalar", lambda e, k=k, pgt=pgt: e.activation(out=sg[k][:], in_=pgt[:, :], func=AF.Sigmoid), reads=[T_ps[k]], writes=[T_sg[k]])
                        if br == 0:
                            P.op("vector", lambda e, k=k, py=py, mk=mk: e.tensor_tensor(out=mg32[mk][:], in0=py[:, :], in1=sg[k][:], op=ALU.mult), reads=[T_ps[2 + k], T_sg[k]], writes=[T_m32[mk]])
                        else:
                            P.op("vector", lambda e, k=k, py=py: e.tensor_tensor(out=sg[k][:], in0=py[:, :], in1=sg[k][:], op=ALU.mult), reads=[T_ps[2 + k], T_sg[k]], writes=[T_sg[k]])
                            if br == 1:
                                P.op("gpsimd", lambda e, k=k, mk=mk: e.tensor_tensor(out=mg32[mk][:], in0=mg32[mk][:], in1=sg[k][:], op=ALU.add), reads=[T_sg[k], T_m32[mk]], writes=[T_m32[mk]])
                            else:
                                P.op("gpsimd", lambda e, k=k, mk=mk, dc=dc: e.tensor_tensor(out=MG[:, dc, tsl], in0=mg32[mk][:], in1=sg[k][:], op=ALU.add), reads=[T_sg[k], T_m32[mk]], writes=[T_MG[tg]])
            for t in range(NT):
                for hf in range(2):
                    pb = 4 + (t * 2 + hf) % 2
                    pd = PSB[pb]
                    P.group("tensor", [(lambda e, c=c, t=t, hf=hf, pd=pd: e.matmul(pd[:, :], lhsT=MG[:, c, t * 128:(t + 1) * 128], rhs=WO[:, c, hf * 512:(hf + 1) * 512], start=(c == 0), stop=(c == 7))) for c in range(8)],
                            reads=[T_MG[t // 4], T_WO[hf]], writes=[T_ps[pb]])
                    P.op("vector", lambda e, t=t, hf=hf, pd=pd: e.tensor_tensor(out=X[:, t, hf * 512:(hf + 1) * 512], in0=pd[:, :], in1=X[:, t, hf * 512:(hf + 1) * 512], op=ALU.add),
                         reads=[T_ps[pb], T_X[t]], writes=[T_X[t]])
            P.barrier()
            P.flush()
            es_a.close()

    def mixer(l, b):
        with contextlib.ExitStack() as es:
            HT = sbuf(es, "m_HT", [128, 8, S], BF16)
            OT = sbuf(es, "m_OT", [128, 8, S], BF16)
            T_HT = [Trk() for _ in range(NT)]
            T_OT = [Trk() for _ in range(NT)]
            with contextlib.ExitStack() as esn:
                norm_T(esn, l, 1, HT, T_HT)
                P.barrier()
                P.flush()
            if stage >= 2:
                mixer_A(l, HT, T_HT, OT, T_OT)
            if stage >= 3:
                mixer_B(l, HT, T_HT, OT, T_OT)
            if stage >= 4:
                mixer_C(l, HT, T_HT, OT, T_OT, 0)
                mixer_C(l, HT, T_HT, OT, T_OT, 1)
            if dbg and l == layers[0] and b == 0:
                nch = {2: 2, 3: 4}.get(stage, 8)
                P.dma("sync", dbg_ot[:, 0:nch, :], OT[:, 0:nch, :], reads=T_OT, writes=[T_out])
            if stage >= 5:
                mixer_out(l, HT, T_HT, OT, T_OT)
            P.barrier()
            P.flush()

    for b in range(n_seq):
        for t in range(NT):
            P.dma("sync", X[:, t, :], x_d[b, t * 128:(t + 1) * 128, :], writes=[T_X[t]])
        rope_tables(b)
        for l in layers:
            if stage >= 1:
                ffn(l, 0)
            if stage >= 2:
                mixer(l, b)
            if stage >= 6:
                ffn(l, 1)
        for t in range(NT):
            P.dma("sync", out_d[b, t * 128:(t + 1) * 128, :], X[:, t, :], reads=[T_X[t]], writes=[Trk()])
    P.barrier()
    P.flush()
    es0.close()
    P.close()
    build.nins = P.nins
    return nc


def prep_shared(inputs):
    norm_g = np.asarray(inputs["norm_g"], np.float32)
    qk = np.asarray(inputs["qk_norm_g"], np.float32)
    ngT = np.ascontiguousarray(norm_g.reshape(2, 3, 8, 128).transpose(3, 0, 1, 2).reshape(128, 48))
    qkT = qk.reshape(12, 64).T
    qkgT = np.ascontiguousarray(np.concatenate([qkT, qkT], axis=0))
    return {
        "w_in": np.ascontiguousarray(inputs["w_in"], np.float32),
        "w_branch": np.ascontiguousarray(inputs["w_branch"], np.float32),
        "w_out": np.ascontiguousarray(inputs["w_out"], np.float32),
        "ffn_w_gate": np.ascontiguousarray(inputs["ffn_w_gate"], np.float32),
        "ffn_w_up": np.ascontiguousarray(inputs["ffn_w_up"], np.float32),
        "ffn_w_down": np.ascontiguousarray(inputs["ffn_w_down"], np.float32),
        "cst": make_consts(),
        "ngT": ngT,
        "qkgT": qkgT,
        "lamp": np.ascontiguousarray(np.asarray(inputs["lambda_params"], np.float32).reshape(512)),
        "subg": np.ascontiguousarray(np.asarray(inputs["diff_subln_g"], np.float32).reshape(256)),
    }


def kernel(**inputs):
    x = np.asarray(inputs["x"], np.float32)
    pos = np.asarray(inputs["positions"], np.int32)
    shared = prep_shared(inputs)
    nc = build(n_seq=2, layers=(0, 1))
    in_maps = []
    for c in range(NCORES):
        m = dict(shared)
        m["x"] = np.ascontiguousarray(x[2 * c:2 * c + 2])
        m["pos"] = np.ascontiguousarray(pos[2 * c:2 * c + 2])
        in_maps.append(m)
    res = run_bass_kernel_spmd(nc, in_maps, core_ids=list(range(NCORES)))
    out = np.concatenate([np.asarray(r["out"]) for r in res.results], axis=0)
    return out.astype(np.float32)
```
